# Optimizing a Trainium2 kernel written in Bass

```python
import math
import jax, jax.numpy as jnp
from jax import lax
import numpy as np

D_MODEL = 1024
BATCH = 2
SEQ = 8192
DEPTH = 2

N_MIXERS = 2
N_ATTN_LAYERS = (DEPTH + N_MIXERS - 1) // N_MIXERS
N_FOURIER_LAYERS = DEPTH // N_MIXERS
N_DIFF_HEADS = 8
DIFF_HEAD_DIM = D_MODEL // N_DIFF_HEADS // 2
DIFF_V_DIM = 2 * DIFF_HEAD_DIM
Q_BLOCK = 128
N_FOURIER_GROUPS = 4
FOURIER_GROUP_DIM = D_MODEL // N_FOURIER_GROUPS
N_EXPERT_GROUPS = 4
EXPERTS_PER_GROUP = 4
N_EXPERTS = N_EXPERT_GROUPS * EXPERTS_PER_GROUP
EXPERT_TOP_K = 2
D_EXPERT = D_MODEL // 2
N_ADA = 6
ADA_INIT_SCALE = 0.5
EPS = 1e-6

kernel_name = "hybrid_diffattn_fnet_hmoe_adaln"


def rmsnorm(x, g):
    xf = x.astype(jnp.float32)
    y = xf * lax.rsqrt(jnp.mean(xf * xf, axis=-1, keepdims=True) + EPS)
    return (y * g.astype(jnp.float32)).astype(x.dtype)


def modulate(h, shift, scale):
    return h * (1.0 + scale[:, None, :]) + shift[:, None, :]


def alibi_slopes(n_heads):
    return 2.0 ** (-8.0 * jnp.arange(1, n_heads + 1, dtype=jnp.float32) / n_heads)


def diff_attention(h, w_in, lam_q1, lam_k1, lam_q2, lam_k2, subln_g, w_out, layer_idx):
    B, S, D = h.shape
    H, dh, dv = N_DIFF_HEADS, DIFF_HEAD_DIM, DIFF_V_DIM
    qkv = h @ w_in
    q, k, v = jnp.split(qkv, 3, axis=-1)
    q = q.reshape(B, S, H, 2, dh) * (dh ** -0.5)
    k = k.reshape(B, S, H, 2, dh)
    v = v.reshape(B, S, H, dv)
    q1, q2 = q[..., 0, :], q[..., 1, :]
    k1, k2 = k[..., 0, :], k[..., 1, :]
    lam_init = 0.8 - 0.6 * math.exp(-0.3 * layer_idx)
    lam = (jnp.exp(jnp.sum(lam_q1.astype(jnp.float32) * lam_k1.astype(jnp.float32)))
           - jnp.exp(jnp.sum(lam_q2.astype(jnp.float32) * lam_k2.astype(jnp.float32)))
           + lam_init)
    slopes = alibi_slopes(H)
    pos = jnp.arange(S, dtype=jnp.float32)
    nb = S // Q_BLOCK
    q1b = q1.reshape(B, nb, Q_BLOCK, H, dh).transpose(1, 0, 3, 2, 4)
    q2b = q2.reshape(B, nb, Q_BLOCK, H, dh).transpose(1, 0, 3, 2, 4)
    posb = pos.reshape(nb, Q_BLOCK)

    def block(args):
        qa, qb, pq = args
        bias = -slopes[:, None, None] * jnp.abs(pq[:, None] - pos[None, :])
        s1 = jnp.einsum('bhqd,bkhd->bhqk', qa, k1, preferred_element_type=jnp.float32) + bias
        s2 = jnp.einsum('bhqd,bkhd->bhqk', qb, k2, preferred_element_type=jnp.float32) + bias
        a = jax.nn.softmax(s1, axis=-1) - lam * jax.nn.softmax(s2, axis=-1)
        return jnp.einsum('bhqk,bkhd->bqhd', a.astype(v.dtype), v)

    o = lax.map(block, (q1b, q2b, posb))
    o = o.transpose(1, 0, 2, 3, 4).reshape(B, S, H, dv)
    o = rmsnorm(o, subln_g) * (1.0 - lam_init)
    return o.reshape(B, S, H * dv) @ w_out


def fourier_mix(h, w_in, w_out):
    B, S, D = h.shape
    u = (h @ w_in).reshape(B, S, N_FOURIER_GROUPS, FOURIER_GROUP_DIM).astype(jnp.float32)
    f = jnp.fft.fft2(u, axes=(1, 3), norm="ortho").real
    return f.reshape(B, S, D).astype(h.dtype) @ w_out


def hier_moe(h, w_rg, b_rg, w_re, b_re, w_gate, w_up, w_down):
    B, S, D = h.shape
    t = h.reshape(B * S, D)
    T = t.shape[0]
    g_logits = (t @ w_rg).astype(jnp.float32) + b_rg.astype(jnp.float32)
    g_prob = jax.nn.softmax(g_logits, axis=-1)
    g_idx = jnp.argmax(g_logits, axis=-1)
    g_w = jnp.take_along_axis(g_prob, g_idx[:, None], axis=-1)
    e_logits = jnp.einsum('td,dge->tge', t, w_re).astype(jnp.float32) + b_re.astype(jnp.float32)
    e_sel = jnp.take_along_axis(e_logits, g_idx[:, None, None], axis=1)[:, 0]
    top_v, top_i = lax.top_k(e_sel, EXPERT_TOP_K)
    top_w = jax.nn.softmax(top_v, axis=-1)
    e_w = jnp.sum(jax.nn.one_hot(top_i, EXPERTS_PER_GROUP, dtype=jnp.float32) * top_w[..., None], axis=1)
    gates = (jax.nn.one_hot(g_idx, N_EXPERT_GROUPS, dtype=jnp.float32)[:, :, None]
             * (g_w[:, :, None] * e_w[:, None, :])).reshape(T, N_EXPERTS)
    hid = jax.nn.silu(jnp.einsum('td,edf->tef', t, w_gate)) * jnp.einsum('td,edf->tef', t, w_up)
    hid = hid * gates.astype(hid.dtype)[:, :, None]
    y = jnp.einsum('tef,efd->td', hid, w_down)
    return y.reshape(B, S, D)


def setup_inputs(seed: int = 0) -> dict:
    key = jax.random.key(seed)
    ks = jax.random.split(key, 24)
    D = D_MODEL

    def nrm(k, shape, scale):
        return jax.random.normal(k, shape, jnp.float32) * scale

    return {
        "x": nrm(ks[0], (BATCH, SEQ, D), 1.0),
        "c": nrm(ks[1], (BATCH, D), 1.0),
        "ada_w": nrm(ks[2], (DEPTH, D, N_ADA * D), ADA_INIT_SCALE * D ** -0.5),
        "ada_b": nrm(ks[3], (DEPTH, N_ADA * D), 0.02),
        "norm_mix": 1.0 + nrm(ks[4], (DEPTH, D), 0.02),
        "norm_ffn": 1.0 + nrm(ks[5], (DEPTH, D), 0.02),
        "attn_w_in": nrm(ks[6], (N_ATTN_LAYERS, D, 3 * D), D ** -0.5),
        "attn_lam_q1": nrm(ks[7], (N_ATTN_LAYERS, DIFF_HEAD_DIM), 0.1),
        "attn_lam_k1": nrm(ks[8], (N_ATTN_LAYERS, DIFF_HEAD_DIM), 0.1),
        "attn_lam_q2": nrm(ks[9], (N_ATTN_LAYERS, DIFF_HEAD_DIM), 0.1),
        "attn_lam_k2": nrm(ks[10], (N_ATTN_LAYERS, DIFF_HEAD_DIM), 0.1),
        "attn_subln": 1.0 + nrm(ks[11], (N_ATTN_LAYERS, DIFF_V_DIM), 0.02),
        "attn_w_out": nrm(ks[12], (N_ATTN_LAYERS, N_DIFF_HEADS * DIFF_V_DIM, D), (N_DIFF_HEADS * DIFF_V_DIM) ** -0.5),
        "fourier_w_in": nrm(ks[13], (N_FOURIER_LAYERS, D, D), D ** -0.5),
        "fourier_w_out": nrm(ks[14], (N_FOURIER_LAYERS, D, D), D ** -0.5),
        "router_group_w": nrm(ks[15], (DEPTH, D, N_EXPERT_GROUPS), D ** -0.5),
        "router_group_b": nrm(ks[16], (DEPTH, N_EXPERT_GROUPS), 0.01),
        "router_expert_w": nrm(ks[17], (DEPTH, D, N_EXPERT_GROUPS, EXPERTS_PER_GROUP), D ** -0.5),
        "router_expert_b": nrm(ks[18], (DEPTH, N_EXPERT_GROUPS, EXPERTS_PER_GROUP), 0.01),
        "expert_w_gate": nrm(ks[19], (DEPTH, N_EXPERTS, D, D_EXPERT), D ** -0.5),
        "expert_w_up": nrm(ks[20], (DEPTH, N_EXPERTS, D, D_EXPERT), D ** -0.5),
        "expert_w_down": nrm(ks[21], (DEPTH, N_EXPERTS, D_EXPERT, D), D_EXPERT ** -0.5),
        "norm_final": 1.0 + nrm(ks[22], (D,), 0.02),
    }


def reference(x, c, ada_w, ada_b, norm_mix, norm_ffn, attn_w_in, attn_lam_q1, attn_lam_k1,
              attn_lam_q2, attn_lam_k2, attn_subln, attn_w_out, fourier_w_in, fourier_w_out,
              router_group_w, router_group_b, router_expert_w, router_expert_b,
              expert_w_gate, expert_w_up, expert_w_down, norm_final):
    cond = jax.nn.silu(c)
    for i in range(DEPTH):
        mod = cond @ ada_w[i] + ada_b[i]
        sh1, sc1, g1, sh2, sc2, g2 = jnp.split(mod, N_ADA, axis=-1)
        h = modulate(rmsnorm(x, norm_mix[i]), sh1, sc1)
        j = i // N_MIXERS
        if i % N_MIXERS == 0:
            y = diff_attention(h, attn_w_in[j], attn_lam_q1[j], attn_lam_k1[j], attn_lam_q2[j],
                               attn_lam_k2[j], attn_subln[j], attn_w_out[j], i)
        else:
            y = fourier_mix(h, fourier_w_in[j], fourier_w_out[j])
        x = x + g1[:, None, :] * y
        h = modulate(rmsnorm(x, norm_ffn[i]), sh2, sc2)
        y = hier_moe(h, router_group_w[i], router_group_b[i], router_expert_w[i], router_expert_b[i],
                     expert_w_gate[i], expert_w_up[i], expert_w_down[i])
        x = x + g2[:, None, :] * y
    return rmsnorm(x, norm_final)
```

```python
import math
import contextlib
import numpy as np
import ml_dtypes
import concourse.bass as bass
import concourse.mybir as mybir
from concourse.bass_utils import run_bass_kernel_spmd

F32 = mybir.dt.float32
BF16 = mybir.dt.bfloat16
AF = mybir.ActivationFunctionType
ALU = mybir.AluOpType
AX = mybir.AxisListType

D = 1024
S = 8192
B = 2
NCORE = 8
TOK = 2048
NT = TOK // 128
H = 8
DE = 512
NE = 16
EPS = 1e-6
LAM_INIT0 = 0.8 - 0.6 * math.exp(-0.3 * 0)

ENGS = ("pe", "act", "dve", "pool", "sp")
EPOCH = 12000
NDMASEM = 10


class _Op:
    __slots__ = ("eng", "fn", "waits", "is_dma", "seq", "sem_slot", "is_cc")


class Prog:
    def __init__(self, nc):
        self.nc = nc
        self.ops = {e: [] for e in ENGS}
        self.cnt = {e: 0 for e in ENGS}
        self.last_w = {}
        self.readers = {}
        self.waited = {e: {} for e in ENGS}
        self.dma_rr = {e: 0 for e in ENGS}
        self.dma_cnt = {}
        self.dma_last = {}
        self.semkeys = set()
        self.pending_fence = {e: [] for e in ENGS}

    def _event(self, op):
        if op.is_dma:
            return (("dma", op.eng, op.sem_slot), op.seq)
        return (("eng", op.eng, (op.seq - 1) // EPOCH), ((op.seq - 1) % EPOCH) + 1)

    def _add_wait(self, op, dep):
        if dep is None or dep is op:
            return
        if (not dep.is_dma) and dep.eng == "pe" and op.eng == "pe" and not op.is_dma:
            return
        key, val = self._event(dep)
        w = self.waited[op.eng]
        if w.get(key, 0) >= val:
            return
        w[key] = val
        op.waits.append((key, val))
        self.semkeys.add(key)

    def fence(self):
        deps = []
        for e in ENGS:
            for op in reversed(self.ops[e]):
                if not op.is_dma:
                    deps.append(op)
                    break
        deps.extend(self.dma_last.values())
        for e in ENGS:
            self.pending_fence[e] = list(deps)

    def _record(self, eng, fn, reads, writes, is_dma):
        op = _Op()
        op.eng = eng
        op.fn = fn
        op.waits = []
        op.is_dma = is_dma
        op.is_cc = False
        if is_dma == "cc":
            op.is_dma = True
            op.is_cc = True
            self.ncc = getattr(self, "ncc", 0) + 1
            op.sem_slot = f"cc{self.ncc}"
            op.seq = 1
            self.semkeys.add(("dma", eng, op.sem_slot))
            self.dma_last[(eng, op.sem_slot)] = op
        elif is_dma:
            slot = self.dma_rr[eng] % NDMASEM
            self.dma_rr[eng] += 1
            k = (eng, slot)
            prev = self.dma_last.get(k)
            self.dma_cnt[k] = self.dma_cnt.get(k, 0) + 1
            op.sem_slot = slot
            op.seq = 16 * self.dma_cnt[k]
            self.semkeys.add(("dma", eng, slot))
            if prev is not None:
                self._add_wait(op, prev)
            self.dma_last[k] = op
        else:
            self.cnt[eng] += 1
            op.seq = self.cnt[eng]
            op.sem_slot = None
            self.semkeys.add(("eng", eng, (op.seq - 1) // EPOCH))
        if self.pending_fence[eng]:
            for d in self.pending_fence[eng]:
                self._add_wait(op, d)
            self.pending_fence[eng] = []
        for r in reads:
            self._add_wait(op, self.last_w.get(r))
        for w in writes:
            self._add_wait(op, self.last_w.get(w))
            for rd in self.readers.get(w, ()):
                self._add_wait(op, rd)
        for r in reads:
            self.readers.setdefault(r, []).append(op)
        for w in writes:
            self.last_w[w] = op
            self.readers[w] = []
        self.ops[eng].append(op)
        return op

    def op(self, eng, fn, reads=(), writes=()):
        return self._record(eng, fn, reads, writes, False)

    def dma(self, eng, fn, reads=(), writes=()):
        return self._record(eng, fn, reads, writes, True)

    def cc(self, kind, rg, src, dst, reads=(), writes=()):
        return self._record("pool", lambda e: e.collective_compute(kind, ALU.bypass, replica_groups=rg,
                                                                    ins=[src], outs=[dst]), reads, writes, "cc")

    def mm(self, out, lhsT, rhs, start=True, stop=True, reads=(), writes=(), **kw):
        return self.op("pe", lambda e: e.matmul(out, lhsT, rhs, start=start, stop=stop, **kw), reads, writes)

    def tr(self, out, in_, ident, reads=(), writes=()):
        return self.op("pe", lambda e: e.transpose(out, in_, ident), reads, writes)

    def act(self, out, in_, func, reads=(), writes=(), **kw):
        o = self.op("act", lambda e: e.activation(out, in_, func, **kw), reads, writes)
        acc = kw.get("accum_out")
        if acc is not None and getattr(self, "act_dummy", None) is not None:
            dm = self.act_dummy
            o = self.op("act", lambda e: e.copy(dm, acc), (), writes)
        return o

    def tt(self, eng, out, a, b, op, reads=(), writes=()):
        return self.op(eng, lambda e: e.tensor_tensor(out, a, b, op), reads, writes)

    def ts(self, eng, out, a, s1, s2, op0, op1=None, reads=(), writes=()):
        if op1 is None:
            return self.op(eng, lambda e: e.tensor_scalar(out, a, s1, None, op0), reads, writes)
        return self.op(eng, lambda e: e.tensor_scalar(out, a, s1, s2, op0, op1), reads, writes)

    def stt(self, eng, out, a, s, b, op0, op1, reads=(), writes=()):
        return self.op(eng, lambda e: e.scalar_tensor_tensor(out, a, s, b, op0, op1), reads, writes)

    def cp(self, eng, out, in_, reads=(), writes=()):
        if eng == "act":
            return self.op(eng, lambda e: e.copy(out, in_), reads, writes)
        return self.op(eng, lambda e: e.tensor_copy(out, in_), reads, writes)

    def memset(self, eng, ap, val, writes=()):
        return self.op(eng, lambda e: e.memset(ap, val), (), writes)

    def load(self, out, in_, reads=(), writes=(), eng="sp"):
        return self.dma(eng, lambda e: e.dma_start(out=out, in_=in_), reads, writes)

    def emit(self):
        nc = self.nc
        with contextlib.ExitStack() as st:
            sems = {}
            for key in sorted(self.semkeys, key=str):
                nm = "s_" + "_".join(str(k) for k in key)
                sems[key] = st.enter_context(nc.semaphore(nm))
            finals = []
            for e in ENGS:
                for op in reversed(self.ops[e]):
                    if not op.is_dma:
                        finals.append(self._event(op))
                        break
            for op in self.dma_last.values():
                finals.append(self._event(op))
            block = st.enter_context(nc.Block())
            engmap = {"pe": block.tensor, "act": block.scalar, "dve": block.vector,
                      "pool": block.gpsimd, "sp": block.sync}

            def make(ename):
                ops = self.ops[ename]

                def body(eng):
                    for op in ops:
                        for key, val in op.waits:
                            eng.wait_ge(sems[key], val)
                        ins = op.fn(eng)
                        key, val = self._event(op)
                        if op.is_cc:
                            ins.then_inc(sems[key])
                        else:
                            ins.then_inc(sems[key], 16 if op.is_dma else 1)
                    if ename == "sp":
                        for key, val in finals:
                            eng.wait_ge(sems[key], val)
                return body

            for e in ENGS:
                engmap[e](make(e))


class Arena:
    def __init__(self, nc, st, words=51000):
        self.t = st.enter_context(nc.sbuf_tensor("arena", [128, words], F32))
        self.words = words
        self.top = 0

    def alloc(self, shape, dt):
        n = int(np.prod(shape))
        w = n if dt == F32 else (n + 1) // 2
        w = (w + 7) // 8 * 8
        a = self.top
        self.top += w
        assert self.top <= self.words, ("SBUF arena overflow", self.top, self.words)
        self.last = a
        return self.view(a, shape, dt)

    def view(self, a, shape, dt):
        n = int(np.prod(shape))
        w = n if dt == F32 else (n + 1) // 2
        w = (w + 7) // 8 * 8
        ap = self.t[:, a:a + w]
        if dt == BF16:
            ap = ap.bitcast(BF16)
        ap = ap[:, 0:n]
        if len(shape) > 1:
            names = [f"d{i}" for i in range(len(shape))]
            kw = {names[i]: int(shape[i]) for i in range(1, len(shape))}
            ap = ap.rearrange(f"p ({' '.join(names)}) -> p {' '.join(names)}", **kw)
        return ap

    def mark(self):
        return self.top

    def release(self, m):
        self.top = m


class Ctx:
    pass


def bc_mid(ap, n):
    return ap[:, :, None].broadcast_to([128, ap.shape[1], n])


def setup_common(cx, ident_d, identf_d):
    P, A = cx.P, cx.A
    cx.ident = A.alloc([128], BF16)
    cx.identf = A.alloc([128], F32)
    P.act_dummy = A.alloc([1], F32)
    P.load(cx.ident, ident_d, writes=["ident"])
    P.load(cx.identf, identf_d, writes=["identf"])


def diag_extract(cx, dst, src_bc, rkeys, wkey):
    P = cx.P
    tmp = cx.diag_tmp
    P.tt("dve", tmp, src_bc.rearrange("p (a b) -> p a b", b=128),
         cx.identf[:, None, :].broadcast_to([128, 8, 128]), ALU.mult,
         reads=list(rkeys) + ["identf"], writes=["diag_tmp"])
    P.op("dve", lambda e: e.tensor_reduce(dst, tmp, AX.X, ALU.add), reads=["diag_tmp"], writes=[wkey])


def stage_mod(cx, c_d, adaw_d, adab_d, nmix_d, nffn_d, tag):
    P, A, ps = cx.P, cx.A, cx.ps
    if getattr(cx, "A1", None) is not None:
        A1, B1, A2, B2, g1bc, g2bc = cx.A1, cx.B1, cx.A2, cx.B2, cx.g1bc, cx.g2bc
    else:
        A1 = A.alloc([8], F32); B1 = A.alloc([8], F32); A2 = A.alloc([8], F32); B2 = A.alloc([8], F32)
        g1bc = A.alloc([D], F32); g2bc = A.alloc([D], F32)
    m = A.mark()
    csb = A.alloc([8], F32)
    cond = A.alloc([8], F32)
    condbc = A.alloc([8, 128], F32)
    modbc = A.alloc([6 * D], F32)
    adab = A.alloc([6 * D], F32)
    nm = A.alloc([D], F32)
    nf = A.alloc([D], F32)
    cx.diag_tmp = A.alloc([8, 128], F32)
    wbuf = [A.alloc([8, 512], F32) for _ in range(2)]
    t = tag
    P.load(csb, c_d, writes=[t + "csb"])
    P.load(adab, adab_d.partition_broadcast(128), writes=[t + "adab"])
    P.load(nm, nmix_d.partition_broadcast(128), writes=[t + "nm"])
    P.load(nf, nffn_d.partition_broadcast(128), writes=[t + "nf"])
    P.act(cond, csb, AF.Silu, reads=[t + "csb"], writes=[t + "cond"])
    P.cp("dve", condbc, bc_mid(cond, 128), reads=[t + "cond"], writes=[t + "condbc"])
    wv = adaw_d.rearrange("(kc p) n -> p kc n", p=128)
    for nch in range(12):
        wb = wbuf[nch % 2]
        wk = f"{t}adaw{nch % 2}"
        P.load(wb, wv[:, :, nch * 512:(nch + 1) * 512], writes=[wk])
        bank = 6 + nch % 2
        for kc in range(8):
            P.mm(ps[:, bank, :], condbc[:, kc, :], wb[:, kc, :],
                 start=(kc == 0), stop=(kc == 7), reads=[wk, t + "condbc"], writes=[f"ps{bank}"])
        sl = slice(nch * 512, (nch + 1) * 512)
        P.tt("dve", modbc[:, sl], ps[:, bank, :], adab[:, sl], ALU.add,
             reads=[f"ps{bank}", t + "adab"], writes=[t + "modbc"])
    sh1, sc1, g1, sh2, sc2, g2 = [modbc[:, i * D:(i + 1) * D] for i in range(6)]
    tmpA = adab[:, 0:D]
    P.stt("dve", tmpA, sc1, 1.0, nm, ALU.add, ALU.mult, reads=[t + "modbc", t + "nm"], writes=[t + "adab"])
    diag_extract(cx, A1, tmpA, [t + "adab"], t + "A1")
    diag_extract(cx, B1, sh1, [t + "modbc"], t + "B1")
    tmpA2 = adab[:, D:2 * D]
    P.stt("dve", tmpA2, sc2, 1.0, nf, ALU.add, ALU.mult, reads=[t + "modbc", t + "nf"], writes=[t + "adab2"])
    diag_extract(cx, A2, tmpA2, [t + "adab2"], t + "A2")
    diag_extract(cx, B2, sh2, [t + "modbc"], t + "B2")
    P.cp("dve", g1bc, g1, reads=[t + "modbc"], writes=[t + "g1bc"])
    P.cp("dve", g2bc, g2, reads=[t + "modbc"], writes=[t + "g2bc"])
    P.fence()
    A.release(m)
    cx.A1, cx.B1, cx.A2, cx.B2, cx.g1bc, cx.g2bc = A1, B1, A2, B2, g1bc, g2bc
    cx.modtag = t


def norm_scratch(cx):
    A = cx.A
    cx.n_junk = A.alloc([D], BF16)
    cx.n_xn = [A.alloc([D], BF16) for _ in range(2)]
    cx.n_st = [A.alloc([4], F32) for _ in range(2)]
    cx.n_tmp = [A.alloc([8, 128], BF16) for _ in range(2)]
    cx.n_i = 0


def norm_T(cx, xt, xkey, Avec, Bvec, vkeys, dst, dkey):
    P, ps = cx.P, cx.ps
    i = cx.n_i % 2
    cx.n_i += 1
    st = cx.n_st[i]
    xn = cx.n_xn[i]
    tmp = cx.n_tmp[i]
    sk, xk, tk = f"nst{i}", f"nxn{i}", f"ntmp{i}"
    P.act(cx.n_junk, xt, AF.Square, reads=[xkey], writes=["njunk", sk], accum_out=st[:, 0:1])
    P.ts("dve", st[:, 1:2], st[:, 0:1], 1.0 / D, EPS, ALU.mult, ALU.add, reads=[sk], writes=[sk])
    P.act(st[:, 2:3], st[:, 1:2], AF.Sqrt, reads=[sk], writes=[sk])
    P.op("dve", lambda e: e.reciprocal(st[:, 3:4], st[:, 2:3]), reads=[sk], writes=[sk])
    P.act(xn, xt, AF.Copy, reads=[xkey, sk], writes=[xk], scale=st[:, 3:4])
    bank = 6 + i
    pst = ps[:, bank, :].bitcast(BF16).rearrange("p (a b) -> p a b", b=128)
    for kc in range(8):
        P.tr(pst[:, kc, :], xn[:, kc * 128:(kc + 1) * 128], cx.ident, reads=[xk, "ident"], writes=[f"ps{bank}"])
    P.tt("dve", tmp, pst, bc_mid(Avec, 128), ALU.mult, reads=[f"ps{bank}"] + list(vkeys), writes=[tk])
    P.tt("pool", dst, tmp, bc_mid(Bvec, 128), ALU.add, reads=[tk] + list(vkeys), writes=[dkey])


def transpose_tok(cx, src, skey, dst, dkey, eng="dve"):
    P, ps = cx.P, cx.ps
    i = cx.n_i % 2
    cx.n_i += 1
    bank = 6 + i
    pst = ps[:, bank, :].bitcast(BF16).rearrange("p (a b) -> p a b", b=128)
    for kc in range(8):
        P.tr(pst[:, kc, :], src[:, kc * 128:(kc + 1) * 128], cx.ident, reads=[skey, "ident"], writes=[f"ps{bank}"])
    P.cp(eng, dst, pst, reads=[f"ps{bank}"], writes=[dkey])


def proj_residual(cx, srcT, skeys, wp, wkey):
    P, ps = cx.P, cx.ps
    it = 0
    for t in range(NT):
        for half in range(2):
            bank = 4 + (it % 2)
            it += 1
            for kc in range(8):
                P.mm(ps[:, bank, :], srcT[:, kc, t * 128:(t + 1) * 128], wp[:, kc, half * 512:(half + 1) * 512],
                     start=(kc == 0), stop=(kc == 7), reads=list(skeys) + [wkey], writes=[f"ps{bank}"])
            xs = cx.xres[:, t, half * 512:(half + 1) * 512]
            P.tt("dve", xs, ps[:, bank, :], xs, ALU.add, reads=[f"ps{bank}"], writes=[f"xres{t}"])


def stage_moe(cx, wr_d, br_d, wg_d, wu_d, wd_d, tag):
    P, A, ps = cx.P, cx.A, cx.ps
    t_ = tag
    m = A.mark()
    h2T = A.alloc([8, TOK], BF16)
    norm_scratch(cx)
    vk = [cx.modtag + "A2", cx.modtag + "B2"]
    for t in range(NT):
        norm_T(cx, cx.xres[:, t, :], f"xres{t}", cx.A2, cx.B2, vk, h2T[:, :, t * 128:(t + 1) * 128], f"{t_}h2T{t}")
    h2keys = [f"{t_}h2T{t}" for t in range(NT)]
    wr = A.alloc([8, 20], BF16)
    brbc = A.alloc([20], F32)
    P.load(wr, wr_d, writes=[t_ + "wr"], eng="pool")
    P.load(brbc, br_d.partition_broadcast(128), writes=[t_ + "br"])
    L = A.alloc([NT, 20], F32)
    rbank = 5
    Lps = ps[:, rbank, :].rearrange("p (a b) -> p a b", b=32)[:, :, 0:20]
    for t in range(NT):
        for kc in range(8):
            P.mm(Lps[:, t, :], h2T[:, kc, t * 128:(t + 1) * 128], wr[:, kc, :], start=(kc == 0), stop=(kc == 7),
                 reads=[f"{t_}h2T{t}", t_ + "wr"], writes=[f"ps{rbank}"])
    P.tt("dve", L, Lps, brbc[:, None, :].broadcast_to([128, NT, 20]), ALU.add,
         reads=[f"ps{rbank}", t_ + "br"], writes=[t_ + "L"])
    Lg = L[:, :, 0:4]
    Le = L[:, :, 4:20].rearrange("p t (g e) -> p t g e", e=4)
    gmax = A.alloc([NT], F32); gsum = A.alloc([NT], F32); gw = A.alloc([NT], F32)
    ohg = A.alloc([NT, 4], F32); eg = A.alloc([NT, 4], F32)
    tmp44 = A.alloc([NT, 4, 4], F32)
    esel = A.alloc([NT, 4], F32); e2 = A.alloc([NT, 4], F32)
    m1 = A.alloc([NT], F32); m2 = A.alloc([NT], F32)
    mk1 = A.alloc([NT, 4], F32); mk2 = A.alloc([NT, 4], F32)
    dd = A.alloc([NT], F32); w1 = A.alloc([NT], F32); w2 = A.alloc([NT], F32)
    ew = A.alloc([NT, 4], F32)
    gates = A.alloc([NT, 4, 4], F32)
    rk = t_ + "rt"

    def bcl(ap, n):
        return ap[:, :, None].broadcast_to([128, NT, n])

    P.op("dve", lambda e: e.tensor_reduce(gmax, Lg, AX.X, ALU.max), reads=[t_ + "L"], writes=[rk])
    P.tt("dve", ohg, Lg, bcl(gmax, 4), ALU.is_equal, reads=[rk, t_ + "L"], writes=[rk])
    P.tt("dve", eg, Lg, bcl(gmax, 4), ALU.subtract, reads=[rk, t_ + "L"], writes=[rk])
    P.act(eg, eg, AF.Exp, reads=[rk], writes=[rk])
    P.op("dve", lambda e: e.tensor_reduce(gsum, eg, AX.X, ALU.add), reads=[rk], writes=[rk])
    P.op("dve", lambda e: e.reciprocal(gw, gsum), reads=[rk], writes=[rk])
    P.tt("dve", tmp44, Le, ohg[:, :, :, None].broadcast_to([128, NT, 4, 4]), ALU.mult, reads=[rk, t_ + "L"], writes=[rk])
    P.op("dve", lambda e: e.tensor_reduce(esel, tmp44.rearrange("p t g e -> p t e g"), AX.X, ALU.add), reads=[rk], writes=[rk])
    P.op("dve", lambda e: e.tensor_reduce(m1, esel, AX.X, ALU.max), reads=[rk], writes=[rk])
    P.tt("dve", mk1, esel, bcl(m1, 4), ALU.is_equal, reads=[rk], writes=[rk])
    P.stt("dve", e2, mk1, -1e30, esel, ALU.mult, ALU.add, reads=[rk], writes=[rk])
    P.op("dve", lambda e: e.tensor_reduce(m2, e2, AX.X, ALU.max), reads=[rk], writes=[rk])
    P.tt("dve", mk2, e2, bcl(m2, 4), ALU.is_equal, reads=[rk], writes=[rk])
    P.tt("dve", dd, m2, m1, ALU.subtract, reads=[rk], writes=[rk])
    P.act(dd, dd, AF.Exp, reads=[rk], writes=[rk])
    P.ts("dve", w1, dd, 1.0, None, ALU.add, reads=[rk], writes=[rk])
    P.op("dve", lambda e: e.reciprocal(w1, w1), reads=[rk], writes=[rk])
    P.tt("dve", w2, dd, w1, ALU.mult, reads=[rk], writes=[rk])
    P.tt("dve", w1, w1, gw, ALU.mult, reads=[rk], writes=[rk])
    P.tt("dve", w2, w2, gw, ALU.mult, reads=[rk], writes=[rk])
    P.tt("dve", ew, mk1, bcl(w1, 4), ALU.mult, reads=[rk], writes=[rk])
    P.tt("dve", mk2, mk2, bcl(w2, 4), ALU.mult, reads=[rk], writes=[rk])
    P.tt("dve", ew, ew, mk2, ALU.add, reads=[rk], writes=[rk])
    P.tt("dve", gates, ohg[:, :, :, None].broadcast_to([128, NT, 4, 4]),
         ew[:, :, None, :].broadcast_to([128, NT, 4, 4]), ALU.mult, reads=[rk], writes=[t_ + "gates"])
    gflat = gates.rearrange("p t g e -> p t (g e)")
    if "L" in DBG and t_ == "e0":
        P.load(DBG["L"].rearrange("(t p) d -> p t d", p=128), L, reads=[t_ + "L"], writes=["dbg_L"])
        P.load(DBG["gates"].rearrange("(t p) d -> p t d", p=128), gflat, reads=[t_ + "gates"], writes=["dbg_g"])
        P.load(DBG["xmid"].rearrange("(t p) d -> p t d", p=128), cx.xres, reads=[f"xres{t}" for t in range(NT)], writes=["dbg_x"])
    wgb = [A.alloc([8, DE], BF16) for _ in range(2)]
    wub = [A.alloc([8, DE], BF16) for _ in range(2)]
    wdb = [A.alloc([4, D], BF16) for _ in range(2)]
    sil = [A.alloc([512], BF16) for _ in range(2)]
    hid = [A.alloc([4, 512], BF16) for _ in range(2)]
    it_gu = 0
    it_y = 0
    it_h = 0
    for e in range(NE):
        bi = e % 2
        kg, ku, kd = f"{t_}wg{bi}", f"{t_}wu{bi}", f"{t_}wd{bi}"
        P.load(wgb[bi], wg_d[e].rearrange("(kc p) n -> p kc n", p=128), writes=[kg], eng="pool")
        P.load(wub[bi], wu_d[e].rearrange("(kc p) n -> p kc n", p=128), writes=[ku], eng="pool")
        P.load(wdb[bi], wd_d[e].rearrange("(kc p) n -> p kc n", p=128), writes=[kd], eng="pool")
        P.tt("pool", wdb[bi], wdb[bi], cx.g2bc[:, None, :].broadcast_to([128, 4, D]), ALU.mult,
             reads=[kd, cx.modtag + "g2bc"], writes=[kd])
        for c in range(4):
            hb = it_h % 2
            it_h += 1
            hk = f"{t_}hid{hb}"
            ckeys = [f"{t_}h2T{t}" for t in range(4 * c, 4 * c + 4)]
            for fc in range(4):
                gb = (it_gu % 2) * 2
                it_gu += 1
                for kc in range(8):
                    P.mm(ps[:, gb, :], wgb[bi][:, kc, fc * 128:(fc + 1) * 128], h2T[:, kc, c * 512:(c + 1) * 512],
                         start=(kc == 0), stop=(kc == 7), reads=[kg] + ckeys, writes=[f"ps{gb}"])
                for kc in range(8):
                    P.mm(ps[:, gb + 1, :], wub[bi][:, kc, fc * 128:(fc + 1) * 128], h2T[:, kc, c * 512:(c + 1) * 512],
                         start=(kc == 0), stop=(kc == 7), reads=[ku] + ckeys, writes=[f"ps{gb + 1}"])
                sb_ = sil[it_gu % 2]
                sk = f"{t_}sil{it_gu % 2}"
                P.act(sb_, ps[:, gb, :], AF.Silu, reads=[f"ps{gb}"], writes=[sk])
                P.tt("dve", hid[hb][:, fc, :], ps[:, gb + 1, :], sb_, ALU.mult, reads=[f"ps{gb + 1}", sk], writes=[hk])
            for ts_ in range(4):
                t = 4 * c + ts_
                for half in range(2):
                    yb = 4 + (it_y % 4)
                    it_y += 1
                    for fc in range(4):
                        P.mm(ps[:, yb, :], hid[hb][:, fc, ts_ * 128:(ts_ + 1) * 128], wdb[bi][:, fc, half * 512:(half + 1) * 512],
                             start=(fc == 0), stop=(fc == 3), reads=[hk, kd], writes=[f"ps{yb}"])
                    xs = cx.xres[:, t, half * 512:(half + 1) * 512]
                    P.stt("dve", xs, ps[:, yb, :], gflat[:, t, e:e + 1], xs, ALU.mult, ALU.add,
                          reads=[f"ps{yb}", t_ + "gates"], writes=[f"xres{t}"])
    P.fence()
    A.release(m)


def layer0_body(cx, xp, w_in, w_out, lam, subln, kaug, qaug, dfix, kscr, qscr, vscr, wr, br, wg, wu, wd):
    P, A, ps = cx.P, cx.A, cx.ps
    mt = cx.modtag
    m_qkv = A.mark()
    win = A.alloc([8, 3 * D], BF16)
    wv = w_in.rearrange("(kc p) n -> p kc n", p=128)
    for i in range(3):
        P.load(win[:, :, i * D:(i + 1) * D], wv[:, :, i * D:(i + 1) * D], writes=[f"win{i}"], eng="pool")
    norm_scratch(cx)
    xt = [A.alloc([D], F32) for _ in range(6)]
    hTc = [A.alloc([8, 512], BF16) for _ in range(2)]
    stgp = [A.alloc([2, H, 512], BF16) for _ in range(2)]
    istg = 0
    vst = [A.alloc([4, H, 129], BF16) for _ in range(2)]
    for i in range(2):
        P.memset("pool", vst[i][:, :, :, 128:129], 1.0, writes=[f"vst{i}"])
    xv = xp.rearrange("(t p) d -> t p d", p=128)
    kscr_v = kscr.rearrange("s h d t -> d s h t")
    qscr_v = qscr.rearrange("s h d t -> d s h t")
    vscr_v = vscr.rearrange("t p h c -> p t h c")
    vk1 = [mt + "A1", mt + "B1"]
    ixt = 0
    ipb = 0
    qkv_state = {"ixt": 0, "istg": 0, "ipb": 0}

    def emit_norm(ch):
        ixt = qkv_state["ixt"]
        cb = ch % 2
        hk = f"hTc{cb}"
        for t in range(4):
            xb = xt[ixt % 6]
            xk = f"xt{ixt % 6}"
            ixt += 1
            P.load(xb, xv[ch * 4 + t], writes=[xk])
            norm_T(cx, xb, xk, cx.A1, cx.B1, vk1, hTc[cb][:, :, t * 128:(t + 1) * 128], hk)
        qkv_state["ixt"] = ixt

    def emit_mm(ch):
        cb = ch % 2
        hk = f"hTc{cb}"
        istg = qkv_state["istg"]
        ipb = qkv_state["ipb"]
        for which in ([1, 0] if ch < 4 else [1]):
            stg = stgp[istg % 2]
            sk = f"stg{istg % 2}"
            istg += 1
            for hb in range(H):
                bank = ipb % 4
                ipb += 1
                col = which * D + hb * 128
                for kc in range(8):
                    P.mm(ps[:, bank, :], win[:, kc, col:col + 128], hTc[cb][:, kc, :], start=(kc == 0), stop=(kc == 7),
                         reads=[f"win{which}", hk], writes=[f"ps{bank}"])
                if which == 1:
                    P.cp("act", stg[0:64, 0, hb, :], ps[0:64, bank, :], reads=[f"ps{bank}"], writes=[sk])
                    P.cp("dve", stg[0:64, 1, hb, :], ps[64:128, bank, :], reads=[f"ps{bank}"], writes=[sk])
                else:
                    P.op("act", lambda e, o=stg[0:64, 0, hb, :], i_=ps[0:64, bank, :]: e.mul(o, i_, 0.125),
                         reads=[f"ps{bank}"], writes=[sk])
                    P.ts("dve", stg[0:64, 1, hb, :], ps[64:128, bank, :], 0.125, None, ALU.mult,
                         reads=[f"ps{bank}"], writes=[sk])
            if which == 1:
                P.load(kscr_v[:, :, :, ch * 512:(ch + 1) * 512], stg[0:64], reads=[sk], writes=[f"kscr{ch}"])
            else:
                P.load(qscr_v[:, :, :, ch * 512:(ch + 1) * 512], stg[0:64], reads=[sk], writes=[f"qscr{ch}"])
        for t in range(4):
            for half in range(2):
                bank = ipb % 4
                ipb += 1
                for kc in range(8):
                    P.mm(ps[:, bank, :], hTc[cb][:, kc, t * 128:(t + 1) * 128],
                         win[:, kc, 2 * D + half * 512:2 * D + (half + 1) * 512], start=(kc == 0), stop=(kc == 7),
                         reads=["win2", hk], writes=[f"ps{bank}"])
                eng = "act" if half == 0 else "dve"
                P.cp(eng, vst[cb][:, t, half * 4:(half + 1) * 4, 0:128],
                     ps[:, bank, :].rearrange("p (h c) -> p h c", c=128), reads=[f"ps{bank}"], writes=[f"vst{cb}"])
        P.load(vscr_v[:, ch * 4:(ch + 1) * 4], vst[cb], reads=[f"vst{cb}"], writes=[f"vscr{ch}"])

        qkv_state["istg"] = istg
        qkv_state["ipb"] = ipb

    emit_norm(0)
    for ch in range(S // 512):
        if ch + 1 < S // 512:
            emit_norm(ch + 1)
        emit_mm(ch)
    P.fence()
    A.release(m_qkv)
    cx.xres = A.alloc([NT, D], F32)
    xres_off = A.last
    m_att = A.mark()
    obf = A.alloc([NT, D], BF16)
    ssq = A.alloc([NT, H], F32)
    lamv = A.alloc([4, 64], F32)
    lamt = A.alloc([2, 64], F32)
    lams = A.alloc([4], F32)
    neglam = A.alloc([1], F32)
    dfx = A.alloc([H, 128], BF16)
    P.load(dfx, dfix, writes=["dfx"])
    P.load(lamv, lam.rearrange("a b -> (a b)").partition_broadcast(128).rearrange("p (a b) -> p a b", b=64), writes=["lamv"])
    P.tt("dve", lamt, lamv[:, 0:4:2, :], lamv[:, 1:4:2, :], ALU.mult, reads=["lamv"], writes=["lamt"])
    P.op("dve", lambda e: e.tensor_reduce(lams[:, 0:2], lamt, AX.X, ALU.add), reads=["lamt"], writes=["lams"])
    P.act(lams[:, 2:4], lams[:, 0:2], AF.Exp, reads=["lams"], writes=["lams"])
    P.tt("dve", neglam, lams[:, 3:4], lams[:, 2:3], ALU.subtract, reads=["lams"], writes=["neglam"])
    P.ts("dve", neglam, neglam, -LAM_INIT0, None, ALU.add, reads=["neglam"], writes=["neglam"])
    ktb = [A.alloc([2, S], BF16)]
    kt_off = A.last
    ktb.append(A.view(xres_off, [2, S], BF16))
    qtb = [A.alloc([2, TOK], BF16) for _ in range(2)]
    qt_off = A.last - (2 * TOK) // 2
    vtb = A.alloc([S // 128, 2, 129], BF16)
    pT = [A.alloc([1024], BF16) for _ in range(3)]
    o32 = [A.alloc([128], F32) for _ in range(4)]
    t32 = [A.alloc([128], F32) for _ in range(1)]
    rr = [A.alloc([4], F32) for _ in range(4)]
    junk = A.alloc([128], BF16)
    vscr_hp = vscr.rearrange("t p (hp hh) c -> p t hp hh c", hh=2)
    NKT = S // 128
    state = {"isb": 0, "ipt": 0, "iep": 0}

    def load_head(h):
        kb = h % 2
        P.load(ktb[kb][0:64], kscr_v[:, :, h, :], writes=[f"ktb{kb}"])
        P.load(ktb[kb][64:72], kaug[h], writes=[f"ktb{kb}"])
        P.load(qtb[kb][0:64], qscr_v[:, :, h, :], writes=[f"qtb{kb}"])
        P.load(qtb[kb][64:72], qaug, writes=[f"qtb{kb}"])

    def load_v(hp):
        for q4 in range(4):
            P.load(vtb[:, q4 * 16:(q4 + 1) * 16], vscr_hp[:, q4 * 16:(q4 + 1) * 16, hp], writes=["vtb"])

    def emit_qk_exp(h, qc, kt):
        kb = h % 2
        kk, qk = f"ktb{kb}", f"qtb{kb}"
        K_, Q_ = ktb[kb], qtb[kb]
        sb0 = 2 * (state["isb"] % 2)
        state["isb"] += 1
        for s_ in range(2):
            bank = sb0 + s_
            outp = ps[:, bank, :]
            kcols = slice(kt * 128, (kt + 1) * 128)
            q0 = qc * 512
            rd = [kk, qk]
            wkey = [f"ps{bank}"]
            if kt >= NT or kt > 4 * qc + 3:
                P.mm(outp, K_[0:72, s_, kcols], Q_[0:72, s_, q0:q0 + 512], reads=rd, writes=wkey)
            elif kt < 4 * qc:
                P.mm(outp, K_[0:68, s_, kcols], Q_[0:68, s_, q0:q0 + 512], reads=rd, writes=wkey)
            else:
                i_ = kt - 4 * qc
                if i_ > 0:
                    P.mm(outp[:, 0:i_ * 128], K_[0:72, s_, kcols], Q_[0:72, s_, q0:q0 + i_ * 128], reads=rd, writes=wkey)
                dsl = slice(i_ * 128, (i_ + 1) * 128)
                P.mm(outp[:, dsl], K_[0:68, s_, kcols], Q_[0:68, s_, q0 + i_ * 128:q0 + (i_ + 1) * 128],
                     start=True, stop=False, reads=rd, writes=wkey, skip_group_check=True)
                P.mm(outp[:, dsl], cx.ident, dfx[:, h, :], start=False, stop=True,
                     reads=["ident", "dfx"], writes=wkey, skip_group_check=True)
                if i_ < 3:
                    P.mm(outp[:, (i_ + 1) * 128:512], K_[0:68, s_, kcols], Q_[0:68, s_, q0 + (i_ + 1) * 128:q0 + 512],
                         reads=rd, writes=wkey, skip_group_check=True)
        pi_ = state["ipt"] % 3
        state["ipt"] += 1
        P.act(pT[pi_], ps[:, sb0:sb0 + 2, :].rearrange("p a b -> p (a b)"), AF.Exp,
              reads=[f"ps{sb0}", f"ps{sb0 + 1}"], writes=[f"pT{pi_}"])
        return pi_

    def emit_pv(h, qc, kt, pi_):
        hh = h % 2
        pb = pT[pi_]
        for s_ in range(2):
            for qs in range(4):
                a = s_ * 4 + qs
                bank = 4 + a // 3
                off = (a % 3) * 132
                P.mm(ps[:, bank, off:off + 129], pb[:, s_ * 512 + qs * 128:s_ * 512 + (qs + 1) * 128],
                     vtb[:, kt, hh, :], start=(kt == 0 and a % 3 == 0), stop=(kt == NKT - 1),
                     reads=[f"pT{pi_}", "vtb"], writes=[f"oacc{a}"], skip_group_check=True)
        if kt == NKT - 1:
            for qs in range(4):
                t = 4 * qc + qs
                eb = state["iep"] % 4
                state["iep"] += 1
                a1, a2 = qs, 4 + qs
                acc1 = ps[:, 4 + a1 // 3, (a1 % 3) * 132:(a1 % 3) * 132 + 129]
                acc2 = ps[:, 4 + a2 // 3, (a2 % 3) * 132:(a2 % 3) * 132 + 129]
                rk = f"rr{eb}"
                g1 = [f"oacc{i}" for i in range(8) if i // 3 == a1 // 3]
                g2 = [f"oacc{i}" for i in range(8) if i // 3 == a2 // 3]
                P.op("dve", lambda e, o=rr[eb][:, 0:1], i_=acc1[:, 128:129]: e.reciprocal(o, i_), reads=g1, writes=[rk])
                P.op("dve", lambda e, o=rr[eb][:, 1:2], i_=acc2[:, 128:129]: e.reciprocal(o, i_), reads=g2, writes=[rk])
                P.tt("dve", rr[eb][:, 2:3], rr[eb][:, 1:2], neglam, ALU.mult, reads=[rk, "neglam"], writes=[rk])
                P.ts("dve", t32[0], acc2[:, 0:128], rr[eb][:, 2:3], None, ALU.mult, reads=g2 + [rk], writes=["t320"])
                P.stt("dve", o32[eb], acc1[:, 0:128], rr[eb][:, 0:1], t32[0], ALU.mult, ALU.add,
                      reads=g1 + [rk, "t320"], writes=[f"o32{eb}"])
                P.act(junk, o32[eb], AF.Square, reads=[f"o32{eb}"], writes=["ajunk", "ssq"], accum_out=ssq[:, t, h:h + 1])
                P.cp("pool", obf[:, t, h * 128:(h + 1) * 128], o32[eb], reads=[f"o32{eb}"], writes=[f"obf{t}"])
            if qc == 3 and h % 2 == 1 and h + 1 < H:
                load_v((h + 1) // 2)

    load_v(0)
    pend = []
    load_head(0)
    for h in range(H):
        if h + 1 < H:
            load_head(h + 1)
        for qc in range(4):
            for kt in range(NKT):
                pi_ = emit_qk_exp(h, qc, kt)
                pend.append((h, qc, kt, pi_))
                if len(pend) > 1:
                    emit_pv(*pend.pop(0))
    while pend:
        emit_pv(*pend.pop(0))
    P.ts("dve", ssq, ssq, 1.0 / 128, EPS, ALU.mult, ALU.add, reads=["ssq"], writes=["ssq"])
    P.act(ssq, ssq, AF.Sqrt, reads=["ssq"], writes=["ssq"])
    P.op("dve", lambda e: e.reciprocal(ssq, ssq), reads=["ssq"], writes=["ssq"])
    for t in range(NT):
        ov = obf[:, t, :].rearrange("p (h c) -> p h c", c=128)
        P.tt("pool", ov, ov, ssq[:, t, :][:, :, None].broadcast_to([128, H, 128]), ALU.mult,
             reads=["ssq", f"obf{t}"], writes=[f"obf{t}"])
    if "obf" in DBG:
        P.load(DBG["obf"].rearrange("(t p) d -> p t d", p=128), obf, reads=[f"obf{t}" for t in range(NT)], writes=["dbg_obf"])
    P.fence()
    P.load(cx.xres, xp[0:TOK].rearrange("(t p) d -> p t d", p=128), writes=[f"xres{t}" for t in range(NT)])
    oT = A.view(kt_off, [8, TOK], BF16)
    wob = A.view(qt_off, [8, D], BF16)
    sub_s = A.alloc([1], F32)
    P.load(sub_s, subln, writes=["subs"])
    P.ts("dve", sub_s, sub_s, 1.0 - LAM_INIT0, None, ALU.mult, reads=["subs"], writes=["subs"])
    P.load(wob, w_out.rearrange("(kc p) n -> p kc n", p=128), writes=["wob"], eng="pool")
    for kc in range(8):
        P.ts("pool", wob[:, kc, :], wob[:, kc, :], sub_s[:, 0:1], None, ALU.mult,
             reads=["wob", "subs"], writes=["wob"])
        P.tt("pool", wob[:, kc, :], wob[:, kc, :], cx.g1bc, ALU.mult,
             reads=["wob", mt + "g1bc"], writes=["wob"])
    for t in range(NT):
        transpose_tok(cx, obf[:, t, :], f"obf{t}", oT[:, :, t * 128:(t + 1) * 128], f"oT{t}", eng=("dve" if t % 2 else "act"))
    proj_residual(cx, oT, [f"oT{t}" for t in range(NT)], wob, "wob")
    P.fence()
    A.release(m_att)
    stage_moe(cx, wr, br, wg, wu, wd, "e0")


def build_A():
    nc = bass.Bass("TRN2", target_bir_lowering=False)
    dram = lambda n, s, dt=F32, kind="ExternalInput": nc.dram_tensor(n, s, dt, kind=kind).ap()
    xp = dram("xp", [S, D])
    c_d = dram("c", [128, 8])
    adaw = dram("ada_w", [D, 6 * D]); adab = dram("ada_b", [6 * D])
    nmix = dram("norm_mix", [D]); nffn = dram("norm_ffn", [D])
    w_in = dram("w_in", [D, 3 * D]); w_out = dram("w_out", [D, D])
    lam = dram("lam", [4, 64]); subln = dram("subln", [128, 1])
    kaug = dram("kaug", [H, 8, 2, S], BF16); qaug = dram("qaug", [8, 2, TOK], BF16)
    dfix = dram("dfix", [128, H, 128], BF16)
    ident_d = dram("ident", [128, 128], BF16); identf_d = dram("identf", [128, 128])
    wr = dram("wr", [128, 8, 20]); br = dram("br", [20])
    wg = dram("wg", [NE, D, DE]); wu = dram("wu", [NE, D, DE]); wd = dram("wd", [NE, DE, D])
    xo = dram("xo", [TOK, D], F32, "ExternalOutput")
    if DBG.get("on"):
        DBG["obf"] = dram("d_obf", [TOK, D], BF16, "ExternalOutput")
        DBG["L"] = dram("d_L", [TOK, 20], F32, "ExternalOutput")
        DBG["gates"] = dram("d_gates", [TOK, 16], F32, "ExternalOutput")
        DBG["xmid"] = dram("d_xmid", [TOK, D], F32, "ExternalOutput")
    kscr = dram("kscr", [2, H, 64, S], BF16, "Internal")
    qscr = dram("qscr", [2, H, 64, TOK], BF16, "Internal")
    vscr = dram("vscr", [S // 128, 128, H, 129], BF16, "Internal")

    P = Prog(nc)
    with contextlib.ExitStack() as st:
        cx = Ctx()
        cx.P = P
        cx.A = A = Arena(nc, st)
        cx.ps = ps = st.enter_context(nc.psum_tensor("ps", [128, 8, 512], F32))
        setup_common(cx, ident_d, identf_d)
        stage_mod(cx, c_d, adaw, adab, nmix, nffn, "m0")
        mt = cx.modtag
        layer0_body(cx, xp, w_in, w_out, lam, subln, kaug, qaug, dfix, kscr, qscr, vscr, wr, br, wg, wu, wd)
        P.load(xo.rearrange("(t p) d -> p t d", p=128), cx.xres, reads=[f"xres{t}" for t in range(NT)], writes=["xo"])
        P.emit()
    return nc


def build_B():
    nc = bass.Bass("TRN2", target_bir_lowering=False)
    dram = lambda n, s, dt=F32, kind="ExternalInput": nc.dram_tensor(n, s, dt, kind=kind).ap()
    x1 = dram("x1", [S, D])
    c_d = dram("c", [128, 8])
    adaw = dram("ada_w", [D, 6 * D]); adab = dram("ada_b", [6 * D])
    nmix = dram("norm_mix", [D]); nffn = dram("norm_ffn", [D])
    fwin = dram("fwin", [D, 256])
    cs_d = dram("cs", [128, 2, 512], BF16)
    r1_d = dram("r1", [128, 256], BF16); r2_d = dram("r2", [128, 256], BF16)
    twr_d = dram("twr", [128, 128]); twi_d = dram("twi", [128, 128])
    bdc_d = dram("bdc", [128, 128], BF16); bds_d = dram("bds", [128, 128], BF16)
    ident_d = dram("ident", [128, 128], BF16); identf_d = dram("identf", [128, 128])
    fo = dram("fo", [S, 256], BF16, "ExternalOutput")
    P = Prog(nc)
    with contextlib.ExitStack() as st:
        cx = Ctx()
        cx.P = P
        cx.A = A = Arena(nc, st)
        cx.ps = ps = st.enter_context(nc.psum_tensor("ps", [128, 8, 512], F32))
        setup_common(cx, ident_d, identf_d)
        stage_mod(cx, c_d, adaw, adab, nmix, nffn, "m1")
        mt = cx.modtag
        cs = A.alloc([2, 512], BF16); r1 = A.alloc([256], BF16); r2 = A.alloc([256], BF16)
        twr = A.alloc([128], F32); twi = A.alloc([128], F32)
        bdc = A.alloc([128], BF16); bds = A.alloc([128], BF16)
        for ap_, d_, k_ in [(cs, cs_d, "cs"), (r1, r1_d, "r1"), (r2, r2_d, "r2"), (twr, twr_d, "twr"),
                            (twi, twi_d, "twi"), (bdc, bdc_d, "bdc"), (bds, bds_d, "bds")]:
            P.load(ap_, d_, writes=[k_])
        uT = A.alloc([2, S], BF16)
        m2 = A.mark()
        winb = A.alloc([8, 256], BF16)
        P.load(winb, fwin.rearrange("(kc p) n -> p kc n", p=128), writes=["winb"], eng="pool")
        norm_scratch(cx)
        xt = [A.alloc([D], F32) for _ in range(3)]
        hTc = [A.alloc([8, 512], BF16) for _ in range(2)]
        xv = x1.rearrange("(t p) d -> t p d", p=128)
        vk1 = [mt + "A1", mt + "B1"]
        ixt = 0
        ipb = 0
        for ch in range(S // 512):
            cb = ch % 2
            hk = f"hTc{cb}"
            for t in range(4):
                xb = xt[ixt % 3]
                xk = f"xt{ixt % 3}"
                ixt += 1
                P.load(xb, xv[ch * 4 + t], writes=[xk])
                norm_T(cx, xb, xk, cx.A1, cx.B1, vk1, hTc[cb][:, :, t * 128:(t + 1) * 128], hk)
            for mc in range(2):
                bank = ipb % 4
                ipb += 1
                for kc in range(8):
                    P.mm(ps[:, bank, :], winb[:, kc, mc * 128:(mc + 1) * 128], hTc[cb][:, kc, :], start=(kc == 0), stop=(kc == 7),
                         reads=["winb", hk], writes=[f"ps{bank}"])
                P.cp("act" if mc == 0 else "dve", uT[:, mc, ch * 512:(ch + 1) * 512], ps[:, bank, :],
                     reads=[f"ps{bank}"], writes=[f"uT{ch}"])
        P.fence()
        A.release(m2)
        zsb = A.alloc([2, 256, 64], BF16)
        zflat = zsb.rearrange("p r c n -> p (r c) n")
        for n2 in range(64):
            bank = n2 % 4
            for mc in range(2):
                P.mm(ps[:, bank, :], uT[:, mc, n2:S:64], cs[:, mc, :], start=(mc == 0), stop=(mc == 1),
                     reads=["cs"], writes=[f"ps{bank}"])
            P.cp("act" if n2 % 2 == 0 else "dve", zflat[:, :, n2], ps[:, bank, :], reads=[f"ps{bank}"], writes=["zsb"])
        P.fence()
        fsb = A.alloc([64, 256], BF16)
        p1 = [A.alloc([4, 2, 128], F32) for _ in range(2)]
        p2 = [A.alloc([4, 2, 128], F32) for _ in range(2)]
        apr = [A.alloc([4, 128], BF16) for _ in range(2)]
        api = [A.alloc([4, 128], BF16) for _ in range(2)]
        twr_b = twr[:, None, None, :].broadcast_to([128, 4, 2, 128])
        twi_b = twi[:, None, None, :].broadcast_to([128, 4, 2, 128])
        for grp in range(32):
            gb = grp % 2
            ab = 2 * gb
            for pi in range(4):
                pp = grp * 4 + pi
                bank = ab + pi // 2
                out = ps[:, bank, (pi % 2) * 256:(pi % 2) * 256 + 256]
                P.mm(out, zsb[:, 0, 2 * pp:2 * pp + 2, :].rearrange("p c n -> p (c n)"), r1, start=True, stop=False,
                     reads=["r1"], writes=[f"ps{bank}"], skip_group_check=True)
                P.mm(out, zsb[:, 1, 2 * pp:2 * pp + 2, :].rearrange("p c n -> p (c n)"), r2, start=False, stop=True,
                     reads=["r2"], writes=[f"ps{bank}"], skip_group_check=True)
            Av = ps[:, ab:ab + 2, :].rearrange("p a (b r k) -> p (a b) r k", r=2, k=128)
            P.tt("dve", p1[gb], Av, twr_b, ALU.mult, reads=[f"ps{ab}", f"ps{ab + 1}", "twr"], writes=[f"p1{gb}"])
            P.tt("dve", p2[gb], Av, twi_b, ALU.mult, reads=[f"ps{ab}", f"ps{ab + 1}", "twi"], writes=[f"p2{gb}"])
            P.tt("pool", apr[gb], p1[gb][:, :, 0, :], p2[gb][:, :, 1, :], ALU.subtract, reads=[f"p1{gb}", f"p2{gb}"], writes=[f"apr{gb}"])
            P.tt("pool", api[gb], p2[gb][:, :, 0, :], p1[gb][:, :, 1, :], ALU.add, reads=[f"p1{gb}", f"p2{gb}"], writes=[f"api{gb}"])
            fb = 4 + gb
            for pi in range(4):
                out = ps[:, fb, pi * 128:(pi + 1) * 128]
                P.mm(out, apr[gb][:, pi, :], bdc, start=True, stop=False, reads=[f"apr{gb}", "bdc"], writes=[f"ps{fb}"], skip_group_check=True)
                P.mm(out, api[gb][:, pi, :], bds, start=False, stop=True, reads=[f"api{gb}", "bds"], writes=[f"ps{fb}"], skip_group_check=True)
            src = ps[:, fb, :].rearrange("p (pi k c) -> p k pi c", pi=4, k=64, c=2)
            dst = fsb[:, :, 8 * grp:8 * grp + 8].rearrange("p k (pi c) -> p k pi c", c=2)
            P.cp("act" if grp % 2 == 0 else "dve", dst, src, reads=[f"ps{fb}"], writes=["fsb"])
        P.load(fo.rearrange("(k2 k1) c -> k1 k2 c", k1=128), fsb, reads=["fsb"], writes=["fo"])
        P.emit()
    return nc


def build_C():
    nc = bass.Bass("TRN2", target_bir_lowering=False)
    dram = lambda n, s, dt=F32, kind="ExternalInput": nc.dram_tensor(n, s, dt, kind=kind).ap()
    x1s = dram("x1s", [TOK, D])
    fsh = dram("fsh", [TOK, D], BF16)
    c_d = dram("c", [128, 8])
    adaw = dram("ada_w", [D, 6 * D]); adab = dram("ada_b", [6 * D])
    nmix = dram("norm_mix", [D]); nffn = dram("norm_ffn", [D])
    fwout = dram("fwout", [D, D])
    nfin = dram("norm_final", [D])
    ident_d = dram("ident", [128, 128], BF16); identf_d = dram("identf", [128, 128])
    wr = dram("wr", [128, 8, 20]); br = dram("br", [20])
    wg = dram("wg", [NE, D, DE]); wu = dram("wu", [NE, D, DE]); wd = dram("wd", [NE, DE, D])
    yo = dram("yo", [TOK, D], F32, "ExternalOutput")
    P = Prog(nc)
    with contextlib.ExitStack() as st:
        cx = Ctx()
        cx.P = P
        cx.A = A = Arena(nc, st)
        cx.ps = ps = st.enter_context(nc.psum_tensor("ps", [128, 8, 512], F32))
        setup_common(cx, ident_d, identf_d)
        stage_mod(cx, c_d, adaw, adab, nmix, nffn, "m1")
        mt = cx.modtag
        cx.xres = A.alloc([NT, D], F32)
        xkeys = [f"xres{t}" for t in range(NT)]
        P.load(cx.xres, x1s.rearrange("(t p) d -> p t d", p=128), writes=xkeys)
        m0 = A.mark()
        norm_scratch(cx)
        fsb = A.alloc([NT, D], BF16)
        fT = A.alloc([8, TOK], BF16)
        wob = A.alloc([8, D], BF16)
        P.load(fsb, fsh.rearrange("(t p) d -> p t d", p=128), writes=["fsb"])
        P.load(wob, fwout.rearrange("(kc p) n -> p kc n", p=128), writes=["wob"], eng="pool")
        for kc in range(8):
            P.tt("pool", wob[:, kc, :], wob[:, kc, :], cx.g1bc, ALU.mult, reads=["wob", mt + "g1bc"], writes=["wob"])
        for t in range(NT):
            transpose_tok(cx, fsb[:, t, :], "fsb", fT[:, :, t * 128:(t + 1) * 128], f"fT{t}", eng=("dve" if t % 2 else "act"))
        proj_residual(cx, fT, [f"fT{t}" for t in range(NT)], wob, "wob")
        P.fence()
        A.release(m0)
        stage_moe(cx, wr, br, wg, wu, wd, "e1")
        nfb = A.alloc([D], F32)
        P.load(nfb, nfin.partition_broadcast(128), writes=["nfb"])
        junk = A.alloc([D], BF16)
        fst = A.alloc([NT, 4], F32)
        ot = [A.alloc([D], F32) for _ in range(2)]
        yv = yo.rearrange("(t p) d -> t p d", p=128)
        for t in range(NT):
            P.act(junk, cx.xres[:, t, :], AF.Square, reads=[f"xres{t}"], writes=["fjunk", f"fst{t}"], accum_out=fst[:, t, 0:1])
            P.ts("dve", fst[:, t, 1:2], fst[:, t, 0:1], 1.0 / D, EPS, ALU.mult, ALU.add, reads=[f"fst{t}"], writes=[f"fst{t}"])
            P.act(fst[:, t, 2:3], fst[:, t, 1:2], AF.Sqrt, reads=[f"fst{t}"], writes=[f"fst{t}"])
            P.op("dve", lambda e, o=fst[:, t, 3:4], i_=fst[:, t, 2:3]: e.reciprocal(o, i_), reads=[f"fst{t}"], writes=[f"fst{t}"])
            ob = ot[t % 2]
            P.stt("dve", ob, cx.xres[:, t, :], fst[:, t, 3:4], nfb, ALU.mult, ALU.mult,
                  reads=[f"xres{t}", f"fst{t}", "nfb"], writes=[f"ot{t % 2}"])
            P.load(yv[t], ob, reads=[f"ot{t % 2}"], writes=[f"yo{t}"])
        P.emit()
    return nc


RG = [[0, 1, 2, 3], [4, 5, 6, 7]]
DEBUG_SKIP0 = False
DBG = {}


def fourier_body(cx, fwin, hTd, hTg, fd, tabs):
    P, A, ps = cx.P, cx.A, cx.ps
    mt = cx.modtag
    cs_d, r1_d, r2_d, twr_d, twi_d, bdc_d, bds_d = tabs
    m_f = A.mark()
    cs = A.alloc([2, 2, 256], BF16); r1 = A.alloc([256], BF16); r2 = A.alloc([256], BF16)
    twr = A.alloc([128], F32); twi = A.alloc([128], F32)
    bdc = A.alloc([128], BF16); bds = A.alloc([128], BF16)
    for ap_, d_, k_ in [(cs, cs_d, "cs"), (r1, r1_d, "r1"), (r2, r2_d, "r2"), (twr, twr_d, "twr"),
                        (twi, twi_d, "twi"), (bdc, bdc_d, "bdc"), (bds, bds_d, "bds")]:
        P.load(ap_, d_, writes=[k_])
    uT = A.alloc([2, S], BF16)
    u_off = A.last
    m2 = A.mark()
    norm_scratch(cx)
    hTo = A.alloc([8, TOK], BF16)
    vk1 = [mt + "A1", mt + "B1"]
    for t in range(NT):
        norm_T(cx, cx.xres[:, t, :], f"xres{t}", cx.A1, cx.B1, vk1, hTo[:, :, t * 128:(t + 1) * 128], f"hTo{t}")
    for i in range(4):
        P.load(hTd[i].rearrange("(kc p) t -> p kc t", p=128), hTo[:, :, i * 512:(i + 1) * 512],
               reads=[f"hTo{t}" for t in range(4 * i, 4 * i + 4)], writes=[f"hTd{i}"])
        P.cc("AllGather", RG, hTd[i], hTg[i], reads=[f"hTd{i}"], writes=[f"hTg{i}"])
    winb = A.alloc([8, 256], BF16)
    P.load(winb, fwin.rearrange("(kc p) n -> p kc n", p=128), writes=["winb"], eng="pool")
    hTc = [A.alloc([8, 512], BF16) for _ in range(2)]
    hTg_v = [g_.rearrange("(r kc p) t -> r p kc t", kc=8, p=128) for g_ in hTg]
    ipb = 0
    for it_ in range(S // 512):
        c4, r_ = it_ // 4, it_ % 4
        ch = 4 * r_ + c4
        cb = it_ % 2
        hk = f"hTc{cb}"
        P.load(hTc[cb], hTg_v[c4][r_], reads=[f"hTg{c4}"], writes=[hk])
        for mc in range(2):
            bank = ipb % 4
            ipb += 1
            for kc in range(8):
                P.mm(ps[:, bank, :], winb[:, kc, mc * 128:(mc + 1) * 128], hTc[cb][:, kc, :], start=(kc == 0), stop=(kc == 7),
                     reads=["winb", hk], writes=[f"ps{bank}"])
            P.cp("act" if mc == 0 else "dve", uT[:, mc, ch * 512:(ch + 1) * 512], ps[:, bank, :],
                 reads=[f"ps{bank}"], writes=[f"uT{ch}"])
    P.fence()
    A.release(m2)
    zsb = A.alloc([2, 128, 64], BF16)
    zflat = zsb.rearrange("p r c n -> p (r c) n")
    fsb = A.alloc([64, 256], BF16)
    p1 = A.alloc([4, 2, 128], F32)
    p2 = A.alloc([4, 2, 128], F32)
    apr = [A.alloc([4, 128], BF16) for _ in range(2)]
    api = [A.alloc([4, 128], BF16) for _ in range(2)]
    twr_b = twr[:, None, None, :].broadcast_to([128, 4, 2, 128])
    twi_b = twi[:, None, None, :].broadcast_to([128, 4, 2, 128])
    for hf in range(2):
        for n2 in range(64):
            bank = n2 % 2
            for mc in range(2):
                P.mm(ps[:, bank, 0:256], uT[:, mc, n2:S:64], cs[:, mc, hf, :], start=(mc == 0), stop=(mc == 1),
                     reads=["cs"], writes=[f"ps{bank}"])
            P.cp("act" if n2 % 2 == 0 else "dve", zflat[:, :, n2], ps[:, bank, 0:256], reads=[f"ps{bank}"], writes=["zsb"])
        for grp in range(16):
            gb = grp % 2
            ab = 2 + 2 * gb
            for pi in range(4):
                pp = grp * 4 + pi
                bank = ab + pi // 2
                out = ps[:, bank, (pi % 2) * 256:(pi % 2) * 256 + 256]
                P.mm(out, zsb[:, 0, 2 * pp:2 * pp + 2, :].rearrange("p c n -> p (c n)"), r1, start=True, stop=False,
                     reads=["r1", "zsb"], writes=[f"ps{bank}"], skip_group_check=True)
                P.mm(out, zsb[:, 1, 2 * pp:2 * pp + 2, :].rearrange("p c n -> p (c n)"), r2, start=False, stop=True,
                     reads=["r2", "zsb"], writes=[f"ps{bank}"], skip_group_check=True)
            Av = ps[:, ab:ab + 2, :].rearrange("p a (b r k) -> p (a b) r k", r=2, k=128)
            P.tt("dve", p1, Av, twr_b, ALU.mult, reads=[f"ps{ab}", f"ps{ab + 1}", "twr"], writes=["p1"])
            P.tt("dve", p2, Av, twi_b, ALU.mult, reads=[f"ps{ab}", f"ps{ab + 1}", "twi"], writes=["p2"])
            P.tt("pool", apr[gb], p1[:, :, 0, :], p2[:, :, 1, :], ALU.subtract, reads=["p1", "p2"], writes=[f"apr{gb}"])
            P.tt("pool", api[gb], p2[:, :, 0, :], p1[:, :, 1, :], ALU.add, reads=["p1", "p2"], writes=[f"api{gb}"])
            fb = 6 + gb
            for pi in range(4):
                out = ps[:, fb, pi * 128:(pi + 1) * 128]
                P.mm(out, apr[gb][:, pi, :], bdc, start=True, stop=False, reads=[f"apr{gb}", "bdc"], writes=[f"ps{fb}"], skip_group_check=True)
                P.mm(out, api[gb][:, pi, :], bds, start=False, stop=True, reads=[f"api{gb}", "bds"], writes=[f"ps{fb}"], skip_group_check=True)
            src = ps[:, fb, :].rearrange("p (pi k c) -> p k pi c", pi=4, k=64, c=2)
            c0 = hf * 128 + 8 * grp
            dst = fsb[:, :, c0:c0 + 8].rearrange("p k (pi c) -> p k pi c", c=2)
            P.cp("act" if grp % 2 == 0 else "dve", dst, src, reads=[f"ps{fb}"], writes=["fsb"])
    for i in range(4):
        P.load(fd[i].rearrange("(k2 k1) c -> k1 k2 c", k1=128), fsb[:, 16 * i:16 * (i + 1), :], reads=["fsb"], writes=[f"fd{i}"])
    P.fence()
    A.release(m_f)


def tail_body(cx, fd, fg, sel_d, fwout, nfin, wr, br, wg, wu, wd, yo):
    P, A, ps = cx.P, cx.A, cx.ps
    mt = cx.modtag
    for i in range(4):
        P.cc("AllGather", RG, fd[i], fg[i], reads=[f"fd{i}"], writes=[f"fg{i}"])
    m0 = A.mark()
    sel = A.alloc([4], F32)
    P.load(sel, sel_d, writes=["sel"])
    fsel = A.alloc([NT, D], BF16)
    wob = A.alloc([8, D], BF16)
    cand = A.alloc([NT, D], BF16)
    c_off = A.last
    P.load(wob, fwout.rearrange("(kc p) n -> p kc n", p=128), writes=["wob"], eng="pool")
    for kc in range(8):
        P.tt("pool", wob[:, kc, :], wob[:, kc, :], cx.g1bc, ALU.mult, reads=["wob", mt + "g1bc"], writes=["wob"])
    for r_ in range(4):
        fg_v = fg[r_].rearrange("(g t p) c -> g p t c", g=4, p=128)
        for g in range(4):
            P.load(cand[:, :, g * 256:(g + 1) * 256], fg_v[g], reads=[f"fg{r_}"], writes=["cand"])
        if r_ == 0:
            P.ts("dve", fsel, cand, sel[:, 0:1], None, ALU.mult, reads=["cand", "sel"], writes=["fsel"])
        else:
            P.stt("dve", fsel, cand, sel[:, r_:r_ + 1], fsel, ALU.mult, ALU.add, reads=["cand", "sel"], writes=["fsel"])
    P.fence()
    fT = A.view(c_off, [8, TOK], BF16)
    for t in range(NT):
        transpose_tok(cx, fsel[:, t, :], "fsel", fT[:, :, t * 128:(t + 1) * 128], f"fT{t}", eng=("dve" if t % 2 else "act"))
    proj_residual(cx, fT, [f"fT{t}" for t in range(NT)], wob, "wob")
    P.fence()
    A.release(m0)
    stage_moe(cx, wr, br, wg, wu, wd, "e1")
    nfb = A.alloc([D], F32)
    P.load(nfb, nfin.partition_broadcast(128), writes=["nfb"])
    junk = A.alloc([D], BF16)
    fst = A.alloc([NT, 4], F32)
    ot = [A.alloc([D], F32) for _ in range(2)]
    yv = yo.rearrange("(t p) d -> t p d", p=128)
    for t in range(NT):
        P.act(junk, cx.xres[:, t, :], AF.Square, reads=[f"xres{t}"], writes=["fjunk", f"fst{t}"], accum_out=fst[:, t, 0:1])
        P.ts("dve", fst[:, t, 1:2], fst[:, t, 0:1], 1.0 / D, EPS, ALU.mult, ALU.add, reads=[f"fst{t}"], writes=[f"fst{t}"])
        P.act(fst[:, t, 2:3], fst[:, t, 1:2], AF.Sqrt, reads=[f"fst{t}"], writes=[f"fst{t}"])
        P.op("dve", lambda e, o=fst[:, t, 3:4], i_=fst[:, t, 2:3]: e.reciprocal(o, i_), reads=[f"fst{t}"], writes=[f"fst{t}"])
        ob = ot[t % 2]
        P.stt("dve", ob, cx.xres[:, t, :], fst[:, t, 3:4], nfb, ALU.mult, ALU.mult,
              reads=[f"xres{t}", f"fst{t}", "nfb"], writes=[f"ot{t % 2}"])
        P.load(yv[t], ob, reads=[f"ot{t % 2}"], writes=[f"yo{t}"])


def build_F():
    nc = bass.Bass("TRN2", target_bir_lowering=False)
    dram = lambda n, s, dt=F32, kind="ExternalInput": nc.dram_tensor(n, s, dt, kind=kind).ap()
    xp = dram("xp", [S, D])
    c_d = dram("c", [128, 8])
    adaw = dram("ada_w", [2, D, 6 * D]); adab = dram("ada_b", [2, 6 * D])
    nmix = dram("norm_mix", [2, D]); nffn = dram("norm_ffn", [2, D])
    if not DEBUG_SKIP0:
        w_in = dram("w_in", [D, 3 * D]); w_out = dram("w_out", [D, D])
        lam = dram("lam", [4, 64]); subln = dram("subln", [128, 1])
        kaug = dram("kaug", [H, 8, 2, S], BF16); qaug = dram("qaug", [8, 2, TOK], BF16)
        dfix = dram("dfix", [128, H, 128], BF16)
    ident_d = dram("ident", [128, 128], BF16); identf_d = dram("identf", [128, 128])
    wr = dram("wr", [2, 128, 8, 20]); br = dram("br", [2, 20])
    wg = dram("wg", [2, NE, D, DE]); wu = dram("wu", [2, NE, D, DE]); wd = dram("wd", [2, NE, DE, D])
    fwin = dram("fwin", [D, 256]); fwout = dram("fwout", [D, D]); nfin = dram("norm_final", [D])
    cs_d = dram("cs", [128, 2, 2, 256], BF16)
    r1_d = dram("r1", [128, 256], BF16); r2_d = dram("r2", [128, 256], BF16)
    twr_d = dram("twr", [128, 128]); twi_d = dram("twi", [128, 128])
    bdc_d = dram("bdc", [128, 128], BF16); bds_d = dram("bds", [128, 128], BF16)
    sel_d = dram("sel", [128, 4])
    yo = dram("yo", [TOK, D], F32, "ExternalOutput")
    if not DEBUG_SKIP0:
        kscr = dram("kscr", [2, H, 64, S], BF16, "Internal")
        qscr = dram("qscr", [2, H, 64, TOK], BF16, "Internal")
        vscr = dram("vscr", [S // 128, 128, H, 129], BF16, "Internal")
    hTd = [dram(f"hTd{i}", [D, 512], BF16, "Internal") for i in range(4)]
    hTg = [dram(f"hTg{i}", [4 * D, 512], BF16, "Internal") for i in range(4)]
    fd = [dram(f"fd{i}", [TOK, 256], BF16, "Internal") for i in range(4)]
    fg = [dram(f"fg{i}", [4 * TOK, 256], BF16, "Internal") for i in range(4)]
    P = Prog(nc)
    with contextlib.ExitStack() as st:
        cx = Ctx()
        cx.P = P
        cx.A = A = Arena(nc, st)
        cx.ps = st.enter_context(nc.psum_tensor("ps", [128, 8, 512], F32))
        setup_common(cx, ident_d, identf_d)
        stage_mod(cx, c_d, adaw[0], adab[0], nmix[0], nffn[0], "m0")
        if DEBUG_SKIP0:
            cx.xres = A.alloc([NT, D], F32)
            P.load(cx.xres, xp[0:TOK].rearrange("(t p) d -> p t d", p=128), writes=[f"xres{t}" for t in range(NT)])
        else:
            layer0_body(cx, xp, w_in, w_out, lam, subln, kaug, qaug, dfix, kscr, qscr, vscr, wr[0], br[0], wg[0], wu[0], wd[0])
        stage_mod(cx, c_d, adaw[1], adab[1], nmix[1], nffn[1], "m1")
        fourier_body(cx, fwin, hTd, hTg, fd, (cs_d, r1_d, r2_d, twr_d, twi_d, bdc_d, bds_d))
        tail_body(cx, fd, fg, sel_d, fwout, nfin, wr[1], br[1], wg[1], wu[1], wd[1], yo)
        P.emit()
    return nc


def _bf(a):
    return np.asarray(a, dtype=np.float32).astype(ml_dtypes.bfloat16)


def _consts():
    ident = _bf(np.eye(128))
    identf = np.eye(128, dtype=np.float32)
    return ident, identf


def _attn_tables(j):
    own = np.arange(TOK * j, TOK * (j + 1))
    others = np.concatenate([np.arange(0, TOK * j), np.arange(TOK * (j + 1), S)])
    perm = np.concatenate([own, others])
    pos = perm.astype(np.float64)
    kt_abs = np.floor(pos / 128) * 128
    kr = pos - kt_abs
    slopes = 2.0 ** (-(np.arange(H) + 1.0))
    kaug = np.zeros((H, 8, 2, S), np.float64)
    after = (perm >= TOK * (j + 1))
    own_m = np.arange(S) < TOK
    use2 = np.where(own_m | after, 1.0, 0.0)
    for h in range(H):
        sl = slopes[h]
        set1 = np.stack([np.full(S, -sl), np.full(S, -sl), sl * kt_abs, sl * kr])
        kaug[h, 0:4, :, :] = set1[:, None, :]
        kaug[h, 4:8, :, :] = (-2.0 * set1 * use2[None, :])[:, None, :]
    qpos = own.astype(np.float64)
    qt_abs = np.floor(qpos / 128) * 128
    qr = qpos - qt_abs
    q4 = np.stack([qt_abs, qr, np.ones(TOK), np.ones(TOK)])
    qaug = np.zeros((8, 2, TOK), np.float64)
    qaug[0:4] = q4[:, None, :]
    qaug[4:8] = q4[:, None, :]
    krr = np.arange(128)[:, None]
    qrr = np.arange(128)[None, :]
    dfix = np.zeros((128, H, 128), np.float64)
    for h in range(H):
        dfix[:, h, :] = -2.0 * slopes[h] * np.maximum(krr - qrr, 0)
    return perm, _bf(kaug), _bf(qaug), _bf(dfix)


_NC_CACHE = {}


def _get(name, fn):
    if name not in _NC_CACHE:
        _NC_CACHE[name] = fn()
    return _NC_CACHE[name]


def run_A(inp):
    ident, identf = _consts()
    maps = []
    for c in range(NCORE):
        b, j = c // 4, c % 4
        perm, kaug, qaug, dfix = _attn_tables(j)
        lam = np.stack([inp["attn_lam_q1"][0], inp["attn_lam_k1"][0], inp["attn_lam_q2"][0], inp["attn_lam_k2"][0]])
        wr = np.concatenate([inp["router_group_w"][0], inp["router_expert_w"][0].reshape(D, 16)], axis=1)
        br = np.concatenate([inp["router_group_b"][0], inp["router_expert_b"][0].reshape(16)])
        maps.append({
            "xp": np.ascontiguousarray(inp["x"][b][perm]),
            "c": np.ascontiguousarray(inp["c"][b].reshape(8, 128).T),
            "ada_w": inp["ada_w"][0], "ada_b": inp["ada_b"][0],
            "norm_mix": inp["norm_mix"][0], "norm_ffn": inp["norm_ffn"][0],
            "w_in": inp["attn_w_in"][0], "w_out": inp["attn_w_out"][0],
            "lam": np.ascontiguousarray(lam), "subln": np.ascontiguousarray(inp["attn_subln"][0].reshape(128, 1)),
            "kaug": kaug, "qaug": qaug, "dfix": dfix, "ident": ident, "identf": identf,
            "wr": np.ascontiguousarray(wr.reshape(8, 128, 20).transpose(1, 0, 2)), "br": np.ascontiguousarray(br),
            "wg": inp["expert_w_gate"][0], "wu": inp["expert_w_up"][0], "wd": inp["expert_w_down"][0],
        })
    nc = _get("A", build_A)
    res = run_bass_kernel_spmd(nc, maps, core_ids=list(range(NCORE)))
    x1 = np.zeros((B, S, D), np.float32)
    for c in range(NCORE):
        b, j = c // 4, c % 4
        x1[b, TOK * j:TOK * (j + 1)] = res.results[c]["xo"]
    if DBG.get("on"):
        DBG["res"] = [{k: np.asarray(v) for k, v in r.items()} for r in res.results]
    return x1


def _fourier_tables():
    m = np.arange(256)[:, None].astype(np.float64); l = np.arange(256)[None, :].astype(np.float64)
    ang = 2 * np.pi * m * l / 256
    cs = np.concatenate([np.cos(ang), -np.sin(ang)], axis=1) / 16.0
    cs = cs.reshape(2, 128, 512).transpose(1, 0, 2)
    n1 = np.arange(128)[:, None].astype(np.float64); k1 = np.arange(128)[None, :].astype(np.float64)
    a = 2 * np.pi * n1 * k1 / 128
    r1 = np.concatenate([np.cos(a), -np.sin(a)], axis=1)
    r2 = np.concatenate([np.sin(a), np.cos(a)], axis=1)
    n2 = (np.arange(128) % 64)[:, None].astype(np.float64)
    tw = 2 * np.pi * n2 * k1 / 8192
    twr = np.cos(tw); twi = -np.sin(tw)
    bdc = np.zeros((128, 128)); bds = np.zeros((128, 128))
    sc = 1.0 / math.sqrt(8192.0)
    for c in range(2):
        nn = np.arange(64)[:, None].astype(np.float64); kk = np.arange(64)[None, :].astype(np.float64)
        a2 = 2 * np.pi * nn * kk / 64
        bdc[c * 64:(c + 1) * 64, c::2] = np.cos(a2) * sc
        bds[c * 64:(c + 1) * 64, c::2] = np.sin(a2) * sc
    return (_bf(cs), _bf(r1), _bf(r2), twr.astype(np.float32), twi.astype(np.float32), _bf(bdc), _bf(bds))


def _moe_router(inp, i):
    wr = np.concatenate([inp["router_group_w"][i], inp["router_expert_w"][i].reshape(D, 16)], axis=1)
    br = np.concatenate([inp["router_group_b"][i], inp["router_expert_b"][i].reshape(16)])
    return np.ascontiguousarray(wr.reshape(8, 128, 20).transpose(1, 0, 2)), np.ascontiguousarray(br)


def run_B(inp, x1):
    ident, identf = _consts()
    cs, r1, r2, twr, twi, bdc, bds = _fourier_tables()
    maps = []
    for c in range(NCORE):
        b, g = c // 4, c % 4
        maps.append({
            "x1": np.ascontiguousarray(x1[b]),
            "c": np.ascontiguousarray(inp["c"][b].reshape(8, 128).T),
            "ada_w": inp["ada_w"][1], "ada_b": inp["ada_b"][1],
            "norm_mix": inp["norm_mix"][1], "norm_ffn": inp["norm_ffn"][1],
            "fwin": np.ascontiguousarray(inp["fourier_w_in"][0][:, 256 * g:256 * (g + 1)]),
            "cs": cs, "r1": r1, "r2": r2, "twr": twr, "twi": twi, "bdc": bdc, "bds": bds,
            "ident": ident, "identf": identf,
        })
    nc = _get("B", build_B)
    res = run_bass_kernel_spmd(nc, maps, core_ids=list(range(NCORE)))
    f = np.zeros((B, S, D), ml_dtypes.bfloat16)
    for c in range(NCORE):
        b, g = c // 4, c % 4
        f[b, :, 256 * g:256 * (g + 1)] = res.results[c]["fo"]
    return f


def run_C(inp, x1, f):
    ident, identf = _consts()
    wr, br = _moe_router(inp, 1)
    maps = []
    for c in range(NCORE):
        b, j = c // 4, c % 4
        maps.append({
            "x1s": np.ascontiguousarray(x1[b, TOK * j:TOK * (j + 1)]),
            "fsh": np.ascontiguousarray(f[b, TOK * j:TOK * (j + 1)]),
            "c": np.ascontiguousarray(inp["c"][b].reshape(8, 128).T),
            "ada_w": inp["ada_w"][1], "ada_b": inp["ada_b"][1],
            "norm_mix": inp["norm_mix"][1], "norm_ffn": inp["norm_ffn"][1],
            "fwout": inp["fourier_w_out"][0], "norm_final": inp["norm_final"],
            "ident": ident, "identf": identf, "wr": wr, "br": br,
            "wg": inp["expert_w_gate"][1], "wu": inp["expert_w_up"][1], "wd": inp["expert_w_down"][1],
        })
    nc = _get("C", build_C)
    res = run_bass_kernel_spmd(nc, maps, core_ids=list(range(NCORE)))
    out = np.zeros((B, S, D), np.float32)
    for c in range(NCORE):
        b, j = c // 4, c % 4
        out[b, TOK * j:TOK * (j + 1)] = res.results[c]["yo"]
    return out


def run_F(inp):
    ident, identf = _consts()
    cs, r1, r2, twr, twi, bdc, bds = _fourier_tables()
    csf = np.asarray(cs).reshape(128, 2, 2, 2, 128).transpose(0, 1, 3, 2, 4).reshape(128, 2, 2, 256)
    csf = np.ascontiguousarray(csf)
    lam = np.ascontiguousarray(np.stack([inp["attn_lam_q1"][0], inp["attn_lam_k1"][0],
                                         inp["attn_lam_q2"][0], inp["attn_lam_k2"][0]]))
    wr0, br0 = _moe_router(inp, 0)
    wr1, br1 = _moe_router(inp, 1)
    wr = np.ascontiguousarray(np.stack([wr0, wr1])); br = np.ascontiguousarray(np.stack([br0, br1]))
    maps = []
    for c in range(NCORE):
        b, j = c // 4, c % 4
        perm, kaug, qaug, dfix = _attn_tables(j)
        sel = np.zeros((128, 4), np.float32)
        sel[:, j] = 1.0
        maps.append({
            "xp": np.ascontiguousarray(inp["x"][b][perm]),
            "c": np.ascontiguousarray(inp["c"][b].reshape(8, 128).T),
            "ada_w": inp["ada_w"], "ada_b": inp["ada_b"],
            "norm_mix": inp["norm_mix"], "norm_ffn": inp["norm_ffn"],
            "w_in": inp["attn_w_in"][0], "w_out": inp["attn_w_out"][0],
            "lam": lam, "subln": np.ascontiguousarray(inp["attn_subln"][0].reshape(128, 1)),
            "kaug": kaug, "qaug": qaug, "dfix": dfix, "ident": ident, "identf": identf,
            "wr": wr, "br": br,
            "wg": inp["expert_w_gate"], "wu": inp["expert_w_up"], "wd": inp["expert_w_down"],
            "fwin": np.ascontiguousarray(inp["fourier_w_in"][0][:, 256 * j:256 * (j + 1)]),
            "fwout": inp["fourier_w_out"][0], "norm_final": inp["norm_final"],
            "cs": csf, "r1": r1, "r2": r2, "twr": twr, "twi": twi, "bdc": bdc, "bds": bds, "sel": sel,
        })
    nc = _get("F", build_F)
    if DEBUG_SKIP0:
        drop = {"w_in", "w_out", "lam", "subln", "kaug", "qaug", "dfix"}
        maps = [{k: v for k, v in m.items() if k not in drop} for m in maps]
    res = run_bass_kernel_spmd(nc, maps, core_ids=list(range(NCORE)))
    out = np.zeros((B, S, D), np.float32)
    for c in range(NCORE):
        b, j = c // 4, c % 4
        out[b, TOK * j:TOK * (j + 1)] = res.results[c]["yo"]
    return out


def kernel(**inputs):
    inp = {k: np.asarray(v, dtype=np.float32) for k, v in inputs.items()}
    return run_F(inp)
```

```python
import math
import contextlib
import numpy as np
import ml_dtypes
import concourse.bass as bass
import concourse.mybir as mybir
from concourse.bass_utils import run_bass_kernel_spmd

F32 = mybir.dt.float32
BF16 = mybir.dt.bfloat16
AF = mybir.ActivationFunctionType
ALU = mybir.AluOpType
AX = mybir.AxisListType

D = 1024
S = 8192
B = 2
NCORE = 8
TOK = 2048
NT = TOK // 128
H = 8
DE = 512
NE = 16
EPS = 1e-6
LAM_INIT0 = 0.8 - 0.6 * math.exp(-0.3 * 0)

ENGS = ("pe", "act", "dve", "pool", "sp")
EPOCH = 12000
NDMASEM = 10


class _Op:
    __slots__ = ("eng", "fn", "waits", "is_dma", "seq", "sem_slot", "is_cc")


class Prog:
    def __init__(self, nc):
        self.nc = nc
        self.ops = {e: [] for e in ENGS}
        self.cnt = {e: 0 for e in ENGS}
        self.last_w = {}
        self.readers = {}
        self.waited = {e: {} for e in ENGS}
        self.dma_rr = {e: 0 for e in ENGS}
        self.dma_cnt = {}
        self.dma_last = {}
        self.semkeys = set()
        self.pending_fence = {e: [] for e in ENGS}

    def _event(self, op):
        if op.is_dma:
            return (("dma", op.eng, op.sem_slot), op.seq)
        return (("eng", op.eng, (op.seq - 1) // EPOCH), ((op.seq - 1) % EPOCH) + 1)

    def _add_wait(self, op, dep):
        if dep is None or dep is op:
            return
        if (not dep.is_dma) and dep.eng == "pe" and op.eng == "pe" and not op.is_dma:
            return
        key, val = self._event(dep)
        w = self.waited[op.eng]
        if w.get(key, 0) >= val:
            return
        w[key] = val
        op.waits.append((key, val))
        self.semkeys.add(key)

    def fence(self):
        deps = []
        for e in ENGS:
            for op in reversed(self.ops[e]):
                if not op.is_dma:
                    deps.append(op)
                    break
        deps.extend(self.dma_last.values())
        for e in ENGS:
            self.pending_fence[e] = list(deps)

    def _record(self, eng, fn, reads, writes, is_dma):
        op = _Op()
        op.eng = eng
        op.fn = fn
        op.waits = []
        op.is_dma = is_dma
        op.is_cc = False
        if is_dma == "cc":
            op.is_dma = True
            op.is_cc = True
            self.ncc = getattr(self, "ncc", 0) + 1
            op.sem_slot = f"cc{self.ncc}"
            op.seq = 1
            self.semkeys.add(("dma", eng, op.sem_slot))
            self.dma_last[(eng, op.sem_slot)] = op
        elif is_dma:
            slot = self.dma_rr[eng] % NDMASEM
            self.dma_rr[eng] += 1
            k = (eng, slot)
            prev = self.dma_last.get(k)
            self.dma_cnt[k] = self.dma_cnt.get(k, 0) + 1
            op.sem_slot = slot
            op.seq = 16 * self.dma_cnt[k]
            self.semkeys.add(("dma", eng, slot))
            if prev is not None:
                self._add_wait(op, prev)
            self.dma_last[k] = op
        else:
            self.cnt[eng] += 1
            op.seq = self.cnt[eng]
            op.sem_slot = None
            self.semkeys.add(("eng", eng, (op.seq - 1) // EPOCH))
        if self.pending_fence[eng]:
            for d in self.pending_fence[eng]:
                self._add_wait(op, d)
            self.pending_fence[eng] = []
        for r in reads:
            self._add_wait(op, self.last_w.get(r))
        for w in writes:
            self._add_wait(op, self.last_w.get(w))
            for rd in self.readers.get(w, ()):
                self._add_wait(op, rd)
        for r in reads:
            self.readers.setdefault(r, []).append(op)
        for w in writes:
            self.last_w[w] = op
            self.readers[w] = []
        self.ops[eng].append(op)
        return op

    def op(self, eng, fn, reads=(), writes=()):
        return self._record(eng, fn, reads, writes, False)

    def dma(self, eng, fn, reads=(), writes=()):
        return self._record(eng, fn, reads, writes, True)

    def cc(self, kind, rg, src, dst, reads=(), writes=()):
        return self._record("pool", lambda e: e.collective_compute(kind, ALU.bypass, replica_groups=rg,
                                                                    ins=[src], outs=[dst]), reads, writes, "cc")

    def mm(self, out, lhsT, rhs, start=True, stop=True, reads=(), writes=(), **kw):
        return self.op("pe", lambda e: e.matmul(out, lhsT, rhs, start=start, stop=stop, **kw), reads, writes)

    def tr(self, out, in_, ident, reads=(), writes=()):
        return self.op("pe", lambda e: e.transpose(out, in_, ident), reads, writes)

    def act(self, out, in_, func, reads=(), writes=(), **kw):
        o = self.op("act", lambda e: e.activation(out, in_, func, **kw), reads, writes)
        acc = kw.get("accum_out")
        if acc is not None and getattr(self, "act_dummy", None) is not None:
            dm = self.act_dummy
            o = self.op("act", lambda e: e.copy(dm, acc), (), writes)
        return o

    def tt(self, eng, out, a, b, op, reads=(), writes=()):
        return self.op(eng, lambda e: e.tensor_tensor(out, a, b, op), reads, writes)

    def ts(self, eng, out, a, s1, s2, op0, op1=None, reads=(), writes=()):
        if op1 is None:
            return self.op(eng, lambda e: e.tensor_scalar(out, a, s1, None, op0), reads, writes)
        return self.op(eng, lambda e: e.tensor_scalar(out, a, s1, s2, op0, op1), reads, writes)

    def stt(self, eng, out, a, s, b, op0, op1, reads=(), writes=()):
        return self.op(eng, lambda e: e.scalar_tensor_tensor(out, a, s, b, op0, op1), reads, writes)

    def cp(self, eng, out, in_, reads=(), writes=()):
        if eng == "act":
            return self.op(eng, lambda e: e.copy(out, in_), reads, writes)
        return self.op(eng, lambda e: e.tensor_copy(out, in_), reads, writes)

    def memset(self, eng, ap, val, writes=()):
        return self.op(eng, lambda e: e.memset(ap, val), (), writes)

    def load(self, out, in_, reads=(), writes=(), eng="sp"):
        return self.dma(eng, lambda e: e.dma_start(out=out, in_=in_), reads, writes)

    def emit(self):
        nc = self.nc
        with contextlib.ExitStack() as st:
            sems = {}
            for key in sorted(self.semkeys, key=str):
                nm = "s_" + "_".join(str(k) for k in key)
                sems[key] = st.enter_context(nc.semaphore(nm))
            finals = []
            for e in ENGS:
                for op in reversed(self.ops[e]):
                    if not op.is_dma:
                        finals.append(self._event(op))
                        break
            for op in self.dma_last.values():
                finals.append(self._event(op))
            block = st.enter_context(nc.Block())
            engmap = {"pe": block.tensor, "act": block.scalar, "dve": block.vector,
                      "pool": block.gpsimd, "sp": block.sync}

            def make(ename):
                ops = self.ops[ename]

                def body(eng):
                    for op in ops:
                        for key, val in op.waits:
                            eng.wait_ge(sems[key], val)
                        ins = op.fn(eng)
                        key, val = self._event(op)
                        if op.is_cc:
                            ins.then_inc(sems[key])
                        else:
                            ins.then_inc(sems[key], 16 if op.is_dma else 1)
                    if ename == "sp":
                        for key, val in finals:
                            eng.wait_ge(sems[key], val)
                return body

            for e in ENGS:
                engmap[e](make(e))


class Arena:
    def __init__(self, nc, st, words=51000):
        self.t = st.enter_context(nc.sbuf_tensor("arena", [128, words], F32))
        self.words = words
        self.top = 0

    def alloc(self, shape, dt):
        n = int(np.prod(shape))
        w = n if dt == F32 else (n + 1) // 2
        w = (w + 7) // 8 * 8
        a = self.top
        self.top += w
        assert self.top <= self.words, ("SBUF arena overflow", self.top, self.words)
        self.last = a
        return self.view(a, shape, dt)

    def view(self, a, shape, dt):
        n = int(np.prod(shape))
        w = n if dt == F32 else (n + 1) // 2
        w = (w + 7) // 8 * 8
        ap = self.t[:, a:a + w]
        if dt == BF16:
            ap = ap.bitcast(BF16)
        ap = ap[:, 0:n]
        if len(shape) > 1:
            names = [f"d{i}" for i in range(len(shape))]
            kw = {names[i]: int(shape[i]) for i in range(1, len(shape))}
            ap = ap.rearrange(f"p ({' '.join(names)}) -> p {' '.join(names)}", **kw)
        return ap

    def mark(self):
        return self.top

    def release(self, m):
        self.top = m


class Ctx:
    pass


def bc_mid(ap, n):
    return ap[:, :, None].broadcast_to([128, ap.shape[1], n])


def setup_common(cx, ident_d, identf_d):
    P, A = cx.P, cx.A
    cx.ident = A.alloc([128], BF16)
    cx.identf = A.alloc([128], F32)
    P.act_dummy = A.alloc([1], F32)
    P.load(cx.ident, ident_d, writes=["ident"])
    P.load(cx.identf, identf_d, writes=["identf"])


def diag_extract(cx, dst, src_bc, rkeys, wkey):
    P = cx.P
    tmp = cx.diag_tmp
    P.tt("dve", tmp, src_bc.rearrange("p (a b) -> p a b", b=128),
         cx.identf[:, None, :].broadcast_to([128, 8, 128]), ALU.mult,
         reads=list(rkeys) + ["identf"], writes=["diag_tmp"])
    P.op("dve", lambda e: e.tensor_reduce(dst, tmp, AX.X, ALU.add), reads=["diag_tmp"], writes=[wkey])


def stage_mod(cx, c_d, adaw_d, adab_d, nmix_d, nffn_d, tag):
    P, A, ps = cx.P, cx.A, cx.ps
    if getattr(cx, "A1", None) is not None:
        A1, B1, A2, B2, g1bc, g2bc = cx.A1, cx.B1, cx.A2, cx.B2, cx.g1bc, cx.g2bc
    else:
        A1 = A.alloc([8], F32); B1 = A.alloc([8], F32); A2 = A.alloc([8], F32); B2 = A.alloc([8], F32)
        g1bc = A.alloc([D], F32); g2bc = A.alloc([D], F32)
    m = A.mark()
    csb = A.alloc([8], F32)
    cond = A.alloc([8], F32)
    condbc = A.alloc([8, 128], F32)
    modbc = A.alloc([6 * D], F32)
    adab = A.alloc([6 * D], F32)
    nm = A.alloc([D], F32)
    nf = A.alloc([D], F32)
    cx.diag_tmp = A.alloc([8, 128], F32)
    wbuf = [A.alloc([8, 512], F32) for _ in range(2)]
    t = tag
    P.load(csb, c_d, writes=[t + "csb"])
    P.load(adab, adab_d.partition_broadcast(128), writes=[t + "adab"])
    P.load(nm, nmix_d.partition_broadcast(128), writes=[t + "nm"])
    P.load(nf, nffn_d.partition_broadcast(128), writes=[t + "nf"])
    P.act(cond, csb, AF.Silu, reads=[t + "csb"], writes=[t + "cond"])
    P.cp("dve", condbc, bc_mid(cond, 128), reads=[t + "cond"], writes=[t + "condbc"])
    wv = adaw_d.rearrange("(kc p) n -> p kc n", p=128)
    for nch in range(12):
        wb = wbuf[nch % 2]
        wk = f"{t}adaw{nch % 2}"
        P.load(wb, wv[:, :, nch * 512:(nch + 1) * 512], writes=[wk])
        bank = 6 + nch % 2
        for kc in range(8):
            P.mm(ps[:, bank, :], condbc[:, kc, :], wb[:, kc, :],
                 start=(kc == 0), stop=(kc == 7), reads=[wk, t + "condbc"], writes=[f"ps{bank}"])
        sl = slice(nch * 512, (nch + 1) * 512)
        P.tt("dve", modbc[:, sl], ps[:, bank, :], adab[:, sl], ALU.add,
             reads=[f"ps{bank}", t + "adab"], writes=[t + "modbc"])
    sh1, sc1, g1, sh2, sc2, g2 = [modbc[:, i * D:(i + 1) * D] for i in range(6)]
    tmpA = adab[:, 0:D]
    P.stt("dve", tmpA, sc1, 1.0, nm, ALU.add, ALU.mult, reads=[t + "modbc", t + "nm"], writes=[t + "adab"])
    diag_extract(cx, A1, tmpA, [t + "adab"], t + "A1")
    diag_extract(cx, B1, sh1, [t + "modbc"], t + "B1")
    tmpA2 = adab[:, D:2 * D]
    P.stt("dve", tmpA2, sc2, 1.0, nf, ALU.add, ALU.mult, reads=[t + "modbc", t + "nf"], writes=[t + "adab2"])
    diag_extract(cx, A2, tmpA2, [t + "adab2"], t + "A2")
    diag_extract(cx, B2, sh2, [t + "modbc"], t + "B2")
    P.cp("dve", g1bc, g1, reads=[t + "modbc"], writes=[t + "g1bc"])
    P.cp("dve", g2bc, g2, reads=[t + "modbc"], writes=[t + "g2bc"])
    P.fence()
    A.release(m)
    cx.A1, cx.B1, cx.A2, cx.B2, cx.g1bc, cx.g2bc = A1, B1, A2, B2, g1bc, g2bc
    cx.modtag = t


def norm_scratch(cx):
    A = cx.A
    cx.n_junk = A.alloc([D], BF16)
    cx.n_xn = [A.alloc([D], BF16) for _ in range(4)]
    cx.n_st = [A.alloc([4, 4], F32) for _ in range(2)]
    cx.n_tmp = [A.alloc([8, 128], BF16) for _ in range(2)]
    cx.n_i = 0
    cx.n_g = 0


def norm_T_multi(cx, xts, xkeys, Avec, Bvec, vkeys, dsts, dkeys):
    P, ps = cx.P, cx.ps
    n = len(xts)
    g = cx.n_g % 2
    cx.n_g += 1
    st = cx.n_st[g]
    sk = f"nst{g}"
    for j in range(n):
        P.act(cx.n_junk, xts[j], AF.Square, reads=[xkeys[j]], writes=["njunk", sk], accum_out=st[:, 0, j:j + 1])
    P.ts("dve", st[:, 1, 0:n], st[:, 0, 0:n], 1.0 / D, EPS, ALU.mult, ALU.add, reads=[sk], writes=[sk])
    P.act(st[:, 2, 0:n], st[:, 1, 0:n], AF.Sqrt, reads=[sk], writes=[sk])
    P.op("dve", lambda e: e.reciprocal(st[:, 3, 0:n], st[:, 2, 0:n]), reads=[sk], writes=[sk])
    for j in range(n):
        xn = cx.n_xn[j]
        xk = f"nxn{j}"
        P.act(xn, xts[j], AF.Copy, reads=[xkeys[j], sk], writes=[xk], scale=st[:, 3, j:j + 1])
        i = cx.n_i % 2
        cx.n_i += 1
        tmp = cx.n_tmp[i]
        tk = f"ntmp{i}"
        bank = 6 + i
        pst = ps[:, bank, :].bitcast(BF16).rearrange("p (a b) -> p a b", b=128)
        for kc in range(8):
            P.tr(pst[:, kc, :], xn[:, kc * 128:(kc + 1) * 128], cx.ident, reads=[xk, "ident"], writes=[f"ps{bank}"])
        P.tt("dve", tmp, pst, bc_mid(Avec, 128), ALU.mult, reads=[f"ps{bank}"] + list(vkeys), writes=[tk])
        P.tt("pool", dsts[j], tmp, bc_mid(Bvec, 128), ALU.add, reads=[tk] + list(vkeys), writes=[dkeys[j]])


def norm_T(cx, xt, xkey, Avec, Bvec, vkeys, dst, dkey):
    norm_T_multi(cx, [xt], [xkey], Avec, Bvec, vkeys, [dst], [dkey])


def transpose_tok(cx, src, skey, dst, dkey, eng="dve"):
    P, ps = cx.P, cx.ps
    i = cx.n_i % 2
    cx.n_i += 1
    bank = 6 + i
    pst = ps[:, bank, :].bitcast(BF16).rearrange("p (a b) -> p a b", b=128)
    for kc in range(8):
        P.tr(pst[:, kc, :], src[:, kc * 128:(kc + 1) * 128], cx.ident, reads=[skey, "ident"], writes=[f"ps{bank}"])
    P.cp(eng, dst, pst, reads=[f"ps{bank}"], writes=[dkey])


def proj_residual(cx, srcT, skeys, wp, wkey):
    P, ps = cx.P, cx.ps
    it = 0
    for t in range(NT):
        for half in range(2):
            bank = 4 + (it % 2)
            it += 1
            for kc in range(8):
                P.mm(ps[:, bank, :], srcT[:, kc, t * 128:(t + 1) * 128], wp[:, kc, half * 512:(half + 1) * 512],
                     start=(kc == 0), stop=(kc == 7), reads=list(skeys) + [wkey], writes=[f"ps{bank}"])
            xs = cx.xres[:, t, half * 512:(half + 1) * 512]
            P.tt("dve", xs, ps[:, bank, :], xs, ALU.add, reads=[f"ps{bank}"], writes=[f"xres{t}"])


def stage_moe(cx, wr_d, br_d, wg_d, wu_d, wd_d, tag):
    P, A, ps = cx.P, cx.A, cx.ps
    t_ = tag
    m = A.mark()
    h2T = A.alloc([8, TOK], BF16)
    norm_scratch(cx)
    vk = [cx.modtag + "A2", cx.modtag + "B2"]
    for t0 in range(0, NT, 4):
        ts4 = list(range(t0, t0 + 4))
        norm_T_multi(cx, [cx.xres[:, t, :] for t in ts4], [f"xres{t}" for t in ts4], cx.A2, cx.B2, vk,
                     [h2T[:, :, t * 128:(t + 1) * 128] for t in ts4], [f"{t_}h2T{t}" for t in ts4])
    h2keys = [f"{t_}h2T{t}" for t in range(NT)]
    wr = A.alloc([8, 20], BF16)
    brbc = A.alloc([20], F32)
    P.load(wr, wr_d, writes=[t_ + "wr"], eng="pool")
    P.load(brbc, br_d.partition_broadcast(128), writes=[t_ + "br"])
    L = A.alloc([NT, 20], F32)
    rbank = 5
    Lps = ps[:, rbank, :].rearrange("p (a b) -> p a b", b=32)[:, :, 0:20]
    for t in range(NT):
        for kc in range(8):
            P.mm(Lps[:, t, :], h2T[:, kc, t * 128:(t + 1) * 128], wr[:, kc, :], start=(kc == 0), stop=(kc == 7),
                 reads=[f"{t_}h2T{t}", t_ + "wr"], writes=[f"ps{rbank}"])
    P.tt("dve", L, Lps, brbc[:, None, :].broadcast_to([128, NT, 20]), ALU.add,
         reads=[f"ps{rbank}", t_ + "br"], writes=[t_ + "L"])
    Lg = L[:, :, 0:4]
    Le = L[:, :, 4:20].rearrange("p t (g e) -> p t g e", e=4)
    gmax = A.alloc([NT], F32); gsum = A.alloc([NT], F32); gw = A.alloc([NT], F32)
    ohg = A.alloc([NT, 4], F32); eg = A.alloc([NT, 4], F32)
    tmp44 = A.alloc([NT, 4, 4], F32)
    esel = A.alloc([NT, 4], F32); e2 = A.alloc([NT, 4], F32)
    m1 = A.alloc([NT], F32); m2 = A.alloc([NT], F32)
    mk1 = A.alloc([NT, 4], F32); mk2 = A.alloc([NT, 4], F32)
    dd = A.alloc([NT], F32); w1 = A.alloc([NT], F32); w2 = A.alloc([NT], F32)
    ew = A.alloc([NT, 4], F32)
    gates = A.alloc([NT, 4, 4], F32)
    rk = t_ + "rt"

    def bcl(ap, n):
        return ap[:, :, None].broadcast_to([128, NT, n])

    P.op("dve", lambda e: e.tensor_reduce(gmax, Lg, AX.X, ALU.max), reads=[t_ + "L"], writes=[rk])
    P.tt("dve", ohg, Lg, bcl(gmax, 4), ALU.is_equal, reads=[rk, t_ + "L"], writes=[rk])
    P.tt("dve", eg, Lg, bcl(gmax, 4), ALU.subtract, reads=[rk, t_ + "L"], writes=[rk])
    P.act(eg, eg, AF.Exp, reads=[rk], writes=[rk])
    P.op("dve", lambda e: e.tensor_reduce(gsum, eg, AX.X, ALU.add), reads=[rk], writes=[rk])
    P.op("dve", lambda e: e.reciprocal(gw, gsum), reads=[rk], writes=[rk])
    P.tt("dve", tmp44, Le, ohg[:, :, :, None].broadcast_to([128, NT, 4, 4]), ALU.mult, reads=[rk, t_ + "L"], writes=[rk])
    P.op("dve", lambda e: e.tensor_reduce(esel, tmp44.rearrange("p t g e -> p t e g"), AX.X, ALU.add), reads=[rk], writes=[rk])
    P.op("dve", lambda e: e.tensor_reduce(m1, esel, AX.X, ALU.max), reads=[rk], writes=[rk])
    P.tt("dve", mk1, esel, bcl(m1, 4), ALU.is_equal, reads=[rk], writes=[rk])
    P.stt("dve", e2, mk1, -1e30, esel, ALU.mult, ALU.add, reads=[rk], writes=[rk])
    P.op("dve", lambda e: e.tensor_reduce(m2, e2, AX.X, ALU.max), reads=[rk], writes=[rk])
    P.tt("dve", mk2, e2, bcl(m2, 4), ALU.is_equal, reads=[rk], writes=[rk])
    P.tt("dve", dd, m2, m1, ALU.subtract, reads=[rk], writes=[rk])
    P.act(dd, dd, AF.Exp, reads=[rk], writes=[rk])
    P.ts("dve", w1, dd, 1.0, None, ALU.add, reads=[rk], writes=[rk])
    P.op("dve", lambda e: e.reciprocal(w1, w1), reads=[rk], writes=[rk])
    P.tt("dve", w2, dd, w1, ALU.mult, reads=[rk], writes=[rk])
    P.tt("dve", w1, w1, gw, ALU.mult, reads=[rk], writes=[rk])
    P.tt("dve", w2, w2, gw, ALU.mult, reads=[rk], writes=[rk])
    P.tt("dve", ew, mk1, bcl(w1, 4), ALU.mult, reads=[rk], writes=[rk])
    P.tt("dve", mk2, mk2, bcl(w2, 4), ALU.mult, reads=[rk], writes=[rk])
    P.tt("dve", ew, ew, mk2, ALU.add, reads=[rk], writes=[rk])
    P.tt("dve", gates, ohg[:, :, :, None].broadcast_to([128, NT, 4, 4]),
         ew[:, :, None, :].broadcast_to([128, NT, 4, 4]), ALU.mult, reads=[rk], writes=[t_ + "gates"])
    gflat = gates.rearrange("p t g e -> p t (g e)")
    if "L" in DBG and t_ == "e0":
        P.load(DBG["L"].rearrange("(t p) d -> p t d", p=128), L, reads=[t_ + "L"], writes=["dbg_L"])
        P.load(DBG["gates"].rearrange("(t p) d -> p t d", p=128), gflat, reads=[t_ + "gates"], writes=["dbg_g"])
        P.load(DBG["xmid"].rearrange("(t p) d -> p t d", p=128), cx.xres, reads=[f"xres{t}" for t in range(NT)], writes=["dbg_x"])
    wgb = [A.alloc([8, DE], BF16) for _ in range(2)]
    wub = [A.alloc([8, DE], BF16) for _ in range(2)]
    wdb = [A.alloc([4, D], BF16) for _ in range(2)]
    sil = [A.alloc([512], BF16) for _ in range(2)]
    hid = [A.alloc([4, 512], BF16) for _ in range(2)]
    it_gu = 0
    it_y = 0
    it_h = 0
    for e in range(NE):
        bi = e % 2
        kg, ku, kd = f"{t_}wg{bi}", f"{t_}wu{bi}", f"{t_}wd{bi}"
        P.load(wgb[bi], wg_d[e].rearrange("(kc p) n -> p kc n", p=128), writes=[kg], eng="pool")
        P.load(wub[bi], wu_d[e].rearrange("(kc p) n -> p kc n", p=128), writes=[ku], eng="pool")
        P.load(wdb[bi], wd_d[e].rearrange("(kc p) n -> p kc n", p=128), writes=[kd], eng="pool")
        P.tt("pool", wdb[bi], wdb[bi], cx.g2bc[:, None, :].broadcast_to([128, 4, D]), ALU.mult,
             reads=[kd, cx.modtag + "g2bc"], writes=[kd])
        for c in range(4):
            hb = it_h % 2
            it_h += 1
            hk = f"{t_}hid{hb}"
            ckeys = [f"{t_}h2T{t}" for t in range(4 * c, 4 * c + 4)]
            for fc in range(4):
                gb = (it_gu % 2) * 2
                it_gu += 1
                for kc in range(8):
                    P.mm(ps[:, gb, :], wgb[bi][:, kc, fc * 128:(fc + 1) * 128], h2T[:, kc, c * 512:(c + 1) * 512],
                         start=(kc == 0), stop=(kc == 7), reads=[kg] + ckeys, writes=[f"ps{gb}"])
                for kc in range(8):
                    P.mm(ps[:, gb + 1, :], wub[bi][:, kc, fc * 128:(fc + 1) * 128], h2T[:, kc, c * 512:(c + 1) * 512],
                         start=(kc == 0), stop=(kc == 7), reads=[ku] + ckeys, writes=[f"ps{gb + 1}"])
                sb_ = sil[it_gu % 2]
                sk = f"{t_}sil{it_gu % 2}"
                P.act(sb_, ps[:, gb, :], AF.Silu, reads=[f"ps{gb}"], writes=[sk])
                P.tt("dve", hid[hb][:, fc, :], ps[:, gb + 1, :], sb_, ALU.mult, reads=[f"ps{gb + 1}", sk], writes=[hk])
            for ts_ in range(4):
                t = 4 * c + ts_
                for half in range(2):
                    yb = 4 + (it_y % 4)
                    it_y += 1
                    for fc in range(4):
                        P.mm(ps[:, yb, :], hid[hb][:, fc, ts_ * 128:(ts_ + 1) * 128], wdb[bi][:, fc, half * 512:(half + 1) * 512],
                             start=(fc == 0), stop=(fc == 3), reads=[hk, kd], writes=[f"ps{yb}"])
                    xs = cx.xres[:, t, half * 512:(half + 1) * 512]
                    P.stt("dve", xs, ps[:, yb, :], gflat[:, t, e:e + 1], xs, ALU.mult, ALU.add,
                          reads=[f"ps{yb}", t_ + "gates"], writes=[f"xres{t}"])
    P.fence()
    A.release(m)


def layer0_body(cx, xp, w_in, w_out, lam, subln, kaug, qaug, dfix, kscr, qscr, vscr, wr, br, wg, wu, wd):
    P, A, ps = cx.P, cx.A, cx.ps
    mt = cx.modtag
    m_qkv = A.mark()
    win = A.alloc([8, 3 * D], BF16)
    wv = w_in.rearrange("(kc p) n -> p kc n", p=128)
    for i in range(3):
        P.load(win[:, :, i * D:(i + 1) * D], wv[:, :, i * D:(i + 1) * D], writes=[f"win{i}"], eng="pool")
    norm_scratch(cx)
    xt = [A.alloc([D], F32) for _ in range(6)]
    hTc = [A.alloc([8, 512], BF16) for _ in range(2)]
    stgp = [A.alloc([2, H, 512], BF16) for _ in range(2)]
    istg = 0
    vst = [A.alloc([4, H, 129], BF16) for _ in range(2)]
    for i in range(2):
        P.memset("pool", vst[i][:, :, :, 128:129], 1.0, writes=[f"vst{i}"])
    xv = xp.rearrange("(t p) d -> t p d", p=128)
    kscr_v = kscr.rearrange("s h d t -> d s h t")
    qscr_v = qscr.rearrange("s h d t -> d s h t")
    vscr_v = vscr.rearrange("t p h c -> p t h c")
    vk1 = [mt + "A1", mt + "B1"]
    ixt = 0
    ipb = 0
    qkv_state = {"ixt": 0, "istg": 0, "ipb": 0}

    def emit_norm(ch):
        ixt = qkv_state["ixt"]
        cb = ch % 2
        hk = f"hTc{cb}"
        xbs, xks = [], []
        for t in range(4):
            xb = xt[ixt % 6]
            xk = f"xt{ixt % 6}"
            ixt += 1
            P.load(xb, xv[ch * 4 + t], writes=[xk])
            xbs.append(xb)
            xks.append(xk)
        norm_T_multi(cx, xbs, xks, cx.A1, cx.B1, vk1,
                     [hTc[cb][:, :, t * 128:(t + 1) * 128] for t in range(4)], [hk] * 4)
        qkv_state["ixt"] = ixt

    def emit_mm(ch):
        cb = ch % 2
        hk = f"hTc{cb}"
        istg = qkv_state["istg"]
        ipb = qkv_state["ipb"]
        for which in ([1, 0] if ch < 4 else [1]):
            stg = stgp[istg % 2]
            sk = f"stg{istg % 2}"
            istg += 1
            for hb in range(H):
                bank = ipb % 4
                ipb += 1
                col = which * D + hb * 128
                for kc in range(8):
                    P.mm(ps[:, bank, :], win[:, kc, col:col + 128], hTc[cb][:, kc, :], start=(kc == 0), stop=(kc == 7),
                         reads=[f"win{which}", hk], writes=[f"ps{bank}"])
                if which == 1:
                    P.cp("act", stg[0:64, 0, hb, :], ps[0:64, bank, :], reads=[f"ps{bank}"], writes=[sk])
                    P.cp("dve", stg[0:64, 1, hb, :], ps[64:128, bank, :], reads=[f"ps{bank}"], writes=[sk])
                else:
                    P.op("act", lambda e, o=stg[0:64, 0, hb, :], i_=ps[0:64, bank, :]: e.mul(o, i_, 0.125),
                         reads=[f"ps{bank}"], writes=[sk])
                    P.ts("dve", stg[0:64, 1, hb, :], ps[64:128, bank, :], 0.125, None, ALU.mult,
                         reads=[f"ps{bank}"], writes=[sk])
            if which == 1:
                P.load(kscr_v[:, :, :, ch * 512:(ch + 1) * 512], stg[0:64], reads=[sk], writes=[f"kscr{ch}"])
            else:
                P.load(qscr_v[:, :, :, ch * 512:(ch + 1) * 512], stg[0:64], reads=[sk], writes=[f"qscr{ch}"])
        for t in range(4):
            for half in range(2):
                bank = ipb % 4
                ipb += 1
                for kc in range(8):
                    P.mm(ps[:, bank, :], hTc[cb][:, kc, t * 128:(t + 1) * 128],
                         win[:, kc, 2 * D + half * 512:2 * D + (half + 1) * 512], start=(kc == 0), stop=(kc == 7),
                         reads=["win2", hk], writes=[f"ps{bank}"])
                eng = "act" if half == 0 else "dve"
                P.cp(eng, vst[cb][:, t, half * 4:(half + 1) * 4, 0:128],
                     ps[:, bank, :].rearrange("p (h c) -> p h c", c=128), reads=[f"ps{bank}"], writes=[f"vst{cb}"])
        P.load(vscr_v[:, ch * 4:(ch + 1) * 4], vst[cb], reads=[f"vst{cb}"], writes=[f"vscr{ch}"])

        qkv_state["istg"] = istg
        qkv_state["ipb"] = ipb

    emit_norm(0)
    for ch in range(S // 512):
        if ch + 1 < S // 512:
            emit_norm(ch + 1)
        emit_mm(ch)
    P.fence()
    A.release(m_qkv)
    cx.xres = A.alloc([NT, D], F32)
    xres_off = A.last
    m_att = A.mark()
    obf = A.alloc([NT, D], BF16)
    ssq = A.alloc([NT, H], F32)
    lamv = A.alloc([4, 64], F32)
    lamt = A.alloc([2, 64], F32)
    lams = A.alloc([4], F32)
    neglam = A.alloc([1], F32)
    dfx = A.alloc([H, 128], BF16)
    P.load(dfx, dfix, writes=["dfx"])
    P.load(lamv, lam.rearrange("a b -> (a b)").partition_broadcast(128).rearrange("p (a b) -> p a b", b=64), writes=["lamv"])
    P.tt("dve", lamt, lamv[:, 0:4:2, :], lamv[:, 1:4:2, :], ALU.mult, reads=["lamv"], writes=["lamt"])
    P.op("dve", lambda e: e.tensor_reduce(lams[:, 0:2], lamt, AX.X, ALU.add), reads=["lamt"], writes=["lams"])
    P.act(lams[:, 2:4], lams[:, 0:2], AF.Exp, reads=["lams"], writes=["lams"])
    P.tt("dve", neglam, lams[:, 3:4], lams[:, 2:3], ALU.subtract, reads=["lams"], writes=["neglam"])
    P.ts("dve", neglam, neglam, -LAM_INIT0, None, ALU.add, reads=["neglam"], writes=["neglam"])
    ktb = [A.alloc([2, S], BF16)]
    kt_off = A.last
    ktb.append(A.view(xres_off, [2, S], BF16))
    qtb = [A.alloc([2, TOK], BF16) for _ in range(2)]
    qt_off = A.last - (2 * TOK) // 2
    vtb = A.alloc([S // 128, 2, 129], BF16)
    pT = [A.alloc([1024], BF16) for _ in range(3)]
    o32 = [A.alloc([128], F32) for _ in range(4)]
    t32 = [A.alloc([128], F32) for _ in range(1)]
    rr = [A.alloc([4], F32) for _ in range(4)]
    junk = A.alloc([128], BF16)
    vscr_hp = vscr.rearrange("t p (hp hh) c -> p t hp hh c", hh=2)
    NKT = S // 128
    state = {"isb": 0, "ipt": 0, "iep": 0}

    def load_head(h):
        kb = h % 2
        P.load(ktb[kb][0:64], kscr_v[:, :, h, :], writes=[f"ktb{kb}"])
        P.load(ktb[kb][64:72], kaug[h], writes=[f"ktb{kb}"])
        P.load(qtb[kb][0:64], qscr_v[:, :, h, :], writes=[f"qtb{kb}"])
        P.load(qtb[kb][64:72], qaug, writes=[f"qtb{kb}"])

    def load_v(hp):
        for q4 in range(4):
            P.load(vtb[:, q4 * 16:(q4 + 1) * 16], vscr_hp[:, q4 * 16:(q4 + 1) * 16, hp], writes=["vtb"])

    def emit_qk_exp(h, qc, kt):
        kb = h % 2
        kk, qk = f"ktb{kb}", f"qtb{kb}"
        K_, Q_ = ktb[kb], qtb[kb]
        sb0 = 2 * (state["isb"] % 2)
        state["isb"] += 1
        for s_ in range(2):
            bank = sb0 + s_
            outp = ps[:, bank, :]
            kcols = slice(kt * 128, (kt + 1) * 128)
            q0 = qc * 512
            rd = [kk, qk]
            wkey = [f"ps{bank}"]
            if kt >= NT or kt > 4 * qc + 3:
                P.mm(outp, K_[0:72, s_, kcols], Q_[0:72, s_, q0:q0 + 512], reads=rd, writes=wkey)
            elif kt < 4 * qc:
                P.mm(outp, K_[0:68, s_, kcols], Q_[0:68, s_, q0:q0 + 512], reads=rd, writes=wkey)
            else:
                i_ = kt - 4 * qc
                if i_ > 0:
                    P.mm(outp[:, 0:i_ * 128], K_[0:72, s_, kcols], Q_[0:72, s_, q0:q0 + i_ * 128], reads=rd, writes=wkey)
                dsl = slice(i_ * 128, (i_ + 1) * 128)
                P.mm(outp[:, dsl], K_[0:68, s_, kcols], Q_[0:68, s_, q0 + i_ * 128:q0 + (i_ + 1) * 128],
                     start=True, stop=False, reads=rd, writes=wkey, skip_group_check=True)
                P.mm(outp[:, dsl], cx.ident, dfx[:, h, :], start=False, stop=True,
                     reads=["ident", "dfx"], writes=wkey, skip_group_check=True)
                if i_ < 3:
                    P.mm(outp[:, (i_ + 1) * 128:512], K_[0:68, s_, kcols], Q_[0:68, s_, q0 + (i_ + 1) * 128:q0 + 512],
                         reads=rd, writes=wkey, skip_group_check=True)
        pi_ = state["ipt"] % 3
        state["ipt"] += 1
        P.act(pT[pi_], ps[:, sb0:sb0 + 2, :].rearrange("p a b -> p (a b)"), AF.Exp,
              reads=[f"ps{sb0}", f"ps{sb0 + 1}"], writes=[f"pT{pi_}"])
        return pi_

    def emit_pv(h, qc, kt, pi_):
        hh = h % 2
        pb = pT[pi_]
        for s_ in range(2):
            for qs in range(4):
                a = s_ * 4 + qs
                bank = 4 + a // 3
                off = (a % 3) * 132
                P.mm(ps[:, bank, off:off + 129], pb[:, s_ * 512 + qs * 128:s_ * 512 + (qs + 1) * 128],
                     vtb[:, kt, hh, :], start=(kt == 0 and a % 3 == 0), stop=(kt == NKT - 1),
                     reads=[f"pT{pi_}", "vtb"], writes=[f"oacc{a}"], skip_group_check=True)
        if kt == NKT - 1:
            for qs in range(4):
                t = 4 * qc + qs
                eb = state["iep"] % 4
                state["iep"] += 1
                a1, a2 = qs, 4 + qs
                acc1 = ps[:, 4 + a1 // 3, (a1 % 3) * 132:(a1 % 3) * 132 + 129]
                acc2 = ps[:, 4 + a2 // 3, (a2 % 3) * 132:(a2 % 3) * 132 + 129]
                rk = f"rr{eb}"
                g1 = [f"oacc{i}" for i in range(8) if i // 3 == a1 // 3]
                g2 = [f"oacc{i}" for i in range(8) if i // 3 == a2 // 3]
                P.op("dve", lambda e, o=rr[eb][:, 0:1], i_=acc1[:, 128:129]: e.reciprocal(o, i_), reads=g1, writes=[rk])
                P.op("dve", lambda e, o=rr[eb][:, 1:2], i_=acc2[:, 128:129]: e.reciprocal(o, i_), reads=g2, writes=[rk])
                P.tt("dve", rr[eb][:, 2:3], rr[eb][:, 1:2], neglam, ALU.mult, reads=[rk, "neglam"], writes=[rk])
                P.ts("dve", t32[0], acc2[:, 0:128], rr[eb][:, 2:3], None, ALU.mult, reads=g2 + [rk], writes=["t320"])
                P.stt("dve", o32[eb], acc1[:, 0:128], rr[eb][:, 0:1], t32[0], ALU.mult, ALU.add,
                      reads=g1 + [rk, "t320"], writes=[f"o32{eb}"])
                P.tt("dve", t32[0], o32[eb], o32[eb], ALU.mult, reads=[f"o32{eb}"], writes=["t320"])
                P.op("dve", lambda e, o=ssq[:, t, h:h + 1], i_=t32[0]: e.tensor_reduce(o, i_, AX.X, ALU.add),
                     reads=["t320"], writes=["ssq"])
                P.cp("pool", obf[:, t, h * 128:(h + 1) * 128], o32[eb], reads=[f"o32{eb}"], writes=[f"obf{t}"])
            if qc == 3 and h % 2 == 1 and h + 1 < H:
                load_v((h + 1) // 2)

    load_v(0)
    pend = []
    load_head(0)
    for h in range(H):
        if h + 1 < H:
            load_head(h + 1)
        for qc in range(4):
            for kt in range(NKT):
                pi_ = emit_qk_exp(h, qc, kt)
                pend.append((h, qc, kt, pi_))
                if len(pend) > 1:
                    emit_pv(*pend.pop(0))
    while pend:
        emit_pv(*pend.pop(0))
    P.ts("dve", ssq, ssq, 1.0 / 128, EPS, ALU.mult, ALU.add, reads=["ssq"], writes=["ssq"])
    P.act(ssq, ssq, AF.Sqrt, reads=["ssq"], writes=["ssq"])
    P.op("dve", lambda e: e.reciprocal(ssq, ssq), reads=["ssq"], writes=["ssq"])
    for t in range(NT):
        ov = obf[:, t, :].rearrange("p (h c) -> p h c", c=128)
        P.tt("pool", ov, ov, ssq[:, t, :][:, :, None].broadcast_to([128, H, 128]), ALU.mult,
             reads=["ssq", f"obf{t}"], writes=[f"obf{t}"])
    if "obf" in DBG:
        P.load(DBG["obf"].rearrange("(t p) d -> p t d", p=128), obf, reads=[f"obf{t}" for t in range(NT)], writes=["dbg_obf"])
    P.fence()
    P.load(cx.xres, xp[0:TOK].rearrange("(t p) d -> p t d", p=128), writes=[f"xres{t}" for t in range(NT)])
    oT = A.view(kt_off, [8, TOK], BF16)
    wob = A.view(qt_off, [8, D], BF16)
    sub_s = A.alloc([1], F32)
    P.load(sub_s, subln, writes=["subs"])
    P.ts("dve", sub_s, sub_s, 1.0 - LAM_INIT0, None, ALU.mult, reads=["subs"], writes=["subs"])
    P.load(wob, w_out.rearrange("(kc p) n -> p kc n", p=128), writes=["wob"], eng="pool")
    for kc in range(8):
        P.ts("pool", wob[:, kc, :], wob[:, kc, :], sub_s[:, 0:1], None, ALU.mult,
             reads=["wob", "subs"], writes=["wob"])
        P.tt("pool", wob[:, kc, :], wob[:, kc, :], cx.g1bc, ALU.mult,
             reads=["wob", mt + "g1bc"], writes=["wob"])
    for t in range(NT):
        transpose_tok(cx, obf[:, t, :], f"obf{t}", oT[:, :, t * 128:(t + 1) * 128], f"oT{t}", eng=("dve" if t % 2 else "act"))
    proj_residual(cx, oT, [f"oT{t}" for t in range(NT)], wob, "wob")
    P.fence()
    A.release(m_att)
    stage_moe(cx, wr, br, wg, wu, wd, "e0")


def build_A():
    nc = bass.Bass("TRN2", target_bir_lowering=False)
    dram = lambda n, s, dt=F32, kind="ExternalInput": nc.dram_tensor(n, s, dt, kind=kind).ap()
    xp = dram("xp", [S, D])
    c_d = dram("c", [128, 8])
    adaw = dram("ada_w", [D, 6 * D]); adab = dram("ada_b", [6 * D])
    nmix = dram("norm_mix", [D]); nffn = dram("norm_ffn", [D])
    w_in = dram("w_in", [D, 3 * D]); w_out = dram("w_out", [D, D])
    lam = dram("lam", [4, 64]); subln = dram("subln", [128, 1])
    kaug = dram("kaug", [H, 8, 2, S], BF16); qaug = dram("qaug", [8, 2, TOK], BF16)
    dfix = dram("dfix", [128, H, 128], BF16)
    ident_d = dram("ident", [128, 128], BF16); identf_d = dram("identf", [128, 128])
    wr = dram("wr", [128, 8, 20]); br = dram("br", [20])
    wg = dram("wg", [NE, D, DE]); wu = dram("wu", [NE, D, DE]); wd = dram("wd", [NE, DE, D])
    xo = dram("xo", [TOK, D], F32, "ExternalOutput")
    if DBG.get("on"):
        DBG["obf"] = dram("d_obf", [TOK, D], BF16, "ExternalOutput")
        DBG["L"] = dram("d_L", [TOK, 20], F32, "ExternalOutput")
        DBG["gates"] = dram("d_gates", [TOK, 16], F32, "ExternalOutput")
        DBG["xmid"] = dram("d_xmid", [TOK, D], F32, "ExternalOutput")
    kscr = dram("kscr", [2, H, 64, S], BF16, "Internal")
    qscr = dram("qscr", [2, H, 64, TOK], BF16, "Internal")
    vscr = dram("vscr", [S // 128, 128, H, 129], BF16, "Internal")

    P = Prog(nc)
    with contextlib.ExitStack() as st:
        cx = Ctx()
        cx.P = P
        cx.A = A = Arena(nc, st)
        cx.ps = ps = st.enter_context(nc.psum_tensor("ps", [128, 8, 512], F32))
        setup_common(cx, ident_d, identf_d)
        stage_mod(cx, c_d, adaw, adab, nmix, nffn, "m0")
        mt = cx.modtag
        layer0_body(cx, xp, w_in, w_out, lam, subln, kaug, qaug, dfix, kscr, qscr, vscr, wr, br, wg, wu, wd)
        P.load(xo.rearrange("(t p) d -> p t d", p=128), cx.xres, reads=[f"xres{t}" for t in range(NT)], writes=["xo"])
        P.emit()
    return nc


def build_B():
    nc = bass.Bass("TRN2", target_bir_lowering=False)
    dram = lambda n, s, dt=F32, kind="ExternalInput": nc.dram_tensor(n, s, dt, kind=kind).ap()
    x1 = dram("x1", [S, D])
    c_d = dram("c", [128, 8])
    adaw = dram("ada_w", [D, 6 * D]); adab = dram("ada_b", [6 * D])
    nmix = dram("norm_mix", [D]); nffn = dram("norm_ffn", [D])
    fwin = dram("fwin", [D, 256])
    cs_d = dram("cs", [128, 2, 512], BF16)
    r1_d = dram("r1", [128, 256], BF16); r2_d = dram("r2", [128, 256], BF16)
    twr_d = dram("twr", [128, 128]); twi_d = dram("twi", [128, 128])
    bdc_d = dram("bdc", [128, 128], BF16); bds_d = dram("bds", [128, 128], BF16)
    ident_d = dram("ident", [128, 128], BF16); identf_d = dram("identf", [128, 128])
    fo = dram("fo", [S, 256], BF16, "ExternalOutput")
    P = Prog(nc)
    with contextlib.ExitStack() as st:
        cx = Ctx()
        cx.P = P
        cx.A = A = Arena(nc, st)
        cx.ps = ps = st.enter_context(nc.psum_tensor("ps", [128, 8, 512], F32))
        setup_common(cx, ident_d, identf_d)
        stage_mod(cx, c_d, adaw, adab, nmix, nffn, "m1")
        mt = cx.modtag
        cs = A.alloc([2, 512], BF16); r1 = A.alloc([256], BF16); r2 = A.alloc([256], BF16)
        twr = A.alloc([128], F32); twi = A.alloc([128], F32)
        bdc = A.alloc([128], BF16); bds = A.alloc([128], BF16)
        for ap_, d_, k_ in [(cs, cs_d, "cs"), (r1, r1_d, "r1"), (r2, r2_d, "r2"), (twr, twr_d, "twr"),
                            (twi, twi_d, "twi"), (bdc, bdc_d, "bdc"), (bds, bds_d, "bds")]:
            P.load(ap_, d_, writes=[k_])
        uT = A.alloc([2, S], BF16)
        m2 = A.mark()
        winb = A.alloc([8, 256], BF16)
        P.load(winb, fwin.rearrange("(kc p) n -> p kc n", p=128), writes=["winb"], eng="pool")
        norm_scratch(cx)
        xt = [A.alloc([D], F32) for _ in range(3)]
        hTc = [A.alloc([8, 512], BF16) for _ in range(2)]
        xv = x1.rearrange("(t p) d -> t p d", p=128)
        vk1 = [mt + "A1", mt + "B1"]
        ixt = 0
        ipb = 0
        for ch in range(S // 512):
            cb = ch % 2
            hk = f"hTc{cb}"
            for t in range(4):
                xb = xt[ixt % 3]
                xk = f"xt{ixt % 3}"
                ixt += 1
                P.load(xb, xv[ch * 4 + t], writes=[xk])
                norm_T(cx, xb, xk, cx.A1, cx.B1, vk1, hTc[cb][:, :, t * 128:(t + 1) * 128], hk)
            for mc in range(2):
                bank = ipb % 4
                ipb += 1
                for kc in range(8):
                    P.mm(ps[:, bank, :], winb[:, kc, mc * 128:(mc + 1) * 128], hTc[cb][:, kc, :], start=(kc == 0), stop=(kc == 7),
                         reads=["winb", hk], writes=[f"ps{bank}"])
                P.cp("act" if mc == 0 else "dve", uT[:, mc, ch * 512:(ch + 1) * 512], ps[:, bank, :],
                     reads=[f"ps{bank}"], writes=[f"uT{ch}"])
        P.fence()
        A.release(m2)
        zsb = A.alloc([2, 256, 64], BF16)
        zflat = zsb.rearrange("p r c n -> p (r c) n")
        for n2 in range(64):
            bank = n2 % 4
            for mc in range(2):
                P.mm(ps[:, bank, :], uT[:, mc, n2:S:64], cs[:, mc, :], start=(mc == 0), stop=(mc == 1),
                     reads=["cs"], writes=[f"ps{bank}"])
            P.cp("act" if n2 % 2 == 0 else "dve", zflat[:, :, n2], ps[:, bank, :], reads=[f"ps{bank}"], writes=["zsb"])
        P.fence()
        fsb = A.alloc([64, 256], BF16)
        p1 = [A.alloc([4, 2, 128], F32) for _ in range(2)]
        p2 = [A.alloc([4, 2, 128], F32) for _ in range(2)]
        apr = [A.alloc([4, 128], BF16) for _ in range(2)]
        api = [A.alloc([4, 128], BF16) for _ in range(2)]
        twr_b = twr[:, None, None, :].broadcast_to([128, 4, 2, 128])
        twi_b = twi[:, None, None, :].broadcast_to([128, 4, 2, 128])
        for grp in range(32):
            gb = grp % 2
            ab = 2 * gb
            for pi in range(4):
                pp = grp * 4 + pi
                bank = ab + pi // 2
                out = ps[:, bank, (pi % 2) * 256:(pi % 2) * 256 + 256]
                P.mm(out, zsb[:, 0, 2 * pp:2 * pp + 2, :].rearrange("p c n -> p (c n)"), r1, start=True, stop=False,
                     reads=["r1"], writes=[f"ps{bank}"], skip_group_check=True)
                P.mm(out, zsb[:, 1, 2 * pp:2 * pp + 2, :].rearrange("p c n -> p (c n)"), r2, start=False, stop=True,
                     reads=["r2"], writes=[f"ps{bank}"], skip_group_check=True)
            Av = ps[:, ab:ab + 2, :].rearrange("p a (b r k) -> p (a b) r k", r=2, k=128)
            P.tt("dve", p1[gb], Av, twr_b, ALU.mult, reads=[f"ps{ab}", f"ps{ab + 1}", "twr"], writes=[f"p1{gb}"])
            P.tt("dve", p2[gb], Av, twi_b, ALU.mult, reads=[f"ps{ab}", f"ps{ab + 1}", "twi"], writes=[f"p2{gb}"])
            P.tt("pool", apr[gb], p1[gb][:, :, 0, :], p2[gb][:, :, 1, :], ALU.subtract, reads=[f"p1{gb}", f"p2{gb}"], writes=[f"apr{gb}"])
            P.tt("pool", api[gb], p2[gb][:, :, 0, :], p1[gb][:, :, 1, :], ALU.add, reads=[f"p1{gb}", f"p2{gb}"], writes=[f"api{gb}"])
            fb = 4 + gb
            for pi in range(4):
                out = ps[:, fb, pi * 128:(pi + 1) * 128]
                P.mm(out, apr[gb][:, pi, :], bdc, start=True, stop=False, reads=[f"apr{gb}", "bdc"], writes=[f"ps{fb}"], skip_group_check=True)
                P.mm(out, api[gb][:, pi, :], bds, start=False, stop=True, reads=[f"api{gb}", "bds"], writes=[f"ps{fb}"], skip_group_check=True)
            src = ps[:, fb, :].rearrange("p (pi k c) -> p k pi c", pi=4, k=64, c=2)
            dst = fsb[:, :, 8 * grp:8 * grp + 8].rearrange("p k (pi c) -> p k pi c", c=2)
            P.cp("act" if grp % 2 == 0 else "dve", dst, src, reads=[f"ps{fb}"], writes=["fsb"])
        P.load(fo.rearrange("(k2 k1) c -> k1 k2 c", k1=128), fsb, reads=["fsb"], writes=["fo"])
        P.emit()
    return nc


def build_C():
    nc = bass.Bass("TRN2", target_bir_lowering=False)
    dram = lambda n, s, dt=F32, kind="ExternalInput": nc.dram_tensor(n, s, dt, kind=kind).ap()
    x1s = dram("x1s", [TOK, D])
    fsh = dram("fsh", [TOK, D], BF16)
    c_d = dram("c", [128, 8])
    adaw = dram("ada_w", [D, 6 * D]); adab = dram("ada_b", [6 * D])
    nmix = dram("norm_mix", [D]); nffn = dram("norm_ffn", [D])
    fwout = dram("fwout", [D, D])
    nfin = dram("norm_final", [D])
    ident_d = dram("ident", [128, 128], BF16); identf_d = dram("identf", [128, 128])
    wr = dram("wr", [128, 8, 20]); br = dram("br", [20])
    wg = dram("wg", [NE, D, DE]); wu = dram("wu", [NE, D, DE]); wd = dram("wd", [NE, DE, D])
    yo = dram("yo", [TOK, D], F32, "ExternalOutput")
    P = Prog(nc)
    with contextlib.ExitStack() as st:
        cx = Ctx()
        cx.P = P
        cx.A = A = Arena(nc, st)
        cx.ps = ps = st.enter_context(nc.psum_tensor("ps", [128, 8, 512], F32))
        setup_common(cx, ident_d, identf_d)
        stage_mod(cx, c_d, adaw, adab, nmix, nffn, "m1")
        mt = cx.modtag
        cx.xres = A.alloc([NT, D], F32)
        xkeys = [f"xres{t}" for t in range(NT)]
        P.load(cx.xres, x1s.rearrange("(t p) d -> p t d", p=128), writes=xkeys)
        m0 = A.mark()
        norm_scratch(cx)
        fsb = A.alloc([NT, D], BF16)
        fT = A.alloc([8, TOK], BF16)
        wob = A.alloc([8, D], BF16)
        P.load(fsb, fsh.rearrange("(t p) d -> p t d", p=128), writes=["fsb"])
        P.load(wob, fwout.rearrange("(kc p) n -> p kc n", p=128), writes=["wob"], eng="pool")
        for kc in range(8):
            P.tt("pool", wob[:, kc, :], wob[:, kc, :], cx.g1bc, ALU.mult, reads=["wob", mt + "g1bc"], writes=["wob"])
        for t in range(NT):
            transpose_tok(cx, fsb[:, t, :], "fsb", fT[:, :, t * 128:(t + 1) * 128], f"fT{t}", eng=("dve" if t % 2 else "act"))
        proj_residual(cx, fT, [f"fT{t}" for t in range(NT)], wob, "wob")
        P.fence()
        A.release(m0)
        stage_moe(cx, wr, br, wg, wu, wd, "e1")
        nfb = A.alloc([D], F32)
        P.load(nfb, nfin.partition_broadcast(128), writes=["nfb"])
        junk = A.alloc([D], BF16)
        fst = A.alloc([NT, 4], F32)
        ot = [A.alloc([D], F32) for _ in range(2)]
        yv = yo.rearrange("(t p) d -> t p d", p=128)
        for t in range(NT):
            P.act(junk, cx.xres[:, t, :], AF.Square, reads=[f"xres{t}"], writes=["fjunk", f"fst{t}"], accum_out=fst[:, t, 0:1])
            P.ts("dve", fst[:, t, 1:2], fst[:, t, 0:1], 1.0 / D, EPS, ALU.mult, ALU.add, reads=[f"fst{t}"], writes=[f"fst{t}"])
            P.act(fst[:, t, 2:3], fst[:, t, 1:2], AF.Sqrt, reads=[f"fst{t}"], writes=[f"fst{t}"])
            P.op("dve", lambda e, o=fst[:, t, 3:4], i_=fst[:, t, 2:3]: e.reciprocal(o, i_), reads=[f"fst{t}"], writes=[f"fst{t}"])
            ob = ot[t % 2]
            P.stt("dve", ob, cx.xres[:, t, :], fst[:, t, 3:4], nfb, ALU.mult, ALU.mult,
                  reads=[f"xres{t}", f"fst{t}", "nfb"], writes=[f"ot{t % 2}"])
            P.load(yv[t], ob, reads=[f"ot{t % 2}"], writes=[f"yo{t}"])
        P.emit()
    return nc


RG = [[0, 1, 2, 3], [4, 5, 6, 7]]
DEBUG_SKIP0 = False
DBG = {}


def fourier_body(cx, fwin, hTd, hTg, fd, tabs):
    P, A, ps = cx.P, cx.A, cx.ps
    mt = cx.modtag
    cs_d, r1_d, r2_d, twr_d, twi_d, bdc_d, bds_d = tabs
    m_f = A.mark()
    cs = A.alloc([2, 2, 256], BF16); r1 = A.alloc([256], BF16); r2 = A.alloc([256], BF16)
    twr = A.alloc([128], F32); twi = A.alloc([128], F32)
    bdc = A.alloc([128], BF16); bds = A.alloc([128], BF16)
    for ap_, d_, k_ in [(cs, cs_d, "cs"), (r1, r1_d, "r1"), (r2, r2_d, "r2"), (twr, twr_d, "twr"),
                        (twi, twi_d, "twi"), (bdc, bdc_d, "bdc"), (bds, bds_d, "bds")]:
        P.load(ap_, d_, writes=[k_])
    uT = A.alloc([2, S], BF16)
    u_off = A.last
    m2 = A.mark()
    norm_scratch(cx)
    hTo = A.alloc([8, TOK], BF16)
    vk1 = [mt + "A1", mt + "B1"]
    for t0 in range(0, NT, 4):
        ts4 = list(range(t0, t0 + 4))
        norm_T_multi(cx, [cx.xres[:, t, :] for t in ts4], [f"xres{t}" for t in ts4], cx.A1, cx.B1, vk1,
                     [hTo[:, :, t * 128:(t + 1) * 128] for t in ts4], [f"hTo{t}" for t in ts4])
    for i in range(4):
        P.load(hTd[i].rearrange("(kc p) t -> p kc t", p=128), hTo[:, :, i * 512:(i + 1) * 512],
               reads=[f"hTo{t}" for t in range(4 * i, 4 * i + 4)], writes=[f"hTd{i}"])
        P.cc("AllGather", RG, hTd[i], hTg[i], reads=[f"hTd{i}"], writes=[f"hTg{i}"])
    winb = A.alloc([8, 256], BF16)
    P.load(winb, fwin.rearrange("(kc p) n -> p kc n", p=128), writes=["winb"], eng="pool")
    hTc = [A.alloc([8, 512], BF16) for _ in range(2)]
    hTg_v = [g_.rearrange("(r kc p) t -> r p kc t", kc=8, p=128) for g_ in hTg]
    ipb = 0
    for it_ in range(S // 512):
        c4, r_ = it_ // 4, it_ % 4
        ch = 4 * r_ + c4
        cb = it_ % 2
        hk = f"hTc{cb}"
        P.load(hTc[cb], hTg_v[c4][r_], reads=[f"hTg{c4}"], writes=[hk])
        for mc in range(2):
            bank = ipb % 4
            ipb += 1
            for kc in range(8):
                P.mm(ps[:, bank, :], winb[:, kc, mc * 128:(mc + 1) * 128], hTc[cb][:, kc, :], start=(kc == 0), stop=(kc == 7),
                     reads=["winb", hk], writes=[f"ps{bank}"])
            P.cp("act" if mc == 0 else "dve", uT[:, mc, ch * 512:(ch + 1) * 512], ps[:, bank, :],
                 reads=[f"ps{bank}"], writes=[f"uT{ch}"])
    P.fence()
    A.release(m2)
    zsb = A.alloc([2, 128, 64], BF16)
    zflat = zsb.rearrange("p r c n -> p (r c) n")
    fsb = A.alloc([64, 256], BF16)
    p1 = A.alloc([4, 2, 128], F32)
    p2 = A.alloc([4, 2, 128], F32)
    apr = [A.alloc([4, 128], BF16) for _ in range(2)]
    api = [A.alloc([4, 128], BF16) for _ in range(2)]
    twr_b = twr[:, None, None, :].broadcast_to([128, 4, 2, 128])
    twi_b = twi[:, None, None, :].broadcast_to([128, 4, 2, 128])
    for hf in range(2):
        for n2 in range(64):
            bank = n2 % 2
            for mc in range(2):
                P.mm(ps[:, bank, 0:256], uT[:, mc, n2:S:64], cs[:, mc, hf, :], start=(mc == 0), stop=(mc == 1),
                     reads=["cs"], writes=[f"ps{bank}"])
            P.cp("act" if n2 % 2 == 0 else "dve", zflat[:, :, n2], ps[:, bank, 0:256], reads=[f"ps{bank}"], writes=["zsb"])
        for grp in range(16):
            gb = grp % 2
            ab = 2 + 2 * gb
            for pi in range(4):
                pp = grp * 4 + pi
                bank = ab + pi // 2
                out = ps[:, bank, (pi % 2) * 256:(pi % 2) * 256 + 256]
                P.mm(out, zsb[:, 0, 2 * pp:2 * pp + 2, :].rearrange("p c n -> p (c n)"), r1, start=True, stop=False,
                     reads=["r1", "zsb"], writes=[f"ps{bank}"], skip_group_check=True)
                P.mm(out, zsb[:, 1, 2 * pp:2 * pp + 2, :].rearrange("p c n -> p (c n)"), r2, start=False, stop=True,
                     reads=["r2", "zsb"], writes=[f"ps{bank}"], skip_group_check=True)
            Av = ps[:, ab:ab + 2, :].rearrange("p a (b r k) -> p (a b) r k", r=2, k=128)
            P.tt("dve", p1, Av, twr_b, ALU.mult, reads=[f"ps{ab}", f"ps{ab + 1}", "twr"], writes=["p1"])
            P.tt("dve", p2, Av, twi_b, ALU.mult, reads=[f"ps{ab}", f"ps{ab + 1}", "twi"], writes=["p2"])
            P.tt("pool", apr[gb], p1[:, :, 0, :], p2[:, :, 1, :], ALU.subtract, reads=["p1", "p2"], writes=[f"apr{gb}"])
            P.tt("pool", api[gb], p2[:, :, 0, :], p1[:, :, 1, :], ALU.add, reads=["p1", "p2"], writes=[f"api{gb}"])
            fb = 6 + gb
            for pi in range(4):
                out = ps[:, fb, pi * 128:(pi + 1) * 128]
                P.mm(out, apr[gb][:, pi, :], bdc, start=True, stop=False, reads=[f"apr{gb}", "bdc"], writes=[f"ps{fb}"], skip_group_check=True)
                P.mm(out, api[gb][:, pi, :], bds, start=False, stop=True, reads=[f"api{gb}", "bds"], writes=[f"ps{fb}"], skip_group_check=True)
            src = ps[:, fb, :].rearrange("p (pi k c) -> p k pi c", pi=4, k=64, c=2)
            c0 = hf * 128 + 8 * grp
            dst = fsb[:, :, c0:c0 + 8].rearrange("p k (pi c) -> p k pi c", c=2)
            P.cp("act" if grp % 2 == 0 else "dve", dst, src, reads=[f"ps{fb}"], writes=["fsb"])
    for i in range(4):
        P.load(fd[i].rearrange("(k2 k1) c -> k1 k2 c", k1=128), fsb[:, 16 * i:16 * (i + 1), :], reads=["fsb"], writes=[f"fd{i}"])
    P.fence()
    A.release(m_f)


def tail_body(cx, fd, fg, sel_d, fwout, nfin, wr, br, wg, wu, wd, yo):
    P, A, ps = cx.P, cx.A, cx.ps
    mt = cx.modtag
    for i in range(4):
        P.cc("AllGather", RG, fd[i], fg[i], reads=[f"fd{i}"], writes=[f"fg{i}"])
    m0 = A.mark()
    sel = A.alloc([4], F32)
    P.load(sel, sel_d, writes=["sel"])
    fsel = A.alloc([NT, D], BF16)
    wob = A.alloc([8, D], BF16)
    cand = A.alloc([NT, D], BF16)
    c_off = A.last
    P.load(wob, fwout.rearrange("(kc p) n -> p kc n", p=128), writes=["wob"], eng="pool")
    for kc in range(8):
        P.tt("pool", wob[:, kc, :], wob[:, kc, :], cx.g1bc, ALU.mult, reads=["wob", mt + "g1bc"], writes=["wob"])
    for r_ in range(4):
        fg_v = fg[r_].rearrange("(g t p) c -> g p t c", g=4, p=128)
        for g in range(4):
            P.load(cand[:, :, g * 256:(g + 1) * 256], fg_v[g], reads=[f"fg{r_}"], writes=["cand"])
        if r_ == 0:
            P.ts("dve", fsel, cand, sel[:, 0:1], None, ALU.mult, reads=["cand", "sel"], writes=["fsel"])
        else:
            P.stt("dve", fsel, cand, sel[:, r_:r_ + 1], fsel, ALU.mult, ALU.add, reads=["cand", "sel"], writes=["fsel"])
    P.fence()
    fT = A.view(c_off, [8, TOK], BF16)
    for t in range(NT):
        transpose_tok(cx, fsel[:, t, :], "fsel", fT[:, :, t * 128:(t + 1) * 128], f"fT{t}", eng=("dve" if t % 2 else "act"))
    proj_residual(cx, fT, [f"fT{t}" for t in range(NT)], wob, "wob")
    P.fence()
    A.release(m0)
    stage_moe(cx, wr, br, wg, wu, wd, "e1")
    nfb = A.alloc([D], F32)
    P.load(nfb, nfin.partition_broadcast(128), writes=["nfb"])
    junk = A.alloc([D], BF16)
    fst = A.alloc([NT, 4], F32)
    ot = [A.alloc([D], F32) for _ in range(2)]
    yv = yo.rearrange("(t p) d -> t p d", p=128)
    for t in range(NT):
        P.act(junk, cx.xres[:, t, :], AF.Square, reads=[f"xres{t}"], writes=["fjunk", f"fst{t}"], accum_out=fst[:, t, 0:1])
        P.ts("dve", fst[:, t, 1:2], fst[:, t, 0:1], 1.0 / D, EPS, ALU.mult, ALU.add, reads=[f"fst{t}"], writes=[f"fst{t}"])
        P.act(fst[:, t, 2:3], fst[:, t, 1:2], AF.Sqrt, reads=[f"fst{t}"], writes=[f"fst{t}"])
        P.op("dve", lambda e, o=fst[:, t, 3:4], i_=fst[:, t, 2:3]: e.reciprocal(o, i_), reads=[f"fst{t}"], writes=[f"fst{t}"])
        ob = ot[t % 2]
        P.stt("dve", ob, cx.xres[:, t, :], fst[:, t, 3:4], nfb, ALU.mult, ALU.mult,
              reads=[f"xres{t}", f"fst{t}", "nfb"], writes=[f"ot{t % 2}"])
        P.load(yv[t], ob, reads=[f"ot{t % 2}"], writes=[f"yo{t}"])


def build_F():
    nc = bass.Bass("TRN2", target_bir_lowering=False)
    dram = lambda n, s, dt=F32, kind="ExternalInput": nc.dram_tensor(n, s, dt, kind=kind).ap()
    xp = dram("xp", [S, D])
    c_d = dram("c", [128, 8])
    adaw = dram("ada_w", [2, D, 6 * D]); adab = dram("ada_b", [2, 6 * D])
    nmix = dram("norm_mix", [2, D]); nffn = dram("norm_ffn", [2, D])
    if not DEBUG_SKIP0:
        w_in = dram("w_in", [D, 3 * D]); w_out = dram("w_out", [D, D])
        lam = dram("lam", [4, 64]); subln = dram("subln", [128, 1])
        kaug = dram("kaug", [H, 8, 2, S], BF16); qaug = dram("qaug", [8, 2, TOK], BF16)
        dfix = dram("dfix", [128, H, 128], BF16)
    ident_d = dram("ident", [128, 128], BF16); identf_d = dram("identf", [128, 128])
    wr = dram("wr", [2, 128, 8, 20]); br = dram("br", [2, 20])
    wg = dram("wg", [2, NE, D, DE]); wu = dram("wu", [2, NE, D, DE]); wd = dram("wd", [2, NE, DE, D])
    fwin = dram("fwin", [D, 256]); fwout = dram("fwout", [D, D]); nfin = dram("norm_final", [D])
    cs_d = dram("cs", [128, 2, 2, 256], BF16)
    r1_d = dram("r1", [128, 256], BF16); r2_d = dram("r2", [128, 256], BF16)
    twr_d = dram("twr", [128, 128]); twi_d = dram("twi", [128, 128])
    bdc_d = dram("bdc", [128, 128], BF16); bds_d = dram("bds", [128, 128], BF16)
    sel_d = dram("sel", [128, 4])
    yo = dram("yo", [TOK, D], F32, "ExternalOutput")
    if not DEBUG_SKIP0:
        kscr = dram("kscr", [2, H, 64, S], BF16, "Internal")
        qscr = dram("qscr", [2, H, 64, TOK], BF16, "Internal")
        vscr = dram("vscr", [S // 128, 128, H, 129], BF16, "Internal")
    hTd = [dram(f"hTd{i}", [D, 512], BF16, "Internal") for i in range(4)]
    hTg = [dram(f"hTg{i}", [4 * D, 512], BF16, "Internal") for i in range(4)]
    fd = [dram(f"fd{i}", [TOK, 256], BF16, "Internal") for i in range(4)]
    fg = [dram(f"fg{i}", [4 * TOK, 256], BF16, "Internal") for i in range(4)]
    P = Prog(nc)
    with contextlib.ExitStack() as st:
        cx = Ctx()
        cx.P = P
        cx.A = A = Arena(nc, st)
        cx.ps = st.enter_context(nc.psum_tensor("ps", [128, 8, 512], F32))
        setup_common(cx, ident_d, identf_d)
        stage_mod(cx, c_d, adaw[0], adab[0], nmix[0], nffn[0], "m0")
        if DEBUG_SKIP0:
            cx.xres = A.alloc([NT, D], F32)
            P.load(cx.xres, xp[0:TOK].rearrange("(t p) d -> p t d", p=128), writes=[f"xres{t}" for t in range(NT)])
        else:
            layer0_body(cx, xp, w_in, w_out, lam, subln, kaug, qaug, dfix, kscr, qscr, vscr, wr[0], br[0], wg[0], wu[0], wd[0])
        stage_mod(cx, c_d, adaw[1], adab[1], nmix[1], nffn[1], "m1")
        fourier_body(cx, fwin, hTd, hTg, fd, (cs_d, r1_d, r2_d, twr_d, twi_d, bdc_d, bds_d))
        tail_body(cx, fd, fg, sel_d, fwout, nfin, wr[1], br[1], wg[1], wu[1], wd[1], yo)
        P.emit()
    return nc


def _bf(a):
    return np.asarray(a, dtype=np.float32).astype(ml_dtypes.bfloat16)


def _consts():
    ident = _bf(np.eye(128))
    identf = np.eye(128, dtype=np.float32)
    return ident, identf


def _attn_tables(j):
    own = np.arange(TOK * j, TOK * (j + 1))
    others = np.concatenate([np.arange(0, TOK * j), np.arange(TOK * (j + 1), S)])
    perm = np.concatenate([own, others])
    pos = perm.astype(np.float64)
    kt_abs = np.floor(pos / 128) * 128
    kr = pos - kt_abs
    slopes = 2.0 ** (-(np.arange(H) + 1.0))
    kaug = np.zeros((H, 8, 2, S), np.float64)
    after = (perm >= TOK * (j + 1))
    own_m = np.arange(S) < TOK
    use2 = np.where(own_m | after, 1.0, 0.0)
    for h in range(H):
        sl = slopes[h]
        set1 = np.stack([np.full(S, -sl), np.full(S, -sl), sl * kt_abs, sl * kr])
        kaug[h, 0:4, :, :] = set1[:, None, :]
        kaug[h, 4:8, :, :] = (-2.0 * set1 * use2[None, :])[:, None, :]
    qpos = own.astype(np.float64)
    qt_abs = np.floor(qpos / 128) * 128
    qr = qpos - qt_abs
    q4 = np.stack([qt_abs, qr, np.ones(TOK), np.ones(TOK)])
    qaug = np.zeros((8, 2, TOK), np.float64)
    qaug[0:4] = q4[:, None, :]
    qaug[4:8] = q4[:, None, :]
    krr = np.arange(128)[:, None]
    qrr = np.arange(128)[None, :]
    dfix = np.zeros((128, H, 128), np.float64)
    for h in range(H):
        dfix[:, h, :] = -2.0 * slopes[h] * np.maximum(krr - qrr, 0)
    return perm, _bf(kaug), _bf(qaug), _bf(dfix)


_NC_CACHE = {}


def _get(name, fn):
    if name not in _NC_CACHE:
        _NC_CACHE[name] = fn()
    return _NC_CACHE[name]


def run_A(inp):
    ident, identf = _consts()
    maps = []
    for c in range(NCORE):
        b, j = c // 4, c % 4
        perm, kaug, qaug, dfix = _attn_tables(j)
        lam = np.stack([inp["attn_lam_q1"][0], inp["attn_lam_k1"][0], inp["attn_lam_q2"][0], inp["attn_lam_k2"][0]])
        wr = np.concatenate([inp["router_group_w"][0], inp["router_expert_w"][0].reshape(D, 16)], axis=1)
        br = np.concatenate([inp["router_group_b"][0], inp["router_expert_b"][0].reshape(16)])
        maps.append({
            "xp": np.ascontiguousarray(inp["x"][b][perm]),
            "c": np.ascontiguousarray(inp["c"][b].reshape(8, 128).T),
            "ada_w": inp["ada_w"][0], "ada_b": inp["ada_b"][0],
            "norm_mix": inp["norm_mix"][0], "norm_ffn": inp["norm_ffn"][0],
            "w_in": inp["attn_w_in"][0], "w_out": inp["attn_w_out"][0],
            "lam": np.ascontiguousarray(lam), "subln": np.ascontiguousarray(inp["attn_subln"][0].reshape(128, 1)),
            "kaug": kaug, "qaug": qaug, "dfix": dfix, "ident": ident, "identf": identf,
            "wr": np.ascontiguousarray(wr.reshape(8, 128, 20).transpose(1, 0, 2)), "br": np.ascontiguousarray(br),
            "wg": inp["expert_w_gate"][0], "wu": inp["expert_w_up"][0], "wd": inp["expert_w_down"][0],
        })
    nc = _get("A", build_A)
    res = run_bass_kernel_spmd(nc, maps, core_ids=list(range(NCORE)))
    x1 = np.zeros((B, S, D), np.float32)
    for c in range(NCORE):
        b, j = c // 4, c % 4
        x1[b, TOK * j:TOK * (j + 1)] = res.results[c]["xo"]
    if DBG.get("on"):
        DBG["res"] = [{k: np.asarray(v) for k, v in r.items()} for r in res.results]
    return x1


def _fourier_tables():
    m = np.arange(256)[:, None].astype(np.float64); l = np.arange(256)[None, :].astype(np.float64)
    ang = 2 * np.pi * m * l / 256
    cs = np.concatenate([np.cos(ang), -np.sin(ang)], axis=1) / 16.0
    cs = cs.reshape(2, 128, 512).transpose(1, 0, 2)
    n1 = np.arange(128)[:, None].astype(np.float64); k1 = np.arange(128)[None, :].astype(np.float64)
    a = 2 * np.pi * n1 * k1 / 128
    r1 = np.concatenate([np.cos(a), -np.sin(a)], axis=1)
    r2 = np.concatenate([np.sin(a), np.cos(a)], axis=1)
    n2 = (np.arange(128) % 64)[:, None].astype(np.float64)
    tw = 2 * np.pi * n2 * k1 / 8192
    twr = np.cos(tw); twi = -np.sin(tw)
    bdc = np.zeros((128, 128)); bds = np.zeros((128, 128))
    sc = 1.0 / math.sqrt(8192.0)
    for c in range(2):
        nn = np.arange(64)[:, None].astype(np.float64); kk = np.arange(64)[None, :].astype(np.float64)
        a2 = 2 * np.pi * nn * kk / 64
        bdc[c * 64:(c + 1) * 64, c::2] = np.cos(a2) * sc
        bds[c * 64:(c + 1) * 64, c::2] = np.sin(a2) * sc
    return (_bf(cs), _bf(r1), _bf(r2), twr.astype(np.float32), twi.astype(np.float32), _bf(bdc), _bf(bds))


def _moe_router(inp, i):
    wr = np.concatenate([inp["router_group_w"][i], inp["router_expert_w"][i].reshape(D, 16)], axis=1)
    br = np.concatenate([inp["router_group_b"][i], inp["router_expert_b"][i].reshape(16)])
    return np.ascontiguousarray(wr.reshape(8, 128, 20).transpose(1, 0, 2)), np.ascontiguousarray(br)


def run_B(inp, x1):
    ident, identf = _consts()
    cs, r1, r2, twr, twi, bdc, bds = _fourier_tables()
    maps = []
    for c in range(NCORE):
        b, g = c // 4, c % 4
        maps.append({
            "x1": np.ascontiguousarray(x1[b]),
            "c": np.ascontiguousarray(inp["c"][b].reshape(8, 128).T),
            "ada_w": inp["ada_w"][1], "ada_b": inp["ada_b"][1],
            "norm_mix": inp["norm_mix"][1], "norm_ffn": inp["norm_ffn"][1],
            "fwin": np.ascontiguousarray(inp["fourier_w_in"][0][:, 256 * g:256 * (g + 1)]),
            "cs": cs, "r1": r1, "r2": r2, "twr": twr, "twi": twi, "bdc": bdc, "bds": bds,
            "ident": ident, "identf": identf,
        })
    nc = _get("B", build_B)
    res = run_bass_kernel_spmd(nc, maps, core_ids=list(range(NCORE)))
    f = np.zeros((B, S, D), ml_dtypes.bfloat16)
    for c in range(NCORE):
        b, g = c // 4, c % 4
        f[b, :, 256 * g:256 * (g + 1)] = res.results[c]["fo"]
    return f


def run_C(inp, x1, f):
    ident, identf = _consts()
    wr, br = _moe_router(inp, 1)
    maps = []
    for c in range(NCORE):
        b, j = c // 4, c % 4
        maps.append({
            "x1s": np.ascontiguousarray(x1[b, TOK * j:TOK * (j + 1)]),
            "fsh": np.ascontiguousarray(f[b, TOK * j:TOK * (j + 1)]),
            "c": np.ascontiguousarray(inp["c"][b].reshape(8, 128).T),
            "ada_w": inp["ada_w"][1], "ada_b": inp["ada_b"][1],
            "norm_mix": inp["norm_mix"][1], "norm_ffn": inp["norm_ffn"][1],
            "fwout": inp["fourier_w_out"][0], "norm_final": inp["norm_final"],
            "ident": ident, "identf": identf, "wr": wr, "br": br,
            "wg": inp["expert_w_gate"][1], "wu": inp["expert_w_up"][1], "wd": inp["expert_w_down"][1],
        })
    nc = _get("C", build_C)
    res = run_bass_kernel_spmd(nc, maps, core_ids=list(range(NCORE)))
    out = np.zeros((B, S, D), np.float32)
    for c in range(NCORE):
        b, j = c // 4, c % 4
        out[b, TOK * j:TOK * (j + 1)] = res.results[c]["yo"]
    return out


def run_F(inp):
    ident, identf = _consts()
    cs, r1, r2, twr, twi, bdc, bds = _fourier_tables()
    csf = np.asarray(cs).reshape(128, 2, 2, 2, 128).transpose(0, 1, 3, 2, 4).reshape(128, 2, 2, 256)
    csf = np.ascontiguousarray(csf)
    lam = np.ascontiguousarray(np.stack([inp["attn_lam_q1"][0], inp["attn_lam_k1"][0],
                                         inp["attn_lam_q2"][0], inp["attn_lam_k2"][0]]))
    wr0, br0 = _moe_router(inp, 0)
    wr1, br1 = _moe_router(inp, 1)
    wr = np.ascontiguousarray(np.stack([wr0, wr1])); br = np.ascontiguousarray(np.stack([br0, br1]))
    maps = []
    for c in range(NCORE):
        b, j = c // 4, c % 4
        perm, kaug, qaug, dfix = _attn_tables(j)
        sel = np.zeros((128, 4), np.float32)
        sel[:, j] = 1.0
        maps.append({
            "xp": np.ascontiguousarray(inp["x"][b][perm]),
            "c": np.ascontiguousarray(inp["c"][b].reshape(8, 128).T),
            "ada_w": inp["ada_w"], "ada_b": inp["ada_b"],
            "norm_mix": inp["norm_mix"], "norm_ffn": inp["norm_ffn"],
            "w_in": inp["attn_w_in"][0], "w_out": inp["attn_w_out"][0],
            "lam": lam, "subln": np.ascontiguousarray(inp["attn_subln"][0].reshape(128, 1)),
            "kaug": kaug, "qaug": qaug, "dfix": dfix, "ident": ident, "identf": identf,
            "wr": wr, "br": br,
            "wg": inp["expert_w_gate"], "wu": inp["expert_w_up"], "wd": inp["expert_w_down"],
            "fwin": np.ascontiguousarray(inp["fourier_w_in"][0][:, 256 * j:256 * (j + 1)]),
            "fwout": inp["fourier_w_out"][0], "norm_final": inp["norm_final"],
            "cs": csf, "r1": r1, "r2": r2, "twr": twr, "twi": twi, "bdc": bdc, "bds": bds, "sel": sel,
        })
    nc = _get("F", build_F)
    if DEBUG_SKIP0:
        drop = {"w_in", "w_out", "lam", "subln", "kaug", "qaug", "dfix"}
        maps = [{k: v for k, v in m.items() if k not in drop} for m in maps]
    res = run_bass_kernel_spmd(nc, maps, core_ids=list(range(NCORE)))
    out = np.zeros((B, S, D), np.float32)
    for c in range(NCORE):
        b, j = c // 4, c % 4
        out[b, TOK * j:TOK * (j + 1)] = res.results[c]["yo"]
    return out


def kernel(**inputs):
    inp = {k: np.asarray(v, dtype=np.float32) for k, v in inputs.items()}
    return run_F(inp)
```

```python
import math
import contextlib
import numpy as np
import ml_dtypes
import concourse.bass as bass
import concourse.mybir as mybir
from concourse.bass_utils import run_bass_kernel_spmd

F32 = mybir.dt.float32
BF16 = mybir.dt.bfloat16
AF = mybir.ActivationFunctionType
ALU = mybir.AluOpType
AX = mybir.AxisListType

D = 1024
S = 8192
B = 2
NCORE = 8
TOK = 2048
NT = TOK // 128
H = 8
DE = 512
NE = 16
EPS = 1e-6
LAM_INIT0 = 0.8 - 0.6 * math.exp(-0.3 * 0)

ENGS = ("pe", "act", "dve", "pool", "sp")
EPOCH = 12000
NDMASEM = 10


class _Op:
    __slots__ = ("eng", "fn", "waits", "is_dma", "seq", "sem_slot", "is_cc")


class Prog:
    def __init__(self, nc):
        self.nc = nc
        self.ops = {e: [] for e in ENGS}
        self.cnt = {e: 0 for e in ENGS}
        self.last_w = {}
        self.readers = {}
        self.waited = {e: {} for e in ENGS}
        self.dma_rr = {e: 0 for e in ENGS}
        self.dma_cnt = {}
        self.dma_last = {}
        self.semkeys = set()
        self.pending_fence = {e: [] for e in ENGS}

    def _event(self, op):
        if op.is_dma:
            return (("dma", op.eng, op.sem_slot), op.seq)
        return (("eng", op.eng, (op.seq - 1) // EPOCH), ((op.seq - 1) % EPOCH) + 1)

    def _add_wait(self, op, dep):
        if dep is None or dep is op:
            return
        if (not dep.is_dma) and dep.eng == "pe" and op.eng == "pe" and not op.is_dma:
            return
        key, val = self._event(dep)
        w = self.waited[op.eng]
        if w.get(key, 0) >= val:
            return
        w[key] = val
        op.waits.append((key, val))
        self.semkeys.add(key)

    def fence(self):
        deps = []
        for e in ENGS:
            for op in reversed(self.ops[e]):
                if not op.is_dma:
                    deps.append(op)
                    break
        deps.extend(self.dma_last.values())
        for e in ENGS:
            self.pending_fence[e] = list(deps)

    def _record(self, eng, fn, reads, writes, is_dma):
        op = _Op()
        op.eng = eng
        op.fn = fn
        op.waits = []
        op.is_dma = is_dma
        op.is_cc = False
        if is_dma == "cc":
            op.is_dma = True
            op.is_cc = True
            self.ncc = getattr(self, "ncc", 0) + 1
            op.sem_slot = f"cc{self.ncc}"
            op.seq = 1
            self.semkeys.add(("dma", eng, op.sem_slot))
            self.dma_last[(eng, op.sem_slot)] = op
        elif is_dma:
            slot = self.dma_rr[eng] % NDMASEM
            self.dma_rr[eng] += 1
            k = (eng, slot)
            prev = self.dma_last.get(k)
            self.dma_cnt[k] = self.dma_cnt.get(k, 0) + 1
            op.sem_slot = slot
            op.seq = 16 * self.dma_cnt[k]
            self.semkeys.add(("dma", eng, slot))
            if prev is not None:
                self._add_wait(op, prev)
            self.dma_last[k] = op
        else:
            self.cnt[eng] += 1
            op.seq = self.cnt[eng]
            op.sem_slot = None
            self.semkeys.add(("eng", eng, (op.seq - 1) // EPOCH))
        if self.pending_fence[eng]:
            for d in self.pending_fence[eng]:
                self._add_wait(op, d)
            self.pending_fence[eng] = []
        for r in reads:
            self._add_wait(op, self.last_w.get(r))
        for w in writes:
            self._add_wait(op, self.last_w.get(w))
            for rd in self.readers.get(w, ()):
                self._add_wait(op, rd)
        for r in reads:
            self.readers.setdefault(r, []).append(op)
        for w in writes:
            self.last_w[w] = op
            self.readers[w] = []
        self.ops[eng].append(op)
        return op

    def op(self, eng, fn, reads=(), writes=()):
        return self._record(eng, fn, reads, writes, False)

    def dma(self, eng, fn, reads=(), writes=()):
        return self._record(eng, fn, reads, writes, True)

    def cc(self, kind, rg, src, dst, reads=(), writes=()):
        return self._record("pool", lambda e: e.collective_compute(kind, ALU.bypass, replica_groups=rg,
                                                                    ins=[src], outs=[dst]), reads, writes, "cc")

    def mm(self, out, lhsT, rhs, start=True, stop=True, reads=(), writes=(), **kw):
        return self.op("pe", lambda e: e.matmul(out, lhsT, rhs, start=start, stop=stop, **kw), reads, writes)

    def tr(self, out, in_, ident, reads=(), writes=()):
        return self.op("pe", lambda e: e.transpose(out, in_, ident), reads, writes)

    def act(self, out, in_, func, reads=(), writes=(), **kw):
        o = self.op("act", lambda e: e.activation(out, in_, func, **kw), reads, writes)
        acc = kw.get("accum_out")
        if acc is not None and getattr(self, "act_dummy", None) is not None:
            dm = self.act_dummy
            o = self.op("act", lambda e: e.copy(dm, acc), (), writes)
        return o

    def tt(self, eng, out, a, b, op, reads=(), writes=()):
        return self.op(eng, lambda e: e.tensor_tensor(out, a, b, op), reads, writes)

    def ts(self, eng, out, a, s1, s2, op0, op1=None, reads=(), writes=()):
        if op1 is None:
            return self.op(eng, lambda e: e.tensor_scalar(out, a, s1, None, op0), reads, writes)
        return self.op(eng, lambda e: e.tensor_scalar(out, a, s1, s2, op0, op1), reads, writes)

    def stt(self, eng, out, a, s, b, op0, op1, reads=(), writes=()):
        return self.op(eng, lambda e: e.scalar_tensor_tensor(out, a, s, b, op0, op1), reads, writes)

    def cp(self, eng, out, in_, reads=(), writes=()):
        if eng == "act":
            return self.op(eng, lambda e: e.copy(out, in_), reads, writes)
        return self.op(eng, lambda e: e.tensor_copy(out, in_), reads, writes)

    def memset(self, eng, ap, val, writes=()):
        return self.op(eng, lambda e: e.memset(ap, val), (), writes)

    def load(self, out, in_, reads=(), writes=(), eng="sp"):
        return self.dma(eng, lambda e: e.dma_start(out=out, in_=in_), reads, writes)

    def emit(self):
        nc = self.nc
        with contextlib.ExitStack() as st:
            sems = {}
            for key in sorted(self.semkeys, key=str):
                nm = "s_" + "_".join(str(k) for k in key)
                sems[key] = st.enter_context(nc.semaphore(nm))
            finals = []
            for e in ENGS:
                for op in reversed(self.ops[e]):
                    if not op.is_dma:
                        finals.append(self._event(op))
                        break
            for op in self.dma_last.values():
                finals.append(self._event(op))
            block = st.enter_context(nc.Block())
            engmap = {"pe": block.tensor, "act": block.scalar, "dve": block.vector,
                      "pool": block.gpsimd, "sp": block.sync}

            def make(ename):
                ops = self.ops[ename]

                def body(eng):
                    for op in ops:
                        for key, val in op.waits:
                            eng.wait_ge(sems[key], val)
                        ins = op.fn(eng)
                        key, val = self._event(op)
                        if op.is_cc:
                            ins.then_inc(sems[key])
                        else:
                            ins.then_inc(sems[key], 16 if op.is_dma else 1)
                    if ename == "sp":
                        for key, val in finals:
                            eng.wait_ge(sems[key], val)
                return body

            for e in ENGS:
                engmap[e](make(e))


class Arena:
    def __init__(self, nc, st, words=51000):
        self.t = st.enter_context(nc.sbuf_tensor("arena", [128, words], F32))
        self.words = words
        self.top = 0

    def alloc(self, shape, dt):
        n = int(np.prod(shape))
        w = n if dt == F32 else (n + 1) // 2
        w = (w + 7) // 8 * 8
        a = self.top
        self.top += w
        assert self.top <= self.words, ("SBUF arena overflow", self.top, self.words)
        self.last = a
        return self.view(a, shape, dt)

    def view(self, a, shape, dt):
        n = int(np.prod(shape))
        w = n if dt == F32 else (n + 1) // 2
        w = (w + 7) // 8 * 8
        ap = self.t[:, a:a + w]
        if dt == BF16:
            ap = ap.bitcast(BF16)
        ap = ap[:, 0:n]
        if len(shape) > 1:
            names = [f"d{i}" for i in range(len(shape))]
            kw = {names[i]: int(shape[i]) for i in range(1, len(shape))}
            ap = ap.rearrange(f"p ({' '.join(names)}) -> p {' '.join(names)}", **kw)
        return ap

    def mark(self):
        return self.top

    def release(self, m):
        self.top = m


class Ctx:
    pass


def bc_mid(ap, n):
    return ap[:, :, None].broadcast_to([128, ap.shape[1], n])


def setup_common(cx, ident_d, identf_d):
    P, A = cx.P, cx.A
    cx.ident = A.alloc([128], BF16)
    cx.identf = A.alloc([128], F32)
    P.act_dummy = A.alloc([1], F32)
    P.load(cx.ident, ident_d, writes=["ident"])
    P.load(cx.identf, identf_d, writes=["identf"])


def diag_extract(cx, dst, src_bc, rkeys, wkey):
    P = cx.P
    tmp = cx.diag_tmp
    P.tt("dve", tmp, src_bc.rearrange("p (a b) -> p a b", b=128),
         cx.identf[:, None, :].broadcast_to([128, 8, 128]), ALU.mult,
         reads=list(rkeys) + ["identf"], writes=["diag_tmp"])
    P.op("dve", lambda e: e.tensor_reduce(dst, tmp, AX.X, ALU.add), reads=["diag_tmp"], writes=[wkey])


def stage_mod(cx, c_d, adaw_d, adab_d, nmix_d, nffn_d, tag):
    P, A, ps = cx.P, cx.A, cx.ps
    if getattr(cx, "A1", None) is not None:
        A1, B1, A2, B2, g1bc, g2bc = cx.A1, cx.B1, cx.A2, cx.B2, cx.g1bc, cx.g2bc
    else:
        A1 = A.alloc([8], F32); B1 = A.alloc([8], F32); A2 = A.alloc([8], F32); B2 = A.alloc([8], F32)
        g1bc = A.alloc([D], F32); g2bc = A.alloc([D], F32)
    m = A.mark()
    csb = A.alloc([8], F32)
    cond = A.alloc([8], F32)
    condbc = A.alloc([8, 128], F32)
    modbc = A.alloc([6 * D], F32)
    adab = A.alloc([6 * D], F32)
    nm = A.alloc([D], F32)
    nf = A.alloc([D], F32)
    cx.diag_tmp = A.alloc([8, 128], F32)
    wbuf = [A.alloc([8, 512], F32) for _ in range(2)]
    t = tag
    P.load(csb, c_d, writes=[t + "csb"])
    P.load(adab, adab_d.partition_broadcast(128), writes=[t + "adab"])
    P.load(nm, nmix_d.partition_broadcast(128), writes=[t + "nm"])
    P.load(nf, nffn_d.partition_broadcast(128), writes=[t + "nf"])
    P.act(cond, csb, AF.Silu, reads=[t + "csb"], writes=[t + "cond"])
    P.cp("dve", condbc, bc_mid(cond, 128), reads=[t + "cond"], writes=[t + "condbc"])
    wv = adaw_d.rearrange("(kc p) n -> p kc n", p=128)
    for nch in range(12):
        wb = wbuf[nch % 2]
        wk = f"{t}adaw{nch % 2}"
        P.load(wb, wv[:, :, nch * 512:(nch + 1) * 512], writes=[wk])
        bank = 6 + nch % 2
        for kc in range(8):
            P.mm(ps[:, bank, :], condbc[:, kc, :], wb[:, kc, :],
                 start=(kc == 0), stop=(kc == 7), reads=[wk, t + "condbc"], writes=[f"ps{bank}"])
        sl = slice(nch * 512, (nch + 1) * 512)
        P.tt("dve", modbc[:, sl], ps[:, bank, :], adab[:, sl], ALU.add,
             reads=[f"ps{bank}", t + "adab"], writes=[t + "modbc"])
    sh1, sc1, g1, sh2, sc2, g2 = [modbc[:, i * D:(i + 1) * D] for i in range(6)]
    tmpA = adab[:, 0:D]
    P.stt("dve", tmpA, sc1, 1.0, nm, ALU.add, ALU.mult, reads=[t + "modbc", t + "nm"], writes=[t + "adab"])
    diag_extract(cx, A1, tmpA, [t + "adab"], t + "A1")
    diag_extract(cx, B1, sh1, [t + "modbc"], t + "B1")
    tmpA2 = adab[:, D:2 * D]
    P.stt("dve", tmpA2, sc2, 1.0, nf, ALU.add, ALU.mult, reads=[t + "modbc", t + "nf"], writes=[t + "adab2"])
    diag_extract(cx, A2, tmpA2, [t + "adab2"], t + "A2")
    diag_extract(cx, B2, sh2, [t + "modbc"], t + "B2")
    P.cp("dve", g1bc, g1, reads=[t + "modbc"], writes=[t + "g1bc"])
    P.cp("dve", g2bc, g2, reads=[t + "modbc"], writes=[t + "g2bc"])
    P.fence()
    A.release(m)
    cx.A1, cx.B1, cx.A2, cx.B2, cx.g1bc, cx.g2bc = A1, B1, A2, B2, g1bc, g2bc
    cx.modtag = t


def norm_scratch(cx):
    A = cx.A
    cx.n_junk = A.alloc([D], BF16)
    cx.n_xn = [A.alloc([D], BF16) for _ in range(4)]
    cx.n_st = [A.alloc([4, 4], F32) for _ in range(2)]
    cx.n_tmp = [A.alloc([8, 128], BF16) for _ in range(2)]
    cx.n_i = 0
    cx.n_g = 0


def norm_T_multi(cx, xts, xkeys, Avec, Bvec, vkeys, dsts, dkeys):
    P, ps = cx.P, cx.ps
    n = len(xts)
    g = cx.n_g % 2
    cx.n_g += 1
    st = cx.n_st[g]
    sk = f"nst{g}"
    for j in range(n):
        P.act(cx.n_junk, xts[j], AF.Square, reads=[xkeys[j]], writes=["njunk", sk], accum_out=st[:, 0, j:j + 1])
    P.ts("dve", st[:, 1, 0:n], st[:, 0, 0:n], 1.0 / D, EPS, ALU.mult, ALU.add, reads=[sk], writes=[sk])
    P.act(st[:, 2, 0:n], st[:, 1, 0:n], AF.Sqrt, reads=[sk], writes=[sk])
    P.op("dve", lambda e: e.reciprocal(st[:, 3, 0:n], st[:, 2, 0:n]), reads=[sk], writes=[sk])
    for j in range(n):
        xn = cx.n_xn[j]
        xk = f"nxn{j}"
        P.act(xn, xts[j], AF.Copy, reads=[xkeys[j], sk], writes=[xk], scale=st[:, 3, j:j + 1])
        i = cx.n_i % 2
        cx.n_i += 1
        tmp = cx.n_tmp[i]
        tk = f"ntmp{i}"
        bank = 6 + i
        pst = ps[:, bank, :].bitcast(BF16).rearrange("p (a b) -> p a b", b=128)
        for kc in range(8):
            P.tr(pst[:, kc, :], xn[:, kc * 128:(kc + 1) * 128], cx.ident, reads=[xk, "ident"], writes=[f"ps{bank}"])
        P.tt("dve", tmp, pst, bc_mid(Avec, 128), ALU.mult, reads=[f"ps{bank}"] + list(vkeys), writes=[tk])
        P.tt("pool", dsts[j], tmp, bc_mid(Bvec, 128), ALU.add, reads=[tk] + list(vkeys), writes=[dkeys[j]])


def norm_T(cx, xt, xkey, Avec, Bvec, vkeys, dst, dkey):
    norm_T_multi(cx, [xt], [xkey], Avec, Bvec, vkeys, [dst], [dkey])


def transpose_tok(cx, src, skey, dst, dkey, eng="dve"):
    P, ps = cx.P, cx.ps
    i = cx.n_i % 2
    cx.n_i += 1
    bank = 6 + i
    pst = ps[:, bank, :].bitcast(BF16).rearrange("p (a b) -> p a b", b=128)
    for kc in range(8):
        P.tr(pst[:, kc, :], src[:, kc * 128:(kc + 1) * 128], cx.ident, reads=[skey, "ident"], writes=[f"ps{bank}"])
    P.cp(eng, dst, pst, reads=[f"ps{bank}"], writes=[dkey])


def proj_residual(cx, srcT, skeys, wp, wkey):
    P, ps = cx.P, cx.ps
    it = 0
    for t in range(NT):
        for half in range(2):
            bank = 4 + (it % 2)
            it += 1
            for kc in range(8):
                P.mm(ps[:, bank, :], srcT[:, kc, t * 128:(t + 1) * 128], wp[:, kc, half * 512:(half + 1) * 512],
                     start=(kc == 0), stop=(kc == 7), reads=list(skeys) + [wkey], writes=[f"ps{bank}"])
            xs = cx.xres[:, t, half * 512:(half + 1) * 512]
            P.tt("dve", xs, ps[:, bank, :], xs, ALU.add, reads=[f"ps{bank}"], writes=[f"xres{t}"])


def stage_moe(cx, wr_d, br_d, wg_d, wu_d, wd_d, tag):
    P, A, ps = cx.P, cx.A, cx.ps
    t_ = tag
    m = A.mark()
    h2T = A.alloc([8, TOK], BF16)
    norm_scratch(cx)
    vk = [cx.modtag + "A2", cx.modtag + "B2"]
    for t0 in range(0, NT, 4):
        ts4 = list(range(t0, t0 + 4))
        norm_T_multi(cx, [cx.xres[:, t, :] for t in ts4], [f"xres{t}" for t in ts4], cx.A2, cx.B2, vk,
                     [h2T[:, :, t * 128:(t + 1) * 128] for t in ts4], [f"{t_}h2T{t}" for t in ts4])
    h2keys = [f"{t_}h2T{t}" for t in range(NT)]
    wr = A.alloc([8, 20], BF16)
    brbc = A.alloc([20], F32)
    P.load(wr, wr_d, writes=[t_ + "wr"], eng="pool")
    P.load(brbc, br_d.partition_broadcast(128), writes=[t_ + "br"])
    L = A.alloc([NT, 20], F32)
    rbank = 5
    Lps = ps[:, rbank, :].rearrange("p (a b) -> p a b", b=32)[:, :, 0:20]
    for t in range(NT):
        for kc in range(8):
            P.mm(Lps[:, t, :], h2T[:, kc, t * 128:(t + 1) * 128], wr[:, kc, :], start=(kc == 0), stop=(kc == 7),
                 reads=[f"{t_}h2T{t}", t_ + "wr"], writes=[f"ps{rbank}"])
    P.tt("dve", L, Lps, brbc[:, None, :].broadcast_to([128, NT, 20]), ALU.add,
         reads=[f"ps{rbank}", t_ + "br"], writes=[t_ + "L"])
    Lg = L[:, :, 0:4]
    Le = L[:, :, 4:20].rearrange("p t (g e) -> p t g e", e=4)
    gmax = A.alloc([NT], F32); gsum = A.alloc([NT], F32); gw = A.alloc([NT], F32)
    ohg = A.alloc([NT, 4], F32); eg = A.alloc([NT, 4], F32)
    tmp44 = A.alloc([NT, 4, 4], F32)
    esel = A.alloc([NT, 4], F32); e2 = A.alloc([NT, 4], F32)
    m1 = A.alloc([NT], F32); m2 = A.alloc([NT], F32)
    mk1 = A.alloc([NT, 4], F32); mk2 = A.alloc([NT, 4], F32)
    dd = A.alloc([NT], F32); w1 = A.alloc([NT], F32); w2 = A.alloc([NT], F32)
    ew = A.alloc([NT, 4], F32)
    gates = A.alloc([NT, 4, 4], F32)
    rk = t_ + "rt"

    def bcl(ap, n):
        return ap[:, :, None].broadcast_to([128, NT, n])

    P.op("dve", lambda e: e.tensor_reduce(gmax, Lg, AX.X, ALU.max), reads=[t_ + "L"], writes=[rk])
    P.tt("dve", ohg, Lg, bcl(gmax, 4), ALU.is_equal, reads=[rk, t_ + "L"], writes=[rk])
    P.tt("dve", eg, Lg, bcl(gmax, 4), ALU.subtract, reads=[rk, t_ + "L"], writes=[rk])
    P.act(eg, eg, AF.Exp, reads=[rk], writes=[rk])
    P.op("dve", lambda e: e.tensor_reduce(gsum, eg, AX.X, ALU.add), reads=[rk], writes=[rk])
    P.op("dve", lambda e: e.reciprocal(gw, gsum), reads=[rk], writes=[rk])
    P.tt("dve", tmp44, Le, ohg[:, :, :, None].broadcast_to([128, NT, 4, 4]), ALU.mult, reads=[rk, t_ + "L"], writes=[rk])
    P.op("dve", lambda e: e.tensor_reduce(esel, tmp44.rearrange("p t g e -> p t e g"), AX.X, ALU.add), reads=[rk], writes=[rk])
    P.op("dve", lambda e: e.tensor_reduce(m1, esel, AX.X, ALU.max), reads=[rk], writes=[rk])
    P.tt("dve", mk1, esel, bcl(m1, 4), ALU.is_equal, reads=[rk], writes=[rk])
    P.stt("dve", e2, mk1, -1e30, esel, ALU.mult, ALU.add, reads=[rk], writes=[rk])
    P.op("dve", lambda e: e.tensor_reduce(m2, e2, AX.X, ALU.max), reads=[rk], writes=[rk])
    P.tt("dve", mk2, e2, bcl(m2, 4), ALU.is_equal, reads=[rk], writes=[rk])
    P.tt("dve", dd, m2, m1, ALU.subtract, reads=[rk], writes=[rk])
    P.act(dd, dd, AF.Exp, reads=[rk], writes=[rk])
    P.ts("dve", w1, dd, 1.0, None, ALU.add, reads=[rk], writes=[rk])
    P.op("dve", lambda e: e.reciprocal(w1, w1), reads=[rk], writes=[rk])
    P.tt("dve", w2, dd, w1, ALU.mult, reads=[rk], writes=[rk])
    P.tt("dve", w1, w1, gw, ALU.mult, reads=[rk], writes=[rk])
    P.tt("dve", w2, w2, gw, ALU.mult, reads=[rk], writes=[rk])
    P.tt("dve", ew, mk1, bcl(w1, 4), ALU.mult, reads=[rk], writes=[rk])
    P.tt("dve", mk2, mk2, bcl(w2, 4), ALU.mult, reads=[rk], writes=[rk])
    P.tt("dve", ew, ew, mk2, ALU.add, reads=[rk], writes=[rk])
    P.tt("dve", gates, ohg[:, :, :, None].broadcast_to([128, NT, 4, 4]),
         ew[:, :, None, :].broadcast_to([128, NT, 4, 4]), ALU.mult, reads=[rk], writes=[t_ + "gates"])
    gflat = gates.rearrange("p t g e -> p t (g e)")
    if "L" in DBG and t_ == "e0":
        P.load(DBG["L"].rearrange("(t p) d -> p t d", p=128), L, reads=[t_ + "L"], writes=["dbg_L"])
        P.load(DBG["gates"].rearrange("(t p) d -> p t d", p=128), gflat, reads=[t_ + "gates"], writes=["dbg_g"])
        P.load(DBG["xmid"].rearrange("(t p) d -> p t d", p=128), cx.xres, reads=[f"xres{t}" for t in range(NT)], writes=["dbg_x"])
    wgb = [A.alloc([8, DE], BF16) for _ in range(2)]
    wub = [A.alloc([8, DE], BF16) for _ in range(2)]
    wdb = [A.alloc([4, D], BF16) for _ in range(2)]
    sil = [A.alloc([512], BF16) for _ in range(2)]
    hid = [A.alloc([4, 512], BF16) for _ in range(2)]
    it_gu = 0
    it_y = 0
    it_h = 0
    for e in range(NE):
        bi = e % 2
        kg, ku, kd = f"{t_}wg{bi}", f"{t_}wu{bi}", f"{t_}wd{bi}"
        P.load(wgb[bi], wg_d[e].rearrange("(kc p) n -> p kc n", p=128), writes=[kg], eng="pool")
        P.load(wub[bi], wu_d[e].rearrange("(kc p) n -> p kc n", p=128), writes=[ku], eng="pool")
        P.load(wdb[bi], wd_d[e].rearrange("(kc p) n -> p kc n", p=128), writes=[kd], eng="pool")
        P.tt("pool", wdb[bi], wdb[bi], cx.g2bc[:, None, :].broadcast_to([128, 4, D]), ALU.mult,
             reads=[kd, cx.modtag + "g2bc"], writes=[kd])
        for c in range(4):
            hb = it_h % 2
            it_h += 1
            hk = f"{t_}hid{hb}"
            ckeys = [f"{t_}h2T{t}" for t in range(4 * c, 4 * c + 4)]
            for fc in range(4):
                gb = (it_gu % 2) * 2
                it_gu += 1
                for kc in range(8):
                    P.mm(ps[:, gb, :], wgb[bi][:, kc, fc * 128:(fc + 1) * 128], h2T[:, kc, c * 512:(c + 1) * 512],
                         start=(kc == 0), stop=(kc == 7), reads=[kg] + ckeys, writes=[f"ps{gb}"])
                for kc in range(8):
                    P.mm(ps[:, gb + 1, :], wub[bi][:, kc, fc * 128:(fc + 1) * 128], h2T[:, kc, c * 512:(c + 1) * 512],
                         start=(kc == 0), stop=(kc == 7), reads=[ku] + ckeys, writes=[f"ps{gb + 1}"])
                sb_ = sil[it_gu % 2]
                sk = f"{t_}sil{it_gu % 2}"
                P.act(sb_, ps[:, gb, :], AF.Silu, reads=[f"ps{gb}"], writes=[sk])
                P.tt("dve", hid[hb][:, fc, :], ps[:, gb + 1, :], sb_, ALU.mult, reads=[f"ps{gb + 1}", sk], writes=[hk])
            for ts_ in range(4):
                t = 4 * c + ts_
                for half in range(2):
                    yb = 4 + (it_y % 4)
                    it_y += 1
                    for fc in range(4):
                        P.mm(ps[:, yb, :], hid[hb][:, fc, ts_ * 128:(ts_ + 1) * 128], wdb[bi][:, fc, half * 512:(half + 1) * 512],
                             start=(fc == 0), stop=(fc == 3), reads=[hk, kd], writes=[f"ps{yb}"])
                    xs = cx.xres[:, t, half * 512:(half + 1) * 512]
                    P.stt("dve", xs, ps[:, yb, :], gflat[:, t, e:e + 1], xs, ALU.mult, ALU.add,
                          reads=[f"ps{yb}", t_ + "gates"], writes=[f"xres{t}"])
    P.fence()
    A.release(m)


def layer0_body(cx, xp, w_in, w_out, lam, subln, kaug, qaug, dfix, kscr, qscr, vscr, wr, br, wg, wu, wd):
    P, A, ps = cx.P, cx.A, cx.ps
    mt = cx.modtag
    m_qkv = A.mark()
    win = A.alloc([8, 3 * D], BF16)
    wv = w_in.rearrange("(kc p) n -> p kc n", p=128)
    for i in range(3):
        P.load(win[:, :, i * D:(i + 1) * D], wv[:, :, i * D:(i + 1) * D], writes=[f"win{i}"], eng="pool")
    norm_scratch(cx)
    xt = [A.alloc([D], F32) for _ in range(6)]
    hTc = [A.alloc([8, 512], BF16) for _ in range(2)]
    stgp = [A.alloc([2, H, 512], BF16) for _ in range(2)]
    istg = 0
    vst = [A.alloc([4, H, 129], BF16) for _ in range(2)]
    for i in range(2):
        P.memset("pool", vst[i][:, :, :, 128:129], 1.0, writes=[f"vst{i}"])
    xv = xp.rearrange("(t p) d -> t p d", p=128)
    kscr_v = kscr.rearrange("s h d t -> d s h t")
    qscr_v = qscr.rearrange("s h d t -> d s h t")
    vscr_v = vscr.rearrange("t p h c -> p t h c")
    vk1 = [mt + "A1", mt + "B1"]
    ixt = 0
    ipb = 0
    qkv_state = {"ixt": 0, "istg": 0, "ipb": 0}

    def emit_norm(ch):
        ixt = qkv_state["ixt"]
        cb = ch % 2
        hk = f"hTc{cb}"
        xbs, xks = [], []
        for t in range(4):
            xb = xt[ixt % 6]
            xk = f"xt{ixt % 6}"
            ixt += 1
            P.load(xb, xv[ch * 4 + t], writes=[xk])
            xbs.append(xb)
            xks.append(xk)
        norm_T_multi(cx, xbs, xks, cx.A1, cx.B1, vk1,
                     [hTc[cb][:, :, t * 128:(t + 1) * 128] for t in range(4)], [hk] * 4)
        qkv_state["ixt"] = ixt

    def emit_mm(ch):
        cb = ch % 2
        hk = f"hTc{cb}"
        istg = qkv_state["istg"]
        ipb = qkv_state["ipb"]
        for which in ([1, 0] if ch < 4 else [1]):
            stg = stgp[istg % 2]
            sk = f"stg{istg % 2}"
            istg += 1
            for hb in range(H):
                bank = ipb % 4
                ipb += 1
                col = which * D + hb * 128
                for kc in range(8):
                    P.mm(ps[:, bank, :], win[:, kc, col:col + 128], hTc[cb][:, kc, :], start=(kc == 0), stop=(kc == 7),
                         reads=[f"win{which}", hk], writes=[f"ps{bank}"])
                if which == 1:
                    P.cp("act", stg[0:64, 0, hb, :], ps[0:64, bank, :], reads=[f"ps{bank}"], writes=[sk])
                    P.cp("dve", stg[0:64, 1, hb, :], ps[64:128, bank, :], reads=[f"ps{bank}"], writes=[sk])
                else:
                    P.op("act", lambda e, o=stg[0:64, 0, hb, :], i_=ps[0:64, bank, :]: e.mul(o, i_, 0.125),
                         reads=[f"ps{bank}"], writes=[sk])
                    P.ts("dve", stg[0:64, 1, hb, :], ps[64:128, bank, :], 0.125, None, ALU.mult,
                         reads=[f"ps{bank}"], writes=[sk])
            if which == 1:
                P.load(kscr_v[:, :, :, ch * 512:(ch + 1) * 512], stg[0:64], reads=[sk], writes=[f"kscr{ch}"])
            else:
                P.load(qscr_v[:, :, :, ch * 512:(ch + 1) * 512], stg[0:64], reads=[sk], writes=[f"qscr{ch}"])
        for t in range(4):
            for half in range(2):
                bank = ipb % 4
                ipb += 1
                for kc in range(8):
                    P.mm(ps[:, bank, :], hTc[cb][:, kc, t * 128:(t + 1) * 128],
                         win[:, kc, 2 * D + half * 512:2 * D + (half + 1) * 512], start=(kc == 0), stop=(kc == 7),
                         reads=["win2", hk], writes=[f"ps{bank}"])
                eng = "act" if half == 0 else "dve"
                P.cp(eng, vst[cb][:, t, half * 4:(half + 1) * 4, 0:128],
                     ps[:, bank, :].rearrange("p (h c) -> p h c", c=128), reads=[f"ps{bank}"], writes=[f"vst{cb}"])
        P.load(vscr_v[:, ch * 4:(ch + 1) * 4], vst[cb], reads=[f"vst{cb}"], writes=[f"vscr{ch}"])

        qkv_state["istg"] = istg
        qkv_state["ipb"] = ipb

    emit_norm(0)
    for ch in range(S // 512):
        if ch + 1 < S // 512:
            emit_norm(ch + 1)
        emit_mm(ch)
    P.fence()
    A.release(m_qkv)
    cx.xres = A.alloc([NT, D], F32)
    xres_off = A.last
    m_att = A.mark()
    obf = A.alloc([NT, D], BF16)
    ssq = A.alloc([NT, H], F32)
    lamv = A.alloc([4, 64], F32)
    lamt = A.alloc([2, 64], F32)
    lams = A.alloc([4], F32)
    neglam = A.alloc([1], F32)
    dfx = A.alloc([H, 128], BF16)
    P.load(dfx, dfix, writes=["dfx"])
    P.load(lamv, lam.rearrange("a b -> (a b)").partition_broadcast(128).rearrange("p (a b) -> p a b", b=64), writes=["lamv"])
    P.tt("dve", lamt, lamv[:, 0:4:2, :], lamv[:, 1:4:2, :], ALU.mult, reads=["lamv"], writes=["lamt"])
    P.op("dve", lambda e: e.tensor_reduce(lams[:, 0:2], lamt, AX.X, ALU.add), reads=["lamt"], writes=["lams"])
    P.act(lams[:, 2:4], lams[:, 0:2], AF.Exp, reads=["lams"], writes=["lams"])
    P.tt("dve", neglam, lams[:, 3:4], lams[:, 2:3], ALU.subtract, reads=["lams"], writes=["neglam"])
    P.ts("dve", neglam, neglam, -LAM_INIT0, None, ALU.add, reads=["neglam"], writes=["neglam"])
    ktb = [A.alloc([2, S], BF16)]
    kt_off = A.last
    ktb.append(A.view(xres_off, [2, S], BF16))
    qtb = [A.alloc([2, TOK], BF16) for _ in range(2)]
    qt_off = A.last - (2 * TOK) // 2
    vtb = A.alloc([S // 128, 2, 129], BF16)
    pT = [A.alloc([1024], BF16) for _ in range(3)]
    o32 = [A.alloc([128], F32) for _ in range(4)]
    t32 = [A.alloc([128], F32) for _ in range(1)]
    rr = [A.alloc([4], F32) for _ in range(4)]
    junk = A.alloc([128], BF16)
    vscr_hp = vscr.rearrange("t p (hp hh) c -> p t hp hh c", hh=2)
    NKT = S // 128
    state = {"isb": 0, "ipt": 0, "iep": 0}

    def load_head(h):
        kb = h % 2
        P.load(ktb[kb][0:64], kscr_v[:, :, h, :], writes=[f"ktb{kb}"])
        P.load(ktb[kb][64:72], kaug[h], writes=[f"ktb{kb}"])
        P.load(qtb[kb][0:64], qscr_v[:, :, h, :], writes=[f"qtb{kb}"])
        P.load(qtb[kb][64:72], qaug, writes=[f"qtb{kb}"])

    def load_v(hp):
        for q4 in range(4):
            P.load(vtb[:, q4 * 16:(q4 + 1) * 16], vscr_hp[:, q4 * 16:(q4 + 1) * 16, hp], writes=["vtb"])

    def emit_qk_exp(h, qc, kt):
        kb = h % 2
        kk, qk = f"ktb{kb}", f"qtb{kb}"
        K_, Q_ = ktb[kb], qtb[kb]
        sb0 = 2 * (state["isb"] % 2)
        state["isb"] += 1
        for s_ in range(2):
            bank = sb0 + s_
            outp = ps[:, bank, :]
            kcols = slice(kt * 128, (kt + 1) * 128)
            q0 = qc * 512
            rd = [kk, qk]
            wkey = [f"ps{bank}"]
            if kt >= NT or kt > 4 * qc + 3:
                P.mm(outp, K_[0:72, s_, kcols], Q_[0:72, s_, q0:q0 + 512], reads=rd, writes=wkey)
            elif kt < 4 * qc:
                P.mm(outp, K_[0:68, s_, kcols], Q_[0:68, s_, q0:q0 + 512], reads=rd, writes=wkey)
            else:
                i_ = kt - 4 * qc
                if i_ > 0:
                    P.mm(outp[:, 0:i_ * 128], K_[0:72, s_, kcols], Q_[0:72, s_, q0:q0 + i_ * 128], reads=rd, writes=wkey)
                dsl = slice(i_ * 128, (i_ + 1) * 128)
                P.mm(outp[:, dsl], K_[0:68, s_, kcols], Q_[0:68, s_, q0 + i_ * 128:q0 + (i_ + 1) * 128],
                     start=True, stop=False, reads=rd, writes=wkey, skip_group_check=True)
                P.mm(outp[:, dsl], cx.ident, dfx[:, h, :], start=False, stop=True,
                     reads=["ident", "dfx"], writes=wkey, skip_group_check=True)
                if i_ < 3:
                    P.mm(outp[:, (i_ + 1) * 128:512], K_[0:68, s_, kcols], Q_[0:68, s_, q0 + (i_ + 1) * 128:q0 + 512],
                         reads=rd, writes=wkey, skip_group_check=True)
        pi_ = state["ipt"] % 3
        state["ipt"] += 1
        P.act(pT[pi_], ps[:, sb0:sb0 + 2, :].rearrange("p a b -> p (a b)"), AF.Exp,
              reads=[f"ps{sb0}", f"ps{sb0 + 1}"], writes=[f"pT{pi_}"])
        return pi_

    def emit_pv(h, qc, kt, pi_):
        hh = h % 2
        pb = pT[pi_]
        for s_ in range(2):
            for qs in range(4):
                a = s_ * 4 + qs
                bank = 4 + a // 3
                off = (a % 3) * 132
                P.mm(ps[:, bank, off:off + 129], pb[:, s_ * 512 + qs * 128:s_ * 512 + (qs + 1) * 128],
                     vtb[:, kt, hh, :], start=(kt == 0 and a % 3 == 0), stop=(kt == NKT - 1),
                     reads=[f"pT{pi_}", "vtb"], writes=[f"oacc{a}"], skip_group_check=True)
        if kt == NKT - 1:
            for qs in range(4):
                t = 4 * qc + qs
                eb = state["iep"] % 4
                state["iep"] += 1
                a1, a2 = qs, 4 + qs
                acc1 = ps[:, 4 + a1 // 3, (a1 % 3) * 132:(a1 % 3) * 132 + 129]
                acc2 = ps[:, 4 + a2 // 3, (a2 % 3) * 132:(a2 % 3) * 132 + 129]
                rk = f"rr{eb}"
                g1 = [f"oacc{i}" for i in range(8) if i // 3 == a1 // 3]
                g2 = [f"oacc{i}" for i in range(8) if i // 3 == a2 // 3]
                P.op("dve", lambda e, o=rr[eb][:, 0:1], i_=acc1[:, 128:129]: e.reciprocal(o, i_), reads=g1, writes=[rk])
                P.op("dve", lambda e, o=rr[eb][:, 1:2], i_=acc2[:, 128:129]: e.reciprocal(o, i_), reads=g2, writes=[rk])
                P.tt("dve", rr[eb][:, 2:3], rr[eb][:, 1:2], neglam, ALU.mult, reads=[rk, "neglam"], writes=[rk])
                P.ts("dve", t32[0], acc2[:, 0:128], rr[eb][:, 2:3], None, ALU.mult, reads=g2 + [rk], writes=["t320"])
                P.stt("dve", o32[eb], acc1[:, 0:128], rr[eb][:, 0:1], t32[0], ALU.mult, ALU.add,
                      reads=g1 + [rk, "t320"], writes=[f"o32{eb}"])
                P.tt("dve", t32[0], o32[eb], o32[eb], ALU.mult, reads=[f"o32{eb}"], writes=["t320"])
                P.op("dve", lambda e, o=ssq[:, t, h:h + 1], i_=t32[0]: e.tensor_reduce(o, i_, AX.X, ALU.add),
                     reads=["t320"], writes=["ssq"])
                P.cp("pool", obf[:, t, h * 128:(h + 1) * 128], o32[eb], reads=[f"o32{eb}"], writes=[f"obf{t}"])
            if qc == 3 and h % 2 == 1 and h + 1 < H:
                load_v((h + 1) // 2)

    load_v(0)
    pend = []
    load_head(0)
    for h in range(H):
        if h + 1 < H:
            load_head(h + 1)
        for qc in range(4):
            for kt in range(NKT):
                pi_ = emit_qk_exp(h, qc, kt)
                pend.append((h, qc, kt, pi_))
                if len(pend) > 2:
                    emit_pv(*pend.pop(0))
    while pend:
        emit_pv(*pend.pop(0))
    P.ts("dve", ssq, ssq, 1.0 / 128, EPS, ALU.mult, ALU.add, reads=["ssq"], writes=["ssq"])
    P.act(ssq, ssq, AF.Sqrt, reads=["ssq"], writes=["ssq"])
    P.op("dve", lambda e: e.reciprocal(ssq, ssq), reads=["ssq"], writes=["ssq"])
    for t in range(NT):
        ov = obf[:, t, :].rearrange("p (h c) -> p h c", c=128)
        P.tt("dve", ov, ov, ssq[:, t, :][:, :, None].broadcast_to([128, H, 128]), ALU.mult,
             reads=["ssq", f"obf{t}"], writes=[f"obf{t}"])
    if "obf" in DBG:
        P.load(DBG["obf"].rearrange("(t p) d -> p t d", p=128), obf, reads=[f"obf{t}" for t in range(NT)], writes=["dbg_obf"])
    P.fence()
    P.load(cx.xres, xp[0:TOK].rearrange("(t p) d -> p t d", p=128), writes=[f"xres{t}" for t in range(NT)])
    oT = A.view(kt_off, [8, TOK], BF16)
    wob = A.view(qt_off, [8, D], BF16)
    sub_s = A.alloc([1], F32)
    P.load(sub_s, subln, writes=["subs"])
    P.ts("dve", sub_s, sub_s, 1.0 - LAM_INIT0, None, ALU.mult, reads=["subs"], writes=["subs"])
    P.load(wob, w_out.rearrange("(kc p) n -> p kc n", p=128), writes=["wob"], eng="pool")
    for kc in range(8):
        P.ts("pool", wob[:, kc, :], wob[:, kc, :], sub_s[:, 0:1], None, ALU.mult,
             reads=["wob", "subs"], writes=["wob"])
        P.tt("pool", wob[:, kc, :], wob[:, kc, :], cx.g1bc, ALU.mult,
             reads=["wob", mt + "g1bc"], writes=["wob"])
    for t in range(NT):
        transpose_tok(cx, obf[:, t, :], f"obf{t}", oT[:, :, t * 128:(t + 1) * 128], f"oT{t}", eng=("dve" if t % 2 else "act"))
    proj_residual(cx, oT, [f"oT{t}" for t in range(NT)], wob, "wob")
    P.fence()
    A.release(m_att)
    stage_moe(cx, wr, br, wg, wu, wd, "e0")


def build_A():
    nc = bass.Bass("TRN2", target_bir_lowering=False)
    dram = lambda n, s, dt=F32, kind="ExternalInput": nc.dram_tensor(n, s, dt, kind=kind).ap()
    xp = dram("xp", [S, D])
    c_d = dram("c", [128, 8])
    adaw = dram("ada_w", [D, 6 * D]); adab = dram("ada_b", [6 * D])
    nmix = dram("norm_mix", [D]); nffn = dram("norm_ffn", [D])
    w_in = dram("w_in", [D, 3 * D]); w_out = dram("w_out", [D, D])
    lam = dram("lam", [4, 64]); subln = dram("subln", [128, 1])
    kaug = dram("kaug", [H, 8, 2, S], BF16); qaug = dram("qaug", [8, 2, TOK], BF16)
    dfix = dram("dfix", [128, H, 128], BF16)
    ident_d = dram("ident", [128, 128], BF16); identf_d = dram("identf", [128, 128])
    wr = dram("wr", [128, 8, 20]); br = dram("br", [20])
    wg = dram("wg", [NE, D, DE]); wu = dram("wu", [NE, D, DE]); wd = dram("wd", [NE, DE, D])
    xo = dram("xo", [TOK, D], F32, "ExternalOutput")
    if DBG.get("on"):
        DBG["obf"] = dram("d_obf", [TOK, D], BF16, "ExternalOutput")
        DBG["L"] = dram("d_L", [TOK, 20], F32, "ExternalOutput")
        DBG["gates"] = dram("d_gates", [TOK, 16], F32, "ExternalOutput")
        DBG["xmid"] = dram("d_xmid", [TOK, D], F32, "ExternalOutput")
    kscr = dram("kscr", [2, H, 64, S], BF16, "Internal")
    qscr = dram("qscr", [2, H, 64, TOK], BF16, "Internal")
    vscr = dram("vscr", [S // 128, 128, H, 129], BF16, "Internal")

    P = Prog(nc)
    with contextlib.ExitStack() as st:
        cx = Ctx()
        cx.P = P
        cx.A = A = Arena(nc, st)
        cx.ps = ps = st.enter_context(nc.psum_tensor("ps", [128, 8, 512], F32))
        setup_common(cx, ident_d, identf_d)
        stage_mod(cx, c_d, adaw, adab, nmix, nffn, "m0")
        mt = cx.modtag
        layer0_body(cx, xp, w_in, w_out, lam, subln, kaug, qaug, dfix, kscr, qscr, vscr, wr, br, wg, wu, wd)
        P.load(xo.rearrange("(t p) d -> p t d", p=128), cx.xres, reads=[f"xres{t}" for t in range(NT)], writes=["xo"])
        P.emit()
    return nc


def build_B():
    nc = bass.Bass("TRN2", target_bir_lowering=False)
    dram = lambda n, s, dt=F32, kind="ExternalInput": nc.dram_tensor(n, s, dt, kind=kind).ap()
    x1 = dram("x1", [S, D])
    c_d = dram("c", [128, 8])
    adaw = dram("ada_w", [D, 6 * D]); adab = dram("ada_b", [6 * D])
    nmix = dram("norm_mix", [D]); nffn = dram("norm_ffn", [D])
    fwin = dram("fwin", [D, 256])
    cs_d = dram("cs", [128, 2, 512], BF16)
    r1_d = dram("r1", [128, 256], BF16); r2_d = dram("r2", [128, 256], BF16)
    twr_d = dram("twr", [128, 128]); twi_d = dram("twi", [128, 128])
    bdc_d = dram("bdc", [128, 128], BF16); bds_d = dram("bds", [128, 128], BF16)
    ident_d = dram("ident", [128, 128], BF16); identf_d = dram("identf", [128, 128])
    fo = dram("fo", [S, 256], BF16, "ExternalOutput")
    P = Prog(nc)
    with contextlib.ExitStack() as st:
        cx = Ctx()
        cx.P = P
        cx.A = A = Arena(nc, st)
        cx.ps = ps = st.enter_context(nc.psum_tensor("ps", [128, 8, 512], F32))
        setup_common(cx, ident_d, identf_d)
        stage_mod(cx, c_d, adaw, adab, nmix, nffn, "m1")
        mt = cx.modtag
        cs = A.alloc([2, 512], BF16); r1 = A.alloc([256], BF16); r2 = A.alloc([256], BF16)
        twr = A.alloc([128], F32); twi = A.alloc([128], F32)
        bdc = A.alloc([128], BF16); bds = A.alloc([128], BF16)
        for ap_, d_, k_ in [(cs, cs_d, "cs"), (r1, r1_d, "r1"), (r2, r2_d, "r2"), (twr, twr_d, "twr"),
                            (twi, twi_d, "twi"), (bdc, bdc_d, "bdc"), (bds, bds_d, "bds")]:
            P.load(ap_, d_, writes=[k_])
        uT = A.alloc([2, S], BF16)
        m2 = A.mark()
        winb = A.alloc([8, 256], BF16)
        P.load(winb, fwin.rearrange("(kc p) n -> p kc n", p=128), writes=["winb"], eng="pool")
        norm_scratch(cx)
        xt = [A.alloc([D], F32) for _ in range(3)]
        hTc = [A.alloc([8, 512], BF16) for _ in range(2)]
        xv = x1.rearrange("(t p) d -> t p d", p=128)
        vk1 = [mt + "A1", mt + "B1"]
        ixt = 0
        ipb = 0
        for ch in range(S // 512):
            cb = ch % 2
            hk = f"hTc{cb}"
            for t in range(4):
                xb = xt[ixt % 3]
                xk = f"xt{ixt % 3}"
                ixt += 1
                P.load(xb, xv[ch * 4 + t], writes=[xk])
                norm_T(cx, xb, xk, cx.A1, cx.B1, vk1, hTc[cb][:, :, t * 128:(t + 1) * 128], hk)
            for mc in range(2):
                bank = ipb % 4
                ipb += 1
                for kc in range(8):
                    P.mm(ps[:, bank, :], winb[:, kc, mc * 128:(mc + 1) * 128], hTc[cb][:, kc, :], start=(kc == 0), stop=(kc == 7),
                         reads=["winb", hk], writes=[f"ps{bank}"])
                P.cp("act" if mc == 0 else "dve", uT[:, mc, ch * 512:(ch + 1) * 512], ps[:, bank, :],
                     reads=[f"ps{bank}"], writes=[f"uT{ch}"])
        P.fence()
        A.release(m2)
        zsb = A.alloc([2, 256, 64], BF16)
        zflat = zsb.rearrange("p r c n -> p (r c) n")
        for n2 in range(64):
            bank = n2 % 4
            for mc in range(2):
                P.mm(ps[:, bank, :], uT[:, mc, n2:S:64], cs[:, mc, :], start=(mc == 0), stop=(mc == 1),
                     reads=["cs"], writes=[f"ps{bank}"])
            P.cp("act" if n2 % 2 == 0 else "dve", zflat[:, :, n2], ps[:, bank, :], reads=[f"ps{bank}"], writes=["zsb"])
        P.fence()
        fsb = A.alloc([64, 256], BF16)
        p1 = [A.alloc([4, 2, 128], F32) for _ in range(2)]
        p2 = [A.alloc([4, 2, 128], F32) for _ in range(2)]
        apr = [A.alloc([4, 128], BF16) for _ in range(2)]
        api = [A.alloc([4, 128], BF16) for _ in range(2)]
        twr_b = twr[:, None, None, :].broadcast_to([128, 4, 2, 128])
        twi_b = twi[:, None, None, :].broadcast_to([128, 4, 2, 128])
        for grp in range(32):
            gb = grp % 2
            ab = 2 * gb
            for pi in range(4):
                pp = grp * 4 + pi
                bank = ab + pi // 2
                out = ps[:, bank, (pi % 2) * 256:(pi % 2) * 256 + 256]
                P.mm(out, zsb[:, 0, 2 * pp:2 * pp + 2, :].rearrange("p c n -> p (c n)"), r1, start=True, stop=False,
                     reads=["r1"], writes=[f"ps{bank}"], skip_group_check=True)
                P.mm(out, zsb[:, 1, 2 * pp:2 * pp + 2, :].rearrange("p c n -> p (c n)"), r2, start=False, stop=True,
                     reads=["r2"], writes=[f"ps{bank}"], skip_group_check=True)
            Av = ps[:, ab:ab + 2, :].rearrange("p a (b r k) -> p (a b) r k", r=2, k=128)
            P.tt("dve", p1[gb], Av, twr_b, ALU.mult, reads=[f"ps{ab}", f"ps{ab + 1}", "twr"], writes=[f"p1{gb}"])
            P.tt("dve", p2[gb], Av, twi_b, ALU.mult, reads=[f"ps{ab}", f"ps{ab + 1}", "twi"], writes=[f"p2{gb}"])
            P.tt("pool", apr[gb], p1[gb][:, :, 0, :], p2[gb][:, :, 1, :], ALU.subtract, reads=[f"p1{gb}", f"p2{gb}"], writes=[f"apr{gb}"])
            P.tt("pool", api[gb], p2[gb][:, :, 0, :], p1[gb][:, :, 1, :], ALU.add, reads=[f"p1{gb}", f"p2{gb}"], writes=[f"api{gb}"])
            fb = 4 + gb
            for pi in range(4):
                out = ps[:, fb, pi * 128:(pi + 1) * 128]
                P.mm(out, apr[gb][:, pi, :], bdc, start=True, stop=False, reads=[f"apr{gb}", "bdc"], writes=[f"ps{fb}"], skip_group_check=True)
                P.mm(out, api[gb][:, pi, :], bds, start=False, stop=True, reads=[f"api{gb}", "bds"], writes=[f"ps{fb}"], skip_group_check=True)
            src = ps[:, fb, :].rearrange("p (pi k c) -> p k pi c", pi=4, k=64, c=2)
            dst = fsb[:, :, 8 * grp:8 * grp + 8].rearrange("p k (pi c) -> p k pi c", c=2)
            P.cp("act" if grp % 2 == 0 else "dve", dst, src, reads=[f"ps{fb}"], writes=["fsb"])
        P.load(fo.rearrange("(k2 k1) c -> k1 k2 c", k1=128), fsb, reads=["fsb"], writes=["fo"])
        P.emit()
    return nc


def build_C():
    nc = bass.Bass("TRN2", target_bir_lowering=False)
    dram = lambda n, s, dt=F32, kind="ExternalInput": nc.dram_tensor(n, s, dt, kind=kind).ap()
    x1s = dram("x1s", [TOK, D])
    fsh = dram("fsh", [TOK, D], BF16)
    c_d = dram("c", [128, 8])
    adaw = dram("ada_w", [D, 6 * D]); adab = dram("ada_b", [6 * D])
    nmix = dram("norm_mix", [D]); nffn = dram("norm_ffn", [D])
    fwout = dram("fwout", [D, D])
    nfin = dram("norm_final", [D])
    ident_d = dram("ident", [128, 128], BF16); identf_d = dram("identf", [128, 128])
    wr = dram("wr", [128, 8, 20]); br = dram("br", [20])
    wg = dram("wg", [NE, D, DE]); wu = dram("wu", [NE, D, DE]); wd = dram("wd", [NE, DE, D])
    yo = dram("yo", [TOK, D], F32, "ExternalOutput")
    P = Prog(nc)
    with contextlib.ExitStack() as st:
        cx = Ctx()
        cx.P = P
        cx.A = A = Arena(nc, st)
        cx.ps = ps = st.enter_context(nc.psum_tensor("ps", [128, 8, 512], F32))
        setup_common(cx, ident_d, identf_d)
        stage_mod(cx, c_d, adaw, adab, nmix, nffn, "m1")
        mt = cx.modtag
        cx.xres = A.alloc([NT, D], F32)
        xkeys = [f"xres{t}" for t in range(NT)]
        P.load(cx.xres, x1s.rearrange("(t p) d -> p t d", p=128), writes=xkeys)
        m0 = A.mark()
        norm_scratch(cx)
        fsb = A.alloc([NT, D], BF16)
        fT = A.alloc([8, TOK], BF16)
        wob = A.alloc([8, D], BF16)
        P.load(fsb, fsh.rearrange("(t p) d -> p t d", p=128), writes=["fsb"])
        P.load(wob, fwout.rearrange("(kc p) n -> p kc n", p=128), writes=["wob"], eng="pool")
        for kc in range(8):
            P.tt("pool", wob[:, kc, :], wob[:, kc, :], cx.g1bc, ALU.mult, reads=["wob", mt + "g1bc"], writes=["wob"])
        for t in range(NT):
            transpose_tok(cx, fsb[:, t, :], "fsb", fT[:, :, t * 128:(t + 1) * 128], f"fT{t}", eng=("dve" if t % 2 else "act"))
        proj_residual(cx, fT, [f"fT{t}" for t in range(NT)], wob, "wob")
        P.fence()
        A.release(m0)
        stage_moe(cx, wr, br, wg, wu, wd, "e1")
        nfb = A.alloc([D], F32)
        P.load(nfb, nfin.partition_broadcast(128), writes=["nfb"])
        junk = A.alloc([D], BF16)
        fst = A.alloc([NT, 4], F32)
        ot = [A.alloc([D], F32) for _ in range(2)]
        yv = yo.rearrange("(t p) d -> t p d", p=128)
        for t in range(NT):
            P.act(junk, cx.xres[:, t, :], AF.Square, reads=[f"xres{t}"], writes=["fjunk", f"fst{t}"], accum_out=fst[:, t, 0:1])
            P.ts("dve", fst[:, t, 1:2], fst[:, t, 0:1], 1.0 / D, EPS, ALU.mult, ALU.add, reads=[f"fst{t}"], writes=[f"fst{t}"])
            P.act(fst[:, t, 2:3], fst[:, t, 1:2], AF.Sqrt, reads=[f"fst{t}"], writes=[f"fst{t}"])
            P.op("dve", lambda e, o=fst[:, t, 3:4], i_=fst[:, t, 2:3]: e.reciprocal(o, i_), reads=[f"fst{t}"], writes=[f"fst{t}"])
            ob = ot[t % 2]
            P.stt("dve", ob, cx.xres[:, t, :], fst[:, t, 3:4], nfb, ALU.mult, ALU.mult,
                  reads=[f"xres{t}", f"fst{t}", "nfb"], writes=[f"ot{t % 2}"])
            P.load(yv[t], ob, reads=[f"ot{t % 2}"], writes=[f"yo{t}"])
        P.emit()
    return nc


RG = [[0, 1, 2, 3], [4, 5, 6, 7]]
DEBUG_SKIP0 = False
DBG = {}


def fourier_body(cx, fwin, hTd, hTg, fd, tabs):
    P, A, ps = cx.P, cx.A, cx.ps
    mt = cx.modtag
    cs_d, r1_d, r2_d, twr_d, twi_d, bdc_d, bds_d = tabs
    m_f = A.mark()
    cs = A.alloc([2, 2, 256], BF16); r1 = A.alloc([256], BF16); r2 = A.alloc([256], BF16)
    twr = A.alloc([128], F32); twi = A.alloc([128], F32)
    bdc = A.alloc([128], BF16); bds = A.alloc([128], BF16)
    for ap_, d_, k_ in [(cs, cs_d, "cs"), (r1, r1_d, "r1"), (r2, r2_d, "r2"), (twr, twr_d, "twr"),
                        (twi, twi_d, "twi"), (bdc, bdc_d, "bdc"), (bds, bds_d, "bds")]:
        P.load(ap_, d_, writes=[k_])
    uT = A.alloc([2, S], BF16)
    u_off = A.last
    m2 = A.mark()
    norm_scratch(cx)
    hTo = A.alloc([8, TOK], BF16)
    vk1 = [mt + "A1", mt + "B1"]
    for t0 in range(0, NT, 4):
        ts4 = list(range(t0, t0 + 4))
        norm_T_multi(cx, [cx.xres[:, t, :] for t in ts4], [f"xres{t}" for t in ts4], cx.A1, cx.B1, vk1,
                     [hTo[:, :, t * 128:(t + 1) * 128] for t in ts4], [f"hTo{t}" for t in ts4])
    for i in range(4):
        P.load(hTd[i].rearrange("(kc p) t -> p kc t", p=128), hTo[:, :, i * 512:(i + 1) * 512],
               reads=[f"hTo{t}" for t in range(4 * i, 4 * i + 4)], writes=[f"hTd{i}"])
        P.cc("AllGather", RG, hTd[i], hTg[i], reads=[f"hTd{i}"], writes=[f"hTg{i}"])
    winb = A.alloc([8, 256], BF16)
    P.load(winb, fwin.rearrange("(kc p) n -> p kc n", p=128), writes=["winb"], eng="pool")
    hTc = [A.alloc([8, 512], BF16) for _ in range(2)]
    hTg_v = [g_.rearrange("(r kc p) t -> r p kc t", kc=8, p=128) for g_ in hTg]
    ipb = 0
    for it_ in range(S // 512):
        c4, r_ = it_ // 4, it_ % 4
        ch = 4 * r_ + c4
        cb = it_ % 2
        hk = f"hTc{cb}"
        P.load(hTc[cb], hTg_v[c4][r_], reads=[f"hTg{c4}"], writes=[hk])
        for mc in range(2):
            bank = ipb % 4
            ipb += 1
            for kc in range(8):
                P.mm(ps[:, bank, :], winb[:, kc, mc * 128:(mc + 1) * 128], hTc[cb][:, kc, :], start=(kc == 0), stop=(kc == 7),
                     reads=["winb", hk], writes=[f"ps{bank}"])
            P.cp("act" if mc == 0 else "dve", uT[:, mc, ch * 512:(ch + 1) * 512], ps[:, bank, :],
                 reads=[f"ps{bank}"], writes=[f"uT{ch}"])
    P.fence()
    A.release(m2)
    zsb = A.alloc([2, 128, 64], BF16)
    zflat = zsb.rearrange("p r c n -> p (r c) n")
    fsb = A.alloc([64, 256], BF16)
    p1 = A.alloc([4, 2, 128], F32)
    p2 = A.alloc([4, 2, 128], F32)
    apr = [A.alloc([4, 128], BF16) for _ in range(2)]
    api = [A.alloc([4, 128], BF16) for _ in range(2)]
    twr_b = twr[:, None, None, :].broadcast_to([128, 4, 2, 128])
    twi_b = twi[:, None, None, :].broadcast_to([128, 4, 2, 128])
    for hf in range(2):
        for n2 in range(64):
            bank = n2 % 2
            for mc in range(2):
                P.mm(ps[:, bank, 0:256], uT[:, mc, n2:S:64], cs[:, mc, hf, :], start=(mc == 0), stop=(mc == 1),
                     reads=["cs"], writes=[f"ps{bank}"])
            P.cp("act" if n2 % 2 == 0 else "dve", zflat[:, :, n2], ps[:, bank, 0:256], reads=[f"ps{bank}"], writes=["zsb"])
        for grp in range(16):
            gb = grp % 2
            ab = 2 + 2 * gb
            for pi in range(4):
                pp = grp * 4 + pi
                bank = ab + pi // 2
                out = ps[:, bank, (pi % 2) * 256:(pi % 2) * 256 + 256]
                P.mm(out, zsb[:, 0, 2 * pp:2 * pp + 2, :].rearrange("p c n -> p (c n)"), r1, start=True, stop=False,
                     reads=["r1", "zsb"], writes=[f"ps{bank}"], skip_group_check=True)
                P.mm(out, zsb[:, 1, 2 * pp:2 * pp + 2, :].rearrange("p c n -> p (c n)"), r2, start=False, stop=True,
                     reads=["r2", "zsb"], writes=[f"ps{bank}"], skip_group_check=True)
            Av = ps[:, ab:ab + 2, :].rearrange("p a (b r k) -> p (a b) r k", r=2, k=128)
            P.tt("dve", p1, Av, twr_b, ALU.mult, reads=[f"ps{ab}", f"ps{ab + 1}", "twr"], writes=["p1"])
            P.tt("dve", p2, Av, twi_b, ALU.mult, reads=[f"ps{ab}", f"ps{ab + 1}", "twi"], writes=["p2"])
            P.tt("pool", apr[gb], p1[:, :, 0, :], p2[:, :, 1, :], ALU.subtract, reads=["p1", "p2"], writes=[f"apr{gb}"])
            P.tt("pool", api[gb], p2[:, :, 0, :], p1[:, :, 1, :], ALU.add, reads=["p1", "p2"], writes=[f"api{gb}"])
            fb = 6 + gb
            for pi in range(4):
                out = ps[:, fb, pi * 128:(pi + 1) * 128]
                P.mm(out, apr[gb][:, pi, :], bdc, start=True, stop=False, reads=[f"apr{gb}", "bdc"], writes=[f"ps{fb}"], skip_group_check=True)
                P.mm(out, api[gb][:, pi, :], bds, start=False, stop=True, reads=[f"api{gb}", "bds"], writes=[f"ps{fb}"], skip_group_check=True)
            src = ps[:, fb, :].rearrange("p (pi k c) -> p k pi c", pi=4, k=64, c=2)
            c0 = hf * 128 + 8 * grp
            dst = fsb[:, :, c0:c0 + 8].rearrange("p k (pi c) -> p k pi c", c=2)
            P.cp("act" if grp % 2 == 0 else "dve", dst, src, reads=[f"ps{fb}"], writes=["fsb"])
    for i in range(4):
        P.load(fd[i].rearrange("(k2 k1) c -> k1 k2 c", k1=128), fsb[:, 16 * i:16 * (i + 1), :], reads=["fsb"], writes=[f"fd{i}"])
    P.fence()
    A.release(m_f)


def tail_body(cx, fd, fg, sel_d, fwout, nfin, wr, br, wg, wu, wd, yo):
    P, A, ps = cx.P, cx.A, cx.ps
    mt = cx.modtag
    for i in range(4):
        P.cc("AllGather", RG, fd[i], fg[i], reads=[f"fd{i}"], writes=[f"fg{i}"])
    m0 = A.mark()
    sel = A.alloc([4], F32)
    P.load(sel, sel_d, writes=["sel"])
    fsel = A.alloc([NT, D], BF16)
    wob = A.alloc([8, D], BF16)
    cand = A.alloc([NT, D], BF16)
    c_off = A.last
    P.load(wob, fwout.rearrange("(kc p) n -> p kc n", p=128), writes=["wob"], eng="pool")
    for kc in range(8):
        P.tt("pool", wob[:, kc, :], wob[:, kc, :], cx.g1bc, ALU.mult, reads=["wob", mt + "g1bc"], writes=["wob"])
    for r_ in range(4):
        fg_v = fg[r_].rearrange("(g t p) c -> g p t c", g=4, p=128)
        for g in range(4):
            P.load(cand[:, :, g * 256:(g + 1) * 256], fg_v[g], reads=[f"fg{r_}"], writes=["cand"])
        if r_ == 0:
            P.ts("dve", fsel, cand, sel[:, 0:1], None, ALU.mult, reads=["cand", "sel"], writes=["fsel"])
        else:
            P.stt("dve", fsel, cand, sel[:, r_:r_ + 1], fsel, ALU.mult, ALU.add, reads=["cand", "sel"], writes=["fsel"])
    P.fence()
    fT = A.view(c_off, [8, TOK], BF16)
    for t in range(NT):
        transpose_tok(cx, fsel[:, t, :], "fsel", fT[:, :, t * 128:(t + 1) * 128], f"fT{t}", eng=("dve" if t % 2 else "act"))
    proj_residual(cx, fT, [f"fT{t}" for t in range(NT)], wob, "wob")
    P.fence()
    A.release(m0)
    stage_moe(cx, wr, br, wg, wu, wd, "e1")
    nfb = A.alloc([D], F32)
    P.load(nfb, nfin.partition_broadcast(128), writes=["nfb"])
    junk = A.alloc([D], BF16)
    fst = A.alloc([NT, 4], F32)
    ot = [A.alloc([D], F32) for _ in range(2)]
    yv = yo.rearrange("(t p) d -> t p d", p=128)
    for t in range(NT):
        P.act(junk, cx.xres[:, t, :], AF.Square, reads=[f"xres{t}"], writes=["fjunk", f"fst{t}"], accum_out=fst[:, t, 0:1])
        P.ts("dve", fst[:, t, 1:2], fst[:, t, 0:1], 1.0 / D, EPS, ALU.mult, ALU.add, reads=[f"fst{t}"], writes=[f"fst{t}"])
        P.act(fst[:, t, 2:3], fst[:, t, 1:2], AF.Sqrt, reads=[f"fst{t}"], writes=[f"fst{t}"])
        P.op("dve", lambda e, o=fst[:, t, 3:4], i_=fst[:, t, 2:3]: e.reciprocal(o, i_), reads=[f"fst{t}"], writes=[f"fst{t}"])
        ob = ot[t % 2]
        P.stt("dve", ob, cx.xres[:, t, :], fst[:, t, 3:4], nfb, ALU.mult, ALU.mult,
              reads=[f"xres{t}", f"fst{t}", "nfb"], writes=[f"ot{t % 2}"])
        P.load(yv[t], ob, reads=[f"ot{t % 2}"], writes=[f"yo{t}"])


def build_F():
    nc = bass.Bass("TRN2", target_bir_lowering=False)
    dram = lambda n, s, dt=F32, kind="ExternalInput": nc.dram_tensor(n, s, dt, kind=kind).ap()
    xp = dram("xp", [S, D])
    c_d = dram("c", [128, 8])
    adaw = dram("ada_w", [2, D, 6 * D]); adab = dram("ada_b", [2, 6 * D])
    nmix = dram("norm_mix", [2, D]); nffn = dram("norm_ffn", [2, D])
    if not DEBUG_SKIP0:
        w_in = dram("w_in", [D, 3 * D]); w_out = dram("w_out", [D, D])
        lam = dram("lam", [4, 64]); subln = dram("subln", [128, 1])
        kaug = dram("kaug", [H, 8, 2, S], BF16); qaug = dram("qaug", [8, 2, TOK], BF16)
        dfix = dram("dfix", [128, H, 128], BF16)
    ident_d = dram("ident", [128, 128], BF16); identf_d = dram("identf", [128, 128])
    wr = dram("wr", [2, 128, 8, 20]); br = dram("br", [2, 20])
    wg = dram("wg", [2, NE, D, DE]); wu = dram("wu", [2, NE, D, DE]); wd = dram("wd", [2, NE, DE, D])
    fwin = dram("fwin", [D, 256]); fwout = dram("fwout", [D, D]); nfin = dram("norm_final", [D])
    cs_d = dram("cs", [128, 2, 2, 256], BF16)
    r1_d = dram("r1", [128, 256], BF16); r2_d = dram("r2", [128, 256], BF16)
    twr_d = dram("twr", [128, 128]); twi_d = dram("twi", [128, 128])
    bdc_d = dram("bdc", [128, 128], BF16); bds_d = dram("bds", [128, 128], BF16)
    sel_d = dram("sel", [128, 4])
    yo = dram("yo", [TOK, D], F32, "ExternalOutput")
    if not DEBUG_SKIP0:
        kscr = dram("kscr", [2, H, 64, S], BF16, "Internal")
        qscr = dram("qscr", [2, H, 64, TOK], BF16, "Internal")
        vscr = dram("vscr", [S // 128, 128, H, 129], BF16, "Internal")
    hTd = [dram(f"hTd{i}", [D, 512], BF16, "Internal") for i in range(4)]
    hTg = [dram(f"hTg{i}", [4 * D, 512], BF16, "Internal") for i in range(4)]
    fd = [dram(f"fd{i}", [TOK, 256], BF16, "Internal") for i in range(4)]
    fg = [dram(f"fg{i}", [4 * TOK, 256], BF16, "Internal") for i in range(4)]
    P = Prog(nc)
    with contextlib.ExitStack() as st:
        cx = Ctx()
        cx.P = P
        cx.A = A = Arena(nc, st)
        cx.ps = st.enter_context(nc.psum_tensor("ps", [128, 8, 512], F32))
        setup_common(cx, ident_d, identf_d)
        stage_mod(cx, c_d, adaw[0], adab[0], nmix[0], nffn[0], "m0")
        if DEBUG_SKIP0:
            cx.xres = A.alloc([NT, D], F32)
            P.load(cx.xres, xp[0:TOK].rearrange("(t p) d -> p t d", p=128), writes=[f"xres{t}" for t in range(NT)])
        else:
            layer0_body(cx, xp, w_in, w_out, lam, subln, kaug, qaug, dfix, kscr, qscr, vscr, wr[0], br[0], wg[0], wu[0], wd[0])
        stage_mod(cx, c_d, adaw[1], adab[1], nmix[1], nffn[1], "m1")
        fourier_body(cx, fwin, hTd, hTg, fd, (cs_d, r1_d, r2_d, twr_d, twi_d, bdc_d, bds_d))
        tail_body(cx, fd, fg, sel_d, fwout, nfin, wr[1], br[1], wg[1], wu[1], wd[1], yo)
        P.emit()
    return nc


def _bf(a):
    return np.asarray(a, dtype=np.float32).astype(ml_dtypes.bfloat16)


def _consts():
    ident = _bf(np.eye(128))
    identf = np.eye(128, dtype=np.float32)
    return ident, identf


def _attn_tables(j):
    own = np.arange(TOK * j, TOK * (j + 1))
    others = np.concatenate([np.arange(0, TOK * j), np.arange(TOK * (j + 1), S)])
    perm = np.concatenate([own, others])
    pos = perm.astype(np.float64)
    kt_abs = np.floor(pos / 128) * 128
    kr = pos - kt_abs
    slopes = 2.0 ** (-(np.arange(H) + 1.0))
    kaug = np.zeros((H, 8, 2, S), np.float64)
    after = (perm >= TOK * (j + 1))
    own_m = np.arange(S) < TOK
    use2 = np.where(own_m | after, 1.0, 0.0)
    for h in range(H):
        sl = slopes[h]
        set1 = np.stack([np.full(S, -sl), np.full(S, -sl), sl * kt_abs, sl * kr])
        kaug[h, 0:4, :, :] = set1[:, None, :]
        kaug[h, 4:8, :, :] = (-2.0 * set1 * use2[None, :])[:, None, :]
    qpos = own.astype(np.float64)
    qt_abs = np.floor(qpos / 128) * 128
    qr = qpos - qt_abs
    q4 = np.stack([qt_abs, qr, np.ones(TOK), np.ones(TOK)])
    qaug = np.zeros((8, 2, TOK), np.float64)
    qaug[0:4] = q4[:, None, :]
    qaug[4:8] = q4[:, None, :]
    krr = np.arange(128)[:, None]
    qrr = np.arange(128)[None, :]
    dfix = np.zeros((128, H, 128), np.float64)
    for h in range(H):
        dfix[:, h, :] = -2.0 * slopes[h] * np.maximum(krr - qrr, 0)
    return perm, _bf(kaug), _bf(qaug), _bf(dfix)


_NC_CACHE = {}


def _get(name, fn):
    if name not in _NC_CACHE:
        _NC_CACHE[name] = fn()
    return _NC_CACHE[name]


def run_A(inp):
    ident, identf = _consts()
    maps = []
    for c in range(NCORE):
        b, j = c // 4, c % 4
        perm, kaug, qaug, dfix = _attn_tables(j)
        lam = np.stack([inp["attn_lam_q1"][0], inp["attn_lam_k1"][0], inp["attn_lam_q2"][0], inp["attn_lam_k2"][0]])
        wr = np.concatenate([inp["router_group_w"][0], inp["router_expert_w"][0].reshape(D, 16)], axis=1)
        br = np.concatenate([inp["router_group_b"][0], inp["router_expert_b"][0].reshape(16)])
        maps.append({
            "xp": np.ascontiguousarray(inp["x"][b][perm]),
            "c": np.ascontiguousarray(inp["c"][b].reshape(8, 128).T),
            "ada_w": inp["ada_w"][0], "ada_b": inp["ada_b"][0],
            "norm_mix": inp["norm_mix"][0], "norm_ffn": inp["norm_ffn"][0],
            "w_in": inp["attn_w_in"][0], "w_out": inp["attn_w_out"][0],
            "lam": np.ascontiguousarray(lam), "subln": np.ascontiguousarray(inp["attn_subln"][0].reshape(128, 1)),
            "kaug": kaug, "qaug": qaug, "dfix": dfix, "ident": ident, "identf": identf,
            "wr": np.ascontiguousarray(wr.reshape(8, 128, 20).transpose(1, 0, 2)), "br": np.ascontiguousarray(br),
            "wg": inp["expert_w_gate"][0], "wu": inp["expert_w_up"][0], "wd": inp["expert_w_down"][0],
        })
    nc = _get("A", build_A)
    res = run_bass_kernel_spmd(nc, maps, core_ids=list(range(NCORE)))
    x1 = np.zeros((B, S, D), np.float32)
    for c in range(NCORE):
        b, j = c // 4, c % 4
        x1[b, TOK * j:TOK * (j + 1)] = res.results[c]["xo"]
    if DBG.get("on"):
        DBG["res"] = [{k: np.asarray(v) for k, v in r.items()} for r in res.results]
    return x1


def _fourier_tables():
    m = np.arange(256)[:, None].astype(np.float64); l = np.arange(256)[None, :].astype(np.float64)
    ang = 2 * np.pi * m * l / 256
    cs = np.concatenate([np.cos(ang), -np.sin(ang)], axis=1) / 16.0
    cs = cs.reshape(2, 128, 512).transpose(1, 0, 2)
    n1 = np.arange(128)[:, None].astype(np.float64); k1 = np.arange(128)[None, :].astype(np.float64)
    a = 2 * np.pi * n1 * k1 / 128
    r1 = np.concatenate([np.cos(a), -np.sin(a)], axis=1)
    r2 = np.concatenate([np.sin(a), np.cos(a)], axis=1)
    n2 = (np.arange(128) % 64)[:, None].astype(np.float64)
    tw = 2 * np.pi * n2 * k1 / 8192
    twr = np.cos(tw); twi = -np.sin(tw)
    bdc = np.zeros((128, 128)); bds = np.zeros((128, 128))
    sc = 1.0 / math.sqrt(8192.0)
    for c in range(2):
        nn = np.arange(64)[:, None].astype(np.float64); kk = np.arange(64)[None, :].astype(np.float64)
        a2 = 2 * np.pi * nn * kk / 64
        bdc[c * 64:(c + 1) * 64, c::2] = np.cos(a2) * sc
        bds[c * 64:(c + 1) * 64, c::2] = np.sin(a2) * sc
    return (_bf(cs), _bf(r1), _bf(r2), twr.astype(np.float32), twi.astype(np.float32), _bf(bdc), _bf(bds))


def _moe_router(inp, i):
    wr = np.concatenate([inp["router_group_w"][i], inp["router_expert_w"][i].reshape(D, 16)], axis=1)
    br = np.concatenate([inp["router_group_b"][i], inp["router_expert_b"][i].reshape(16)])
    return np.ascontiguousarray(wr.reshape(8, 128, 20).transpose(1, 0, 2)), np.ascontiguousarray(br)


def run_B(inp, x1):
    ident, identf = _consts()
    cs, r1, r2, twr, twi, bdc, bds = _fourier_tables()
    maps = []
    for c in range(NCORE):
        b, g = c // 4, c % 4
        maps.append({
            "x1": np.ascontiguousarray(x1[b]),
            "c": np.ascontiguousarray(inp["c"][b].reshape(8, 128).T),
            "ada_w": inp["ada_w"][1], "ada_b": inp["ada_b"][1],
            "norm_mix": inp["norm_mix"][1], "norm_ffn": inp["norm_ffn"][1],
            "fwin": np.ascontiguousarray(inp["fourier_w_in"][0][:, 256 * g:256 * (g + 1)]),
            "cs": cs, "r1": r1, "r2": r2, "twr": twr, "twi": twi, "bdc": bdc, "bds": bds,
            "ident": ident, "identf": identf,
        })
    nc = _get("B", build_B)
    res = run_bass_kernel_spmd(nc, maps, core_ids=list(range(NCORE)))
    f = np.zeros((B, S, D), ml_dtypes.bfloat16)
    for c in range(NCORE):
        b, g = c // 4, c % 4
        f[b, :, 256 * g:256 * (g + 1)] = res.results[c]["fo"]
    return f


def run_C(inp, x1, f):
    ident, identf = _consts()
    wr, br = _moe_router(inp, 1)
    maps = []
    for c in range(NCORE):
        b, j = c // 4, c % 4
        maps.append({
            "x1s": np.ascontiguousarray(x1[b, TOK * j:TOK * (j + 1)]),
            "fsh": np.ascontiguousarray(f[b, TOK * j:TOK * (j + 1)]),
            "c": np.ascontiguousarray(inp["c"][b].reshape(8, 128).T),
            "ada_w": inp["ada_w"][1], "ada_b": inp["ada_b"][1],
            "norm_mix": inp["norm_mix"][1], "norm_ffn": inp["norm_ffn"][1],
            "fwout": inp["fourier_w_out"][0], "norm_final": inp["norm_final"],
            "ident": ident, "identf": identf, "wr": wr, "br": br,
            "wg": inp["expert_w_gate"][1], "wu": inp["expert_w_up"][1], "wd": inp["expert_w_down"][1],
        })
    nc = _get("C", build_C)
    res = run_bass_kernel_spmd(nc, maps, core_ids=list(range(NCORE)))
    out = np.zeros((B, S, D), np.float32)
    for c in range(NCORE):
        b, j = c // 4, c % 4
        out[b, TOK * j:TOK * (j + 1)] = res.results[c]["yo"]
    return out


def run_F(inp):
    ident, identf = _consts()
    cs, r1, r2, twr, twi, bdc, bds = _fourier_tables()
    csf = np.asarray(cs).reshape(128, 2, 2, 2, 128).transpose(0, 1, 3, 2, 4).reshape(128, 2, 2, 256)
    csf = np.ascontiguousarray(csf)
    lam = np.ascontiguousarray(np.stack([inp["attn_lam_q1"][0], inp["attn_lam_k1"][0],
                                         inp["attn_lam_q2"][0], inp["attn_lam_k2"][0]]))
    wr0, br0 = _moe_router(inp, 0)
    wr1, br1 = _moe_router(inp, 1)
    wr = np.ascontiguousarray(np.stack([wr0, wr1])); br = np.ascontiguousarray(np.stack([br0, br1]))
    maps = []
    for c in range(NCORE):
        b, j = c // 4, c % 4
        perm, kaug, qaug, dfix = _attn_tables(j)
        sel = np.zeros((128, 4), np.float32)
        sel[:, j] = 1.0
        maps.append({
            "xp": np.ascontiguousarray(inp["x"][b][perm]),
            "c": np.ascontiguousarray(inp["c"][b].reshape(8, 128).T),
            "ada_w": inp["ada_w"], "ada_b": inp["ada_b"],
            "norm_mix": inp["norm_mix"], "norm_ffn": inp["norm_ffn"],
            "w_in": inp["attn_w_in"][0], "w_out": inp["attn_w_out"][0],
            "lam": lam, "subln": np.ascontiguousarray(inp["attn_subln"][0].reshape(128, 1)),
            "kaug": kaug, "qaug": qaug, "dfix": dfix, "ident": ident, "identf": identf,
            "wr": wr, "br": br,
            "wg": inp["expert_w_gate"], "wu": inp["expert_w_up"], "wd": inp["expert_w_down"],
            "fwin": np.ascontiguousarray(inp["fourier_w_in"][0][:, 256 * j:256 * (j + 1)]),
            "fwout": inp["fourier_w_out"][0], "norm_final": inp["norm_final"],
            "cs": csf, "r1": r1, "r2": r2, "twr": twr, "twi": twi, "bdc": bdc, "bds": bds, "sel": sel,
        })
    nc = _get("F", build_F)
    if DEBUG_SKIP0:
        drop = {"w_in", "w_out", "lam", "subln", "kaug", "qaug", "dfix"}
        maps = [{k: v for k, v in m.items() if k not in drop} for m in maps]
    res = run_bass_kernel_spmd(nc, maps, core_ids=list(range(NCORE)))
    out = np.zeros((B, S, D), np.float32)
    for c in range(NCORE):
        b, j = c // 4, c % 4
        out[b, TOK * j:TOK * (j + 1)] = res.results[c]["yo"]
    return out


def kernel(**inputs):
    inp = {k: np.asarray(v, dtype=np.float32) for k, v in inputs.items()}
    return run_F(inp)
```

```python
import math
import contextlib
import numpy as np
import ml_dtypes
import concourse.bass as bass
import concourse.mybir as mybir
from concourse.bass_utils import run_bass_kernel_spmd

F32 = mybir.dt.float32
BF16 = mybir.dt.bfloat16
AF = mybir.ActivationFunctionType
ALU = mybir.AluOpType
AX = mybir.AxisListType

D = 1024
S = 8192
B = 2
NCORE = 8
TOK = 2048
NT = TOK // 128
H = 8
DE = 512
NE = 16
EPS = 1e-6
LAM_INIT0 = 0.8 - 0.6 * math.exp(-0.3 * 0)

ENGS = ("pe", "act", "dve", "pool", "sp")
EPOCH = 12000
NDMASEM = 10


class _Op:
    __slots__ = ("eng", "fn", "waits", "is_dma", "seq", "sem_slot", "is_cc")


class Prog:
    def __init__(self, nc):
        self.nc = nc
        self.ops = {e: [] for e in ENGS}
        self.cnt = {e: 0 for e in ENGS}
        self.last_w = {}
        self.readers = {}
        self.waited = {e: {} for e in ENGS}
        self.dma_rr = {e: 0 for e in ENGS}
        self.dma_cnt = {}
        self.dma_last = {}
        self.semkeys = set()
        self.pending_fence = {e: [] for e in ENGS}

    def _event(self, op):
        if op.is_dma:
            return (("dma", op.eng, op.sem_slot), op.seq)
        return (("eng", op.eng, (op.seq - 1) // EPOCH), ((op.seq - 1) % EPOCH) + 1)

    def _add_wait(self, op, dep):
        if dep is None or dep is op:
            return
        if (not dep.is_dma) and dep.eng == "pe" and op.eng == "pe" and not op.is_dma:
            return
        key, val = self._event(dep)
        w = self.waited[op.eng]
        if w.get(key, 0) >= val:
            return
        w[key] = val
        op.waits.append((key, val))
        self.semkeys.add(key)

    def fence(self):
        deps = []
        for e in ENGS:
            for op in reversed(self.ops[e]):
                if not op.is_dma:
                    deps.append(op)
                    break
        deps.extend(self.dma_last.values())
        for e in ENGS:
            self.pending_fence[e] = list(deps)

    def _record(self, eng, fn, reads, writes, is_dma):
        op = _Op()
        op.eng = eng
        op.fn = fn
        op.waits = []
        op.is_dma = is_dma
        op.is_cc = False
        if is_dma == "cc":
            op.is_dma = True
            op.is_cc = True
            self.ncc = getattr(self, "ncc", 0) + 1
            op.sem_slot = f"cc{self.ncc}"
            op.seq = 1
            self.semkeys.add(("dma", eng, op.sem_slot))
            self.dma_last[(eng, op.sem_slot)] = op
        elif is_dma:
            slot = self.dma_rr[eng] % NDMASEM
            self.dma_rr[eng] += 1
            k = (eng, slot)
            prev = self.dma_last.get(k)
            self.dma_cnt[k] = self.dma_cnt.get(k, 0) + 1
            op.sem_slot = slot
            op.seq = 16 * self.dma_cnt[k]
            self.semkeys.add(("dma", eng, slot))
            if prev is not None:
                self._add_wait(op, prev)
            self.dma_last[k] = op
        else:
            self.cnt[eng] += 1
            op.seq = self.cnt[eng]
            op.sem_slot = None
            self.semkeys.add(("eng", eng, (op.seq - 1) // EPOCH))
        if self.pending_fence[eng]:
            for d in self.pending_fence[eng]:
                self._add_wait(op, d)
            self.pending_fence[eng] = []
        for r in reads:
            self._add_wait(op, self.last_w.get(r))
        for w in writes:
            self._add_wait(op, self.last_w.get(w))
            for rd in self.readers.get(w, ()):
                self._add_wait(op, rd)
        for r in reads:
            self.readers.setdefault(r, []).append(op)
        for w in writes:
            self.last_w[w] = op
            self.readers[w] = []
        self.ops[eng].append(op)
        return op

    def op(self, eng, fn, reads=(), writes=()):
        return self._record(eng, fn, reads, writes, False)

    def dma(self, eng, fn, reads=(), writes=()):
        return self._record(eng, fn, reads, writes, True)

    def cc(self, kind, rg, src, dst, reads=(), writes=()):
        return self._record("pool", lambda e: e.collective_compute(kind, ALU.bypass, replica_groups=rg,
                                                                    ins=[src], outs=[dst]), reads, writes, "cc")

    def mm(self, out, lhsT, rhs, start=True, stop=True, reads=(), writes=(), **kw):
        return self.op("pe", lambda e: e.matmul(out, lhsT, rhs, start=start, stop=stop, **kw), reads, writes)

    def tr(self, out, in_, ident, reads=(), writes=()):
        return self.op("pe", lambda e: e.transpose(out, in_, ident), reads, writes)

    def act(self, out, in_, func, reads=(), writes=(), **kw):
        o = self.op("act", lambda e: e.activation(out, in_, func, **kw), reads, writes)
        acc = kw.get("accum_out")
        if acc is not None and getattr(self, "act_dummy", None) is not None:
            dm = self.act_dummy
            o = self.op("act", lambda e: e.copy(dm, acc), (), writes)
        return o

    def tt(self, eng, out, a, b, op, reads=(), writes=()):
        return self.op(eng, lambda e: e.tensor_tensor(out, a, b, op), reads, writes)

    def ts(self, eng, out, a, s1, s2, op0, op1=None, reads=(), writes=()):
        if op1 is None:
            return self.op(eng, lambda e: e.tensor_scalar(out, a, s1, None, op0), reads, writes)
        return self.op(eng, lambda e: e.tensor_scalar(out, a, s1, s2, op0, op1), reads, writes)

    def stt(self, eng, out, a, s, b, op0, op1, reads=(), writes=()):
        return self.op(eng, lambda e: e.scalar_tensor_tensor(out, a, s, b, op0, op1), reads, writes)

    def cp(self, eng, out, in_, reads=(), writes=()):
        if eng == "act":
            return self.op(eng, lambda e: e.copy(out, in_), reads, writes)
        return self.op(eng, lambda e: e.tensor_copy(out, in_), reads, writes)

    def memset(self, eng, ap, val, writes=()):
        return self.op(eng, lambda e: e.memset(ap, val), (), writes)

    def load(self, out, in_, reads=(), writes=(), eng="sp"):
        return self.dma(eng, lambda e: e.dma_start(out=out, in_=in_), reads, writes)

    def emit(self):
        nc = self.nc
        with contextlib.ExitStack() as st:
            sems = {}
            for key in sorted(self.semkeys, key=str):
                nm = "s_" + "_".join(str(k) for k in key)
                sems[key] = st.enter_context(nc.semaphore(nm))
            finals = []
            for e in ENGS:
                for op in reversed(self.ops[e]):
                    if not op.is_dma:
                        finals.append(self._event(op))
                        break
            for op in self.dma_last.values():
                finals.append(self._event(op))
            block = st.enter_context(nc.Block())
            engmap = {"pe": block.tensor, "act": block.scalar, "dve": block.vector,
                      "pool": block.gpsimd, "sp": block.sync}

            def make(ename):
                ops = self.ops[ename]

                def body(eng):
                    for op in ops:
                        for key, val in op.waits:
                            eng.wait_ge(sems[key], val)
                        ins = op.fn(eng)
                        key, val = self._event(op)
                        if op.is_cc:
                            ins.then_inc(sems[key])
                        else:
                            ins.then_inc(sems[key], 16 if op.is_dma else 1)
                    if ename == "sp":
                        for key, val in finals:
                            eng.wait_ge(sems[key], val)
                return body

            for e in ENGS:
                engmap[e](make(e))


class Arena:
    def __init__(self, nc, st, words=51000):
        self.t = st.enter_context(nc.sbuf_tensor("arena", [128, words], F32))
        self.words = words
        self.top = 0

    def alloc(self, shape, dt):
        n = int(np.prod(shape))
        w = n if dt == F32 else (n + 1) // 2
        w = (w + 7) // 8 * 8
        a = self.top
        self.top += w
        assert self.top <= self.words, ("SBUF arena overflow", self.top, self.words)
        self.last = a
        return self.view(a, shape, dt)

    def view(self, a, shape, dt):
        n = int(np.prod(shape))
        w = n if dt == F32 else (n + 1) // 2
        w = (w + 7) // 8 * 8
        ap = self.t[:, a:a + w]
        if dt == BF16:
            ap = ap.bitcast(BF16)
        ap = ap[:, 0:n]
        if len(shape) > 1:
            names = [f"d{i}" for i in range(len(shape))]
            kw = {names[i]: int(shape[i]) for i in range(1, len(shape))}
            ap = ap.rearrange(f"p ({' '.join(names)}) -> p {' '.join(names)}", **kw)
        return ap

    def mark(self):
        return self.top

    def release(self, m):
        self.top = m


class Ctx:
    pass


def bc_mid(ap, n):
    return ap[:, :, None].broadcast_to([128, ap.shape[1], n])


def setup_common(cx, ident_d, identf_d):
    P, A = cx.P, cx.A
    cx.ident = A.alloc([128], BF16)
    cx.identf = A.alloc([128], F32)
    P.act_dummy = A.alloc([1], F32)
    P.load(cx.ident, ident_d, writes=["ident"])
    P.load(cx.identf, identf_d, writes=["identf"])


def diag_extract(cx, dst, src_bc, rkeys, wkey):
    P = cx.P
    tmp = cx.diag_tmp
    P.tt("dve", tmp, src_bc.rearrange("p (a b) -> p a b", b=128),
         cx.identf[:, None, :].broadcast_to([128, 8, 128]), ALU.mult,
         reads=list(rkeys) + ["identf"], writes=["diag_tmp"])
    P.op("dve", lambda e: e.tensor_reduce(dst, tmp, AX.X, ALU.add), reads=["diag_tmp"], writes=[wkey])


def stage_mod(cx, c_d, adaw_d, adab_d, nmix_d, nffn_d, tag):
    P, A, ps = cx.P, cx.A, cx.ps
    if getattr(cx, "A1", None) is not None:
        A1, B1, A2, B2, g1bc, g2bc = cx.A1, cx.B1, cx.A2, cx.B2, cx.g1bc, cx.g2bc
    else:
        A1 = A.alloc([8], F32); B1 = A.alloc([8], F32); A2 = A.alloc([8], F32); B2 = A.alloc([8], F32)
        g1bc = A.alloc([D], F32); g2bc = A.alloc([D], F32)
    m = A.mark()
    csb = A.alloc([8], F32)
    cond = A.alloc([8], F32)
    condbc = A.alloc([8, 128], F32)
    modbc = A.alloc([6 * D], F32)
    adab = A.alloc([6 * D], F32)
    nm = A.alloc([D], F32)
    nf = A.alloc([D], F32)
    cx.diag_tmp = A.alloc([8, 128], F32)
    wbuf = [A.alloc([8, 512], F32) for _ in range(2)]
    t = tag
    P.load(csb, c_d, writes=[t + "csb"])
    P.load(adab, adab_d.partition_broadcast(128), writes=[t + "adab"])
    P.load(nm, nmix_d.partition_broadcast(128), writes=[t + "nm"])
    P.load(nf, nffn_d.partition_broadcast(128), writes=[t + "nf"])
    P.act(cond, csb, AF.Silu, reads=[t + "csb"], writes=[t + "cond"])
    P.cp("dve", condbc, bc_mid(cond, 128), reads=[t + "cond"], writes=[t + "condbc"])
    wv = adaw_d.rearrange("(kc p) n -> p kc n", p=128)
    for nch in range(12):
        wb = wbuf[nch % 2]
        wk = f"{t}adaw{nch % 2}"
        P.load(wb, wv[:, :, nch * 512:(nch + 1) * 512], writes=[wk])
        bank = 6 + nch % 2
        for kc in range(8):
            P.mm(ps[:, bank, :], condbc[:, kc, :], wb[:, kc, :],
                 start=(kc == 0), stop=(kc == 7), reads=[wk, t + "condbc"], writes=[f"ps{bank}"])
        sl = slice(nch * 512, (nch + 1) * 512)
        P.tt("dve", modbc[:, sl], ps[:, bank, :], adab[:, sl], ALU.add,
             reads=[f"ps{bank}", t + "adab"], writes=[t + "modbc"])
    sh1, sc1, g1, sh2, sc2, g2 = [modbc[:, i * D:(i + 1) * D] for i in range(6)]
    tmpA = adab[:, 0:D]
    P.stt("dve", tmpA, sc1, 1.0, nm, ALU.add, ALU.mult, reads=[t + "modbc", t + "nm"], writes=[t + "adab"])
    diag_extract(cx, A1, tmpA, [t + "adab"], t + "A1")
    diag_extract(cx, B1, sh1, [t + "modbc"], t + "B1")
    tmpA2 = adab[:, D:2 * D]
    P.stt("dve", tmpA2, sc2, 1.0, nf, ALU.add, ALU.mult, reads=[t + "modbc", t + "nf"], writes=[t + "adab2"])
    diag_extract(cx, A2, tmpA2, [t + "adab2"], t + "A2")
    diag_extract(cx, B2, sh2, [t + "modbc"], t + "B2")
    P.cp("dve", g1bc, g1, reads=[t + "modbc"], writes=[t + "g1bc"])
    P.cp("dve", g2bc, g2, reads=[t + "modbc"], writes=[t + "g2bc"])
    P.fence()
    A.release(m)
    cx.A1, cx.B1, cx.A2, cx.B2, cx.g1bc, cx.g2bc = A1, B1, A2, B2, g1bc, g2bc
    cx.modtag = t


def norm_scratch(cx):
    A = cx.A
    cx.n_junk = A.alloc([D], BF16)
    cx.n_xn = [A.alloc([D], BF16) for _ in range(4)]
    cx.n_st = [A.alloc([4, 4], F32) for _ in range(2)]
    cx.n_tmp = [A.alloc([8, 128], BF16) for _ in range(2)]
    cx.n_i = 0
    cx.n_g = 0


def norm_part_a(cx, xts, xkeys):
    P = cx.P
    n = len(xts)
    g = cx.n_g % 2
    cx.n_g += 1
    st = cx.n_st[g]
    sk = f"nst{g}"
    for j in range(n):
        P.act(cx.n_junk, xts[j], AF.Square, reads=[xkeys[j]], writes=["njunk", sk], accum_out=st[:, 0, j:j + 1])
    P.ts("dve", st[:, 1, 0:n], st[:, 0, 0:n], 1.0 / D, EPS, ALU.mult, ALU.add, reads=[sk], writes=[sk])
    P.act(st[:, 2, 0:n], st[:, 1, 0:n], AF.Sqrt, reads=[sk], writes=[sk])
    P.op("dve", lambda e: e.reciprocal(st[:, 3, 0:n], st[:, 2, 0:n]), reads=[sk], writes=[sk])
    for j in range(n):
        P.act(cx.n_xn[j], xts[j], AF.Copy, reads=[xkeys[j], sk], writes=[f"nxn{j}"], scale=st[:, 3, j:j + 1])
    return n


def norm_part_b(cx, n, Avec, Bvec, vkeys, dsts, dkeys):
    P, ps = cx.P, cx.ps
    for j in range(n):
        xn = cx.n_xn[j]
        xk = f"nxn{j}"
        i = cx.n_i % 2
        cx.n_i += 1
        tmp = cx.n_tmp[i]
        tk = f"ntmp{i}"
        bank = 6 + i
        pst = ps[:, bank, :].bitcast(BF16).rearrange("p (a b) -> p a b", b=128)
        for kc in range(8):
            P.tr(pst[:, kc, :], xn[:, kc * 128:(kc + 1) * 128], cx.ident, reads=[xk, "ident"], writes=[f"ps{bank}"])
        P.tt("dve", tmp, pst, bc_mid(Avec, 128), ALU.mult, reads=[f"ps{bank}"] + list(vkeys), writes=[tk])
        P.tt("pool", dsts[j], tmp, bc_mid(Bvec, 128), ALU.add, reads=[tk] + list(vkeys), writes=[dkeys[j]])


def norm_T_multi(cx, xts, xkeys, Avec, Bvec, vkeys, dsts, dkeys):
    n = norm_part_a(cx, xts, xkeys)
    norm_part_b(cx, n, Avec, Bvec, vkeys, dsts, dkeys)


def norm_T(cx, xt, xkey, Avec, Bvec, vkeys, dst, dkey):
    norm_T_multi(cx, [xt], [xkey], Avec, Bvec, vkeys, [dst], [dkey])


def transpose_tok(cx, src, skey, dst, dkey, eng="dve"):
    P, ps = cx.P, cx.ps
    i = cx.n_i % 2
    cx.n_i += 1
    bank = 6 + i
    pst = ps[:, bank, :].bitcast(BF16).rearrange("p (a b) -> p a b", b=128)
    for kc in range(8):
        P.tr(pst[:, kc, :], src[:, kc * 128:(kc + 1) * 128], cx.ident, reads=[skey, "ident"], writes=[f"ps{bank}"])
    P.cp(eng, dst, pst, reads=[f"ps{bank}"], writes=[dkey])


def proj_residual(cx, srcT, skeys, wp, wkey):
    P, ps = cx.P, cx.ps
    it = 0
    for t in range(NT):
        for half in range(2):
            bank = 4 + (it % 2)
            it += 1
            for kc in range(8):
                P.mm(ps[:, bank, :], srcT[:, kc, t * 128:(t + 1) * 128], wp[:, kc, half * 512:(half + 1) * 512],
                     start=(kc == 0), stop=(kc == 7), reads=list(skeys) + [wkey], writes=[f"ps{bank}"])
            xs = cx.xres[:, t, half * 512:(half + 1) * 512]
            P.tt("dve", xs, ps[:, bank, :], xs, ALU.add, reads=[f"ps{bank}"], writes=[f"xres{t}"])


def stage_moe(cx, wr_d, br_d, wg_d, wu_d, wd_d, tag):
    P, A, ps = cx.P, cx.A, cx.ps
    t_ = tag
    m = A.mark()
    h2T = A.alloc([8, TOK], BF16)
    norm_scratch(cx)
    vk = [cx.modtag + "A2", cx.modtag + "B2"]
    for t0 in range(0, NT, 4):
        ts4 = list(range(t0, t0 + 4))
        norm_T_multi(cx, [cx.xres[:, t, :] for t in ts4], [f"xres{t}" for t in ts4], cx.A2, cx.B2, vk,
                     [h2T[:, :, t * 128:(t + 1) * 128] for t in ts4], [f"{t_}h2T{t}" for t in ts4])
    h2keys = [f"{t_}h2T{t}" for t in range(NT)]
    wr = A.alloc([8, 20], BF16)
    brbc = A.alloc([20], F32)
    P.load(wr, wr_d, writes=[t_ + "wr"], eng="pool")
    P.load(brbc, br_d.partition_broadcast(128), writes=[t_ + "br"])
    L = A.alloc([NT, 20], F32)
    rbank = 5
    Lps = ps[:, rbank, :].rearrange("p (a b) -> p a b", b=32)[:, :, 0:20]
    for t in range(NT):
        for kc in range(8):
            P.mm(Lps[:, t, :], h2T[:, kc, t * 128:(t + 1) * 128], wr[:, kc, :], start=(kc == 0), stop=(kc == 7),
                 reads=[f"{t_}h2T{t}", t_ + "wr"], writes=[f"ps{rbank}"])
    P.tt("dve", L, Lps, brbc[:, None, :].broadcast_to([128, NT, 20]), ALU.add,
         reads=[f"ps{rbank}", t_ + "br"], writes=[t_ + "L"])
    Lg = L[:, :, 0:4]
    Le = L[:, :, 4:20].rearrange("p t (g e) -> p t g e", e=4)
    gmax = A.alloc([NT], F32); gsum = A.alloc([NT], F32); gw = A.alloc([NT], F32)
    ohg = A.alloc([NT, 4], F32); eg = A.alloc([NT, 4], F32)
    tmp44 = A.alloc([NT, 4, 4], F32)
    esel = A.alloc([NT, 4], F32); e2 = A.alloc([NT, 4], F32)
    m1 = A.alloc([NT], F32); m2 = A.alloc([NT], F32)
    mk1 = A.alloc([NT, 4], F32); mk2 = A.alloc([NT, 4], F32)
    dd = A.alloc([NT], F32); w1 = A.alloc([NT], F32); w2 = A.alloc([NT], F32)
    ew = A.alloc([NT, 4], F32)
    gates = A.alloc([NT, 4, 4], F32)
    rk = t_ + "rt"

    def bcl(ap, n):
        return ap[:, :, None].broadcast_to([128, NT, n])

    P.op("dve", lambda e: e.tensor_reduce(gmax, Lg, AX.X, ALU.max), reads=[t_ + "L"], writes=[rk])
    P.tt("dve", ohg, Lg, bcl(gmax, 4), ALU.is_equal, reads=[rk, t_ + "L"], writes=[rk])
    P.tt("dve", eg, Lg, bcl(gmax, 4), ALU.subtract, reads=[rk, t_ + "L"], writes=[rk])
    P.act(eg, eg, AF.Exp, reads=[rk], writes=[rk])
    P.op("dve", lambda e: e.tensor_reduce(gsum, eg, AX.X, ALU.add), reads=[rk], writes=[rk])
    P.op("dve", lambda e: e.reciprocal(gw, gsum), reads=[rk], writes=[rk])
    P.tt("dve", tmp44, Le, ohg[:, :, :, None].broadcast_to([128, NT, 4, 4]), ALU.mult, reads=[rk, t_ + "L"], writes=[rk])
    P.op("dve", lambda e: e.tensor_reduce(esel, tmp44.rearrange("p t g e -> p t e g"), AX.X, ALU.add), reads=[rk], writes=[rk])
    P.op("dve", lambda e: e.tensor_reduce(m1, esel, AX.X, ALU.max), reads=[rk], writes=[rk])
    P.tt("dve", mk1, esel, bcl(m1, 4), ALU.is_equal, reads=[rk], writes=[rk])
    P.stt("dve", e2, mk1, -1e30, esel, ALU.mult, ALU.add, reads=[rk], writes=[rk])
    P.op("dve", lambda e: e.tensor_reduce(m2, e2, AX.X, ALU.max), reads=[rk], writes=[rk])
    P.tt("dve", mk2, e2, bcl(m2, 4), ALU.is_equal, reads=[rk], writes=[rk])
    P.tt("dve", dd, m2, m1, ALU.subtract, reads=[rk], writes=[rk])
    P.act(dd, dd, AF.Exp, reads=[rk], writes=[rk])
    P.ts("dve", w1, dd, 1.0, None, ALU.add, reads=[rk], writes=[rk])
    P.op("dve", lambda e: e.reciprocal(w1, w1), reads=[rk], writes=[rk])
    P.tt("dve", w2, dd, w1, ALU.mult, reads=[rk], writes=[rk])
    P.tt("dve", w1, w1, gw, ALU.mult, reads=[rk], writes=[rk])
    P.tt("dve", w2, w2, gw, ALU.mult, reads=[rk], writes=[rk])
    P.tt("dve", ew, mk1, bcl(w1, 4), ALU.mult, reads=[rk], writes=[rk])
    P.tt("dve", mk2, mk2, bcl(w2, 4), ALU.mult, reads=[rk], writes=[rk])
    P.tt("dve", ew, ew, mk2, ALU.add, reads=[rk], writes=[rk])
    P.tt("dve", gates, ohg[:, :, :, None].broadcast_to([128, NT, 4, 4]),
         ew[:, :, None, :].broadcast_to([128, NT, 4, 4]), ALU.mult, reads=[rk], writes=[t_ + "gates"])
    gflat = gates.rearrange("p t g e -> p t (g e)")
    if "L" in DBG and t_ == "e0":
        P.load(DBG["L"].rearrange("(t p) d -> p t d", p=128), L, reads=[t_ + "L"], writes=["dbg_L"])
        P.load(DBG["gates"].rearrange("(t p) d -> p t d", p=128), gflat, reads=[t_ + "gates"], writes=["dbg_g"])
        P.load(DBG["xmid"].rearrange("(t p) d -> p t d", p=128), cx.xres, reads=[f"xres{t}" for t in range(NT)], writes=["dbg_x"])
    wgb = [A.alloc([8, DE], BF16) for _ in range(2)]
    wub = [A.alloc([8, DE], BF16) for _ in range(2)]
    wdb = [A.alloc([4, D], BF16) for _ in range(2)]
    sil = [A.alloc([512], BF16) for _ in range(2)]
    hid = [A.alloc([4, 512], BF16) for _ in range(2)]
    it_gu = 0
    it_y = 0
    it_h = 0
    for e in range(NE):
        bi = e % 2
        kg, ku, kd = f"{t_}wg{bi}", f"{t_}wu{bi}", f"{t_}wd{bi}"
        P.load(wgb[bi], wg_d[e].rearrange("(kc p) n -> p kc n", p=128), writes=[kg], eng="pool")
        P.load(wub[bi], wu_d[e].rearrange("(kc p) n -> p kc n", p=128), writes=[ku], eng="pool")
        P.load(wdb[bi], wd_d[e].rearrange("(kc p) n -> p kc n", p=128), writes=[kd], eng="pool")
        P.tt("pool", wdb[bi], wdb[bi], cx.g2bc[:, None, :].broadcast_to([128, 4, D]), ALU.mult,
             reads=[kd, cx.modtag + "g2bc"], writes=[kd])
        for c in range(4):
            hb = it_h % 2
            it_h += 1
            hk = f"{t_}hid{hb}"
            ckeys = [f"{t_}h2T{t}" for t in range(4 * c, 4 * c + 4)]
            for fc in range(4):
                gb = (it_gu % 2) * 2
                it_gu += 1
                for kc in range(8):
                    P.mm(ps[:, gb, :], wgb[bi][:, kc, fc * 128:(fc + 1) * 128], h2T[:, kc, c * 512:(c + 1) * 512],
                         start=(kc == 0), stop=(kc == 7), reads=[kg] + ckeys, writes=[f"ps{gb}"])
                for kc in range(8):
                    P.mm(ps[:, gb + 1, :], wub[bi][:, kc, fc * 128:(fc + 1) * 128], h2T[:, kc, c * 512:(c + 1) * 512],
                         start=(kc == 0), stop=(kc == 7), reads=[ku] + ckeys, writes=[f"ps{gb + 1}"])
                sb_ = sil[it_gu % 2]
                sk = f"{t_}sil{it_gu % 2}"
                P.act(sb_, ps[:, gb, :], AF.Silu, reads=[f"ps{gb}"], writes=[sk])
                P.tt("dve", hid[hb][:, fc, :], ps[:, gb + 1, :], sb_, ALU.mult, reads=[f"ps{gb + 1}", sk], writes=[hk])
            for ts_ in range(4):
                t = 4 * c + ts_
                for half in range(2):
                    yb = 4 + (it_y % 4)
                    it_y += 1
                    for fc in range(4):
                        P.mm(ps[:, yb, :], hid[hb][:, fc, ts_ * 128:(ts_ + 1) * 128], wdb[bi][:, fc, half * 512:(half + 1) * 512],
                             start=(fc == 0), stop=(fc == 3), reads=[hk, kd], writes=[f"ps{yb}"])
                    xs = cx.xres[:, t, half * 512:(half + 1) * 512]
                    P.stt("dve", xs, ps[:, yb, :], gflat[:, t, e:e + 1], xs, ALU.mult, ALU.add,
                          reads=[f"ps{yb}", t_ + "gates"], writes=[f"xres{t}"])
    P.fence()
    A.release(m)


def layer0_body(cx, xp, w_in, w_out, lam, subln, kaug, qaug, dfix, kscr, qscr, vscr, wr, br, wg, wu, wd):
    P, A, ps = cx.P, cx.A, cx.ps
    mt = cx.modtag
    m_qkv = A.mark()
    win = A.alloc([8, 3 * D], BF16)
    wv = w_in.rearrange("(kc p) n -> p kc n", p=128)
    for i in range(3):
        P.load(win[:, :, i * D:(i + 1) * D], wv[:, :, i * D:(i + 1) * D], writes=[f"win{i}"], eng="pool")
    norm_scratch(cx)
    xt = [A.alloc([D], F32) for _ in range(6)]
    hTc = [A.alloc([8, 512], BF16) for _ in range(2)]
    stgp = [A.alloc([2, H, 512], BF16) for _ in range(2)]
    istg = 0
    vst = [A.alloc([4, H, 129], BF16) for _ in range(2)]
    for i in range(2):
        P.memset("pool", vst[i][:, :, :, 128:129], 1.0, writes=[f"vst{i}"])
    xv = xp.rearrange("(t p) d -> t p d", p=128)
    kscr_v = kscr.rearrange("s h d t -> d s h t")
    qscr_v = qscr.rearrange("s h d t -> d s h t")
    vscr_v = vscr.rearrange("t p h c -> p t h c")
    vk1 = [mt + "A1", mt + "B1"]
    ixt = 0
    ipb = 0
    qkv_state = {"ixt": 0, "istg": 0, "ipb": 0}

    def emit_norm(ch):
        ixt = qkv_state["ixt"]
        cb = ch % 2
        hk = f"hTc{cb}"
        xbs, xks = [], []
        for t in range(4):
            xb = xt[ixt % 6]
            xk = f"xt{ixt % 6}"
            ixt += 1
            P.load(xb, xv[ch * 4 + t], writes=[xk])
            xbs.append(xb)
            xks.append(xk)
        norm_part_a(cx, xbs, xks)
        qkv_state["ixt"] = ixt

    def emit_norm_b(ch):
        cb = ch % 2
        hk = f"hTc{cb}"
        norm_part_b(cx, 4, cx.A1, cx.B1, vk1, [hTc[cb][:, :, t * 128:(t + 1) * 128] for t in range(4)], [hk] * 4)

    def emit_mm(ch):
        cb = ch % 2
        hk = f"hTc{cb}"
        istg = qkv_state["istg"]
        ipb = qkv_state["ipb"]
        for which in ([1, 0] if ch < 4 else [1]):
            stg = stgp[istg % 2]
            sk = f"stg{istg % 2}"
            istg += 1
            for hb in range(H):
                bank = ipb % 4
                ipb += 1
                col = which * D + hb * 128
                for kc in range(8):
                    P.mm(ps[:, bank, :], win[:, kc, col:col + 128], hTc[cb][:, kc, :], start=(kc == 0), stop=(kc == 7),
                         reads=[f"win{which}", hk], writes=[f"ps{bank}"])
                if which == 1:
                    P.cp("act", stg[0:64, 0, hb, :], ps[0:64, bank, :], reads=[f"ps{bank}"], writes=[sk])
                    P.cp("dve", stg[0:64, 1, hb, :], ps[64:128, bank, :], reads=[f"ps{bank}"], writes=[sk])
                else:
                    P.op("act", lambda e, o=stg[0:64, 0, hb, :], i_=ps[0:64, bank, :]: e.mul(o, i_, 0.125),
                         reads=[f"ps{bank}"], writes=[sk])
                    P.ts("dve", stg[0:64, 1, hb, :], ps[64:128, bank, :], 0.125, None, ALU.mult,
                         reads=[f"ps{bank}"], writes=[sk])
            if which == 1:
                P.load(kscr_v[:, :, :, ch * 512:(ch + 1) * 512], stg[0:64], reads=[sk], writes=[f"kscr{ch}"])
            else:
                P.load(qscr_v[:, :, :, ch * 512:(ch + 1) * 512], stg[0:64], reads=[sk], writes=[f"qscr{ch}"])
        for t in range(4):
            for half in range(2):
                bank = ipb % 4
                ipb += 1
                for kc in range(8):
                    P.mm(ps[:, bank, :], hTc[cb][:, kc, t * 128:(t + 1) * 128],
                         win[:, kc, 2 * D + half * 512:2 * D + (half + 1) * 512], start=(kc == 0), stop=(kc == 7),
                         reads=["win2", hk], writes=[f"ps{bank}"])
                eng = "act" if half == 0 else "dve"
                P.cp(eng, vst[cb][:, t, half * 4:(half + 1) * 4, 0:128],
                     ps[:, bank, :].rearrange("p (h c) -> p h c", c=128), reads=[f"ps{bank}"], writes=[f"vst{cb}"])
        P.load(vscr_v[:, ch * 4:(ch + 1) * 4], vst[cb], reads=[f"vst{cb}"], writes=[f"vscr{ch}"])

        qkv_state["istg"] = istg
        qkv_state["ipb"] = ipb

    emit_norm(0)
    emit_norm_b(0)
    for ch in range(S // 512):
        if ch + 1 < S // 512:
            emit_norm(ch + 1)
        emit_mm(ch)
        if ch + 1 < S // 512:
            emit_norm_b(ch + 1)
    P.fence()
    A.release(m_qkv)
    cx.xres = A.alloc([NT, D], F32)
    xres_off = A.last
    m_att = A.mark()
    obf = A.alloc([NT, D], BF16)
    ssq = A.alloc([NT, H], F32)
    lamv = A.alloc([4, 64], F32)
    lamt = A.alloc([2, 64], F32)
    lams = A.alloc([4], F32)
    neglam = A.alloc([1], F32)
    dfx = A.alloc([H, 128], BF16)
    P.load(dfx, dfix, writes=["dfx"])
    P.load(lamv, lam.rearrange("a b -> (a b)").partition_broadcast(128).rearrange("p (a b) -> p a b", b=64), writes=["lamv"])
    P.tt("dve", lamt, lamv[:, 0:4:2, :], lamv[:, 1:4:2, :], ALU.mult, reads=["lamv"], writes=["lamt"])
    P.op("dve", lambda e: e.tensor_reduce(lams[:, 0:2], lamt, AX.X, ALU.add), reads=["lamt"], writes=["lams"])
    P.act(lams[:, 2:4], lams[:, 0:2], AF.Exp, reads=["lams"], writes=["lams"])
    P.tt("dve", neglam, lams[:, 3:4], lams[:, 2:3], ALU.subtract, reads=["lams"], writes=["neglam"])
    P.ts("dve", neglam, neglam, -LAM_INIT0, None, ALU.add, reads=["neglam"], writes=["neglam"])
    ktb = [A.alloc([2, S], BF16)]
    kt_off = A.last
    ktb.append(A.view(xres_off, [2, S], BF16))
    qtb = [A.alloc([2, TOK], BF16) for _ in range(2)]
    qt_off = A.last - (2 * TOK) // 2
    vtb = A.alloc([S // 128, 2, 129], BF16)
    pT = [A.alloc([1024], BF16) for _ in range(3)]
    o32 = [A.alloc([128], F32) for _ in range(4)]
    t32 = [A.alloc([128], F32) for _ in range(1)]
    rr = [A.alloc([4], F32) for _ in range(4)]
    junk = A.alloc([128], BF16)
    vscr_hp = vscr.rearrange("t p (hp hh) c -> p t hp hh c", hh=2)
    NKT = S // 128
    state = {"isb": 0, "ipt": 0, "iep": 0}

    def load_head(h):
        kb = h % 2
        P.load(ktb[kb][0:64], kscr_v[:, :, h, :], writes=[f"ktb{kb}"])
        P.load(ktb[kb][64:72], kaug[h], writes=[f"ktb{kb}"])
        P.load(qtb[kb][0:64], qscr_v[:, :, h, :], writes=[f"qtb{kb}"])
        P.load(qtb[kb][64:72], qaug, writes=[f"qtb{kb}"])

    def load_v(hp):
        for q4 in range(4):
            P.load(vtb[:, q4 * 16:(q4 + 1) * 16], vscr_hp[:, q4 * 16:(q4 + 1) * 16, hp], writes=["vtb"])

    def emit_qk_exp(h, qc, kt):
        kb = h % 2
        kk, qk = f"ktb{kb}", f"qtb{kb}"
        K_, Q_ = ktb[kb], qtb[kb]
        sb0 = 2 * (state["isb"] % 2)
        state["isb"] += 1
        for s_ in range(2):
            bank = sb0 + s_
            outp = ps[:, bank, :]
            kcols = slice(kt * 128, (kt + 1) * 128)
            q0 = qc * 512
            rd = [kk, qk]
            wkey = [f"ps{bank}"]
            if kt >= NT or kt > 4 * qc + 3:
                P.mm(outp, K_[0:72, s_, kcols], Q_[0:72, s_, q0:q0 + 512], reads=rd, writes=wkey)
            elif kt < 4 * qc:
                P.mm(outp, K_[0:68, s_, kcols], Q_[0:68, s_, q0:q0 + 512], reads=rd, writes=wkey)
            else:
                i_ = kt - 4 * qc
                if i_ > 0:
                    P.mm(outp[:, 0:i_ * 128], K_[0:72, s_, kcols], Q_[0:72, s_, q0:q0 + i_ * 128], reads=rd, writes=wkey)
                dsl = slice(i_ * 128, (i_ + 1) * 128)
                P.mm(outp[:, dsl], K_[0:68, s_, kcols], Q_[0:68, s_, q0 + i_ * 128:q0 + (i_ + 1) * 128],
                     start=True, stop=False, reads=rd, writes=wkey, skip_group_check=True)
                P.mm(outp[:, dsl], cx.ident, dfx[:, h, :], start=False, stop=True,
                     reads=["ident", "dfx"], writes=wkey, skip_group_check=True)
                if i_ < 3:
                    P.mm(outp[:, (i_ + 1) * 128:512], K_[0:68, s_, kcols], Q_[0:68, s_, q0 + (i_ + 1) * 128:q0 + 512],
                         reads=rd, writes=wkey, skip_group_check=True)
        pi_ = state["ipt"] % 3
        state["ipt"] += 1
        P.act(pT[pi_], ps[:, sb0:sb0 + 2, :].rearrange("p a b -> p (a b)"), AF.Exp,
              reads=[f"ps{sb0}", f"ps{sb0 + 1}"], writes=[f"pT{pi_}"])
        return pi_

    def emit_pv(h, qc, kt, pi_):
        hh = h % 2
        pb = pT[pi_]
        for s_ in range(2):
            for qs in range(4):
                a = s_ * 4 + qs
                bank = 4 + a // 3
                off = (a % 3) * 132
                P.mm(ps[:, bank, off:off + 129], pb[:, s_ * 512 + qs * 128:s_ * 512 + (qs + 1) * 128],
                     vtb[:, kt, hh, :], start=(kt == 0 and a % 3 == 0), stop=(kt == NKT - 1),
                     reads=[f"pT{pi_}", "vtb"], writes=[f"oacc{a}"], skip_group_check=True)
        if kt == NKT - 1:
            for qs in range(4):
                t = 4 * qc + qs
                eb = state["iep"] % 4
                state["iep"] += 1
                a1, a2 = qs, 4 + qs
                acc1 = ps[:, 4 + a1 // 3, (a1 % 3) * 132:(a1 % 3) * 132 + 129]
                acc2 = ps[:, 4 + a2 // 3, (a2 % 3) * 132:(a2 % 3) * 132 + 129]
                rk = f"rr{eb}"
                g1 = [f"oacc{i}" for i in range(8) if i // 3 == a1 // 3]
                g2 = [f"oacc{i}" for i in range(8) if i // 3 == a2 // 3]
                P.op("dve", lambda e, o=rr[eb][:, 0:1], i_=acc1[:, 128:129]: e.reciprocal(o, i_), reads=g1, writes=[rk])
                P.op("dve", lambda e, o=rr[eb][:, 1:2], i_=acc2[:, 128:129]: e.reciprocal(o, i_), reads=g2, writes=[rk])
                P.tt("dve", rr[eb][:, 2:3], rr[eb][:, 1:2], neglam, ALU.mult, reads=[rk, "neglam"], writes=[rk])
                P.ts("dve", t32[0], acc2[:, 0:128], rr[eb][:, 2:3], None, ALU.mult, reads=g2 + [rk], writes=["t320"])
                P.stt("dve", o32[eb], acc1[:, 0:128], rr[eb][:, 0:1], t32[0], ALU.mult, ALU.add,
                      reads=g1 + [rk, "t320"], writes=[f"o32{eb}"])
                P.tt("dve", t32[0], o32[eb], o32[eb], ALU.mult, reads=[f"o32{eb}"], writes=["t320"])
                P.op("dve", lambda e, o=ssq[:, t, h:h + 1], i_=t32[0]: e.tensor_reduce(o, i_, AX.X, ALU.add),
                     reads=["t320"], writes=["ssq"])
                P.cp("pool", obf[:, t, h * 128:(h + 1) * 128], o32[eb], reads=[f"o32{eb}"], writes=[f"obf{t}"])
            if qc == 3 and h % 2 == 1 and h + 1 < H:
                load_v((h + 1) // 2)

    load_v(0)
    pend = []
    load_head(0)
    for h in range(H):
        if h + 1 < H:
            load_head(h + 1)
        for qc in range(4):
            for kt in range(NKT):
                pi_ = emit_qk_exp(h, qc, kt)
                pend.append((h, qc, kt, pi_))
                if len(pend) > 2:
                    emit_pv(*pend.pop(0))
    while pend:
        emit_pv(*pend.pop(0))
    P.ts("dve", ssq, ssq, 1.0 / 128, EPS, ALU.mult, ALU.add, reads=["ssq"], writes=["ssq"])
    P.act(ssq, ssq, AF.Sqrt, reads=["ssq"], writes=["ssq"])
    P.op("dve", lambda e: e.reciprocal(ssq, ssq), reads=["ssq"], writes=["ssq"])
    for t in range(NT):
        ov = obf[:, t, :].rearrange("p (h c) -> p h c", c=128)
        P.tt("dve", ov, ov, ssq[:, t, :][:, :, None].broadcast_to([128, H, 128]), ALU.mult,
             reads=["ssq", f"obf{t}"], writes=[f"obf{t}"])
    if "obf" in DBG:
        P.load(DBG["obf"].rearrange("(t p) d -> p t d", p=128), obf, reads=[f"obf{t}" for t in range(NT)], writes=["dbg_obf"])
    P.fence()
    P.load(cx.xres, xp[0:TOK].rearrange("(t p) d -> p t d", p=128), writes=[f"xres{t}" for t in range(NT)])
    oT = A.view(kt_off, [8, TOK], BF16)
    wob = A.view(qt_off, [8, D], BF16)
    sub_s = A.alloc([1], F32)
    P.load(sub_s, subln, writes=["subs"])
    P.ts("dve", sub_s, sub_s, 1.0 - LAM_INIT0, None, ALU.mult, reads=["subs"], writes=["subs"])
    P.load(wob, w_out.rearrange("(kc p) n -> p kc n", p=128), writes=["wob"], eng="pool")
    for kc in range(8):
        P.ts("pool", wob[:, kc, :], wob[:, kc, :], sub_s[:, 0:1], None, ALU.mult,
             reads=["wob", "subs"], writes=["wob"])
        P.tt("pool", wob[:, kc, :], wob[:, kc, :], cx.g1bc, ALU.mult,
             reads=["wob", mt + "g1bc"], writes=["wob"])
    for t in range(NT):
        transpose_tok(cx, obf[:, t, :], f"obf{t}", oT[:, :, t * 128:(t + 1) * 128], f"oT{t}", eng=("dve" if t % 2 else "act"))
    proj_residual(cx, oT, [f"oT{t}" for t in range(NT)], wob, "wob")
    P.fence()
    A.release(m_att)
    stage_moe(cx, wr, br, wg, wu, wd, "e0")


def build_A():
    nc = bass.Bass("TRN2", target_bir_lowering=False)
    dram = lambda n, s, dt=F32, kind="ExternalInput": nc.dram_tensor(n, s, dt, kind=kind).ap()
    xp = dram("xp", [S, D])
    c_d = dram("c", [128, 8])
    adaw = dram("ada_w", [D, 6 * D]); adab = dram("ada_b", [6 * D])
    nmix = dram("norm_mix", [D]); nffn = dram("norm_ffn", [D])
    w_in = dram("w_in", [D, 3 * D]); w_out = dram("w_out", [D, D])
    lam = dram("lam", [4, 64]); subln = dram("subln", [128, 1])
    kaug = dram("kaug", [H, 8, 2, S], BF16); qaug = dram("qaug", [8, 2, TOK], BF16)
    dfix = dram("dfix", [128, H, 128], BF16)
    ident_d = dram("ident", [128, 128], BF16); identf_d = dram("identf", [128, 128])
    wr = dram("wr", [128, 8, 20]); br = dram("br", [20])
    wg = dram("wg", [NE, D, DE]); wu = dram("wu", [NE, D, DE]); wd = dram("wd", [NE, DE, D])
    xo = dram("xo", [TOK, D], F32, "ExternalOutput")
    if DBG.get("on"):
        DBG["obf"] = dram("d_obf", [TOK, D], BF16, "ExternalOutput")
        DBG["L"] = dram("d_L", [TOK, 20], F32, "ExternalOutput")
        DBG["gates"] = dram("d_gates", [TOK, 16], F32, "ExternalOutput")
        DBG["xmid"] = dram("d_xmid", [TOK, D], F32, "ExternalOutput")
    kscr = dram("kscr", [2, H, 64, S], BF16, "Internal")
    qscr = dram("qscr", [2, H, 64, TOK], BF16, "Internal")
    vscr = dram("vscr", [S // 128, 128, H, 129], BF16, "Internal")

    P = Prog(nc)
    with contextlib.ExitStack() as st:
        cx = Ctx()
        cx.P = P
        cx.A = A = Arena(nc, st)
        cx.ps = ps = st.enter_context(nc.psum_tensor("ps", [128, 8, 512], F32))
        setup_common(cx, ident_d, identf_d)
        stage_mod(cx, c_d, adaw, adab, nmix, nffn, "m0")
        mt = cx.modtag
        layer0_body(cx, xp, w_in, w_out, lam, subln, kaug, qaug, dfix, kscr, qscr, vscr, wr, br, wg, wu, wd)
        P.load(xo.rearrange("(t p) d -> p t d", p=128), cx.xres, reads=[f"xres{t}" for t in range(NT)], writes=["xo"])
        P.emit()
    return nc


def build_B():
    nc = bass.Bass("TRN2", target_bir_lowering=False)
    dram = lambda n, s, dt=F32, kind="ExternalInput": nc.dram_tensor(n, s, dt, kind=kind).ap()
    x1 = dram("x1", [S, D])
    c_d = dram("c", [128, 8])
    adaw = dram("ada_w", [D, 6 * D]); adab = dram("ada_b", [6 * D])
    nmix = dram("norm_mix", [D]); nffn = dram("norm_ffn", [D])
    fwin = dram("fwin", [D, 256])
    cs_d = dram("cs", [128, 2, 512], BF16)
    r1_d = dram("r1", [128, 256], BF16); r2_d = dram("r2", [128, 256], BF16)
    twr_d = dram("twr", [128, 128]); twi_d = dram("twi", [128, 128])
    bdc_d = dram("bdc", [128, 128], BF16); bds_d = dram("bds", [128, 128], BF16)
    ident_d = dram("ident", [128, 128], BF16); identf_d = dram("identf", [128, 128])
    fo = dram("fo", [S, 256], BF16, "ExternalOutput")
    P = Prog(nc)
    with contextlib.ExitStack() as st:
        cx = Ctx()
        cx.P = P
        cx.A = A = Arena(nc, st)
        cx.ps = ps = st.enter_context(nc.psum_tensor("ps", [128, 8, 512], F32))
        setup_common(cx, ident_d, identf_d)
        stage_mod(cx, c_d, adaw, adab, nmix, nffn, "m1")
        mt = cx.modtag
        cs = A.alloc([2, 512], BF16); r1 = A.alloc([256], BF16); r2 = A.alloc([256], BF16)
        twr = A.alloc([128], F32); twi = A.alloc([128], F32)
        bdc = A.alloc([128], BF16); bds = A.alloc([128], BF16)
        for ap_, d_, k_ in [(cs, cs_d, "cs"), (r1, r1_d, "r1"), (r2, r2_d, "r2"), (twr, twr_d, "twr"),
                            (twi, twi_d, "twi"), (bdc, bdc_d, "bdc"), (bds, bds_d, "bds")]:
            P.load(ap_, d_, writes=[k_])
        uT = A.alloc([2, S], BF16)
        m2 = A.mark()
        winb = A.alloc([8, 256], BF16)
        P.load(winb, fwin.rearrange("(kc p) n -> p kc n", p=128), writes=["winb"], eng="pool")
        norm_scratch(cx)
        xt = [A.alloc([D], F32) for _ in range(3)]
        hTc = [A.alloc([8, 512], BF16) for _ in range(2)]
        xv = x1.rearrange("(t p) d -> t p d", p=128)
        vk1 = [mt + "A1", mt + "B1"]
        ixt = 0
        ipb = 0
        for ch in range(S // 512):
            cb = ch % 2
            hk = f"hTc{cb}"
            for t in range(4):
                xb = xt[ixt % 3]
                xk = f"xt{ixt % 3}"
                ixt += 1
                P.load(xb, xv[ch * 4 + t], writes=[xk])
                norm_T(cx, xb, xk, cx.A1, cx.B1, vk1, hTc[cb][:, :, t * 128:(t + 1) * 128], hk)
            for mc in range(2):
                bank = ipb % 4
                ipb += 1
                for kc in range(8):
                    P.mm(ps[:, bank, :], winb[:, kc, mc * 128:(mc + 1) * 128], hTc[cb][:, kc, :], start=(kc == 0), stop=(kc == 7),
                         reads=["winb", hk], writes=[f"ps{bank}"])
                P.cp("act" if mc == 0 else "dve", uT[:, mc, ch * 512:(ch + 1) * 512], ps[:, bank, :],
                     reads=[f"ps{bank}"], writes=[f"uT{ch}"])
        P.fence()
        A.release(m2)
        zsb = A.alloc([2, 256, 64], BF16)
        zflat = zsb.rearrange("p r c n -> p (r c) n")
        for n2 in range(64):
            bank = n2 % 4
            for mc in range(2):
                P.mm(ps[:, bank, :], uT[:, mc, n2:S:64], cs[:, mc, :], start=(mc == 0), stop=(mc == 1),
                     reads=["cs"], writes=[f"ps{bank}"])
            P.cp("act" if n2 % 2 == 0 else "dve", zflat[:, :, n2], ps[:, bank, :], reads=[f"ps{bank}"], writes=["zsb"])
        P.fence()
        fsb = A.alloc([64, 256], BF16)
        p1 = [A.alloc([4, 2, 128], F32) for _ in range(2)]
        p2 = [A.alloc([4, 2, 128], F32) for _ in range(2)]
        apr = [A.alloc([4, 128], BF16) for _ in range(2)]
        api = [A.alloc([4, 128], BF16) for _ in range(2)]
        twr_b = twr[:, None, None, :].broadcast_to([128, 4, 2, 128])
        twi_b = twi[:, None, None, :].broadcast_to([128, 4, 2, 128])
        for grp in range(32):
            gb = grp % 2
            ab = 2 * gb
            for pi in range(4):
                pp = grp * 4 + pi
                bank = ab + pi // 2
                out = ps[:, bank, (pi % 2) * 256:(pi % 2) * 256 + 256]
                P.mm(out, zsb[:, 0, 2 * pp:2 * pp + 2, :].rearrange("p c n -> p (c n)"), r1, start=True, stop=False,
                     reads=["r1"], writes=[f"ps{bank}"], skip_group_check=True)
                P.mm(out, zsb[:, 1, 2 * pp:2 * pp + 2, :].rearrange("p c n -> p (c n)"), r2, start=False, stop=True,
                     reads=["r2"], writes=[f"ps{bank}"], skip_group_check=True)
            Av = ps[:, ab:ab + 2, :].rearrange("p a (b r k) -> p (a b) r k", r=2, k=128)
            P.tt("dve", p1[gb], Av, twr_b, ALU.mult, reads=[f"ps{ab}", f"ps{ab + 1}", "twr"], writes=[f"p1{gb}"])
            P.tt("dve", p2[gb], Av, twi_b, ALU.mult, reads=[f"ps{ab}", f"ps{ab + 1}", "twi"], writes=[f"p2{gb}"])
            P.tt("pool", apr[gb], p1[gb][:, :, 0, :], p2[gb][:, :, 1, :], ALU.subtract, reads=[f"p1{gb}", f"p2{gb}"], writes=[f"apr{gb}"])
            P.tt("pool", api[gb], p2[gb][:, :, 0, :], p1[gb][:, :, 1, :], ALU.add, reads=[f"p1{gb}", f"p2{gb}"], writes=[f"api{gb}"])
            fb = 4 + gb
            for pi in range(4):
                out = ps[:, fb, pi * 128:(pi + 1) * 128]
                P.mm(out, apr[gb][:, pi, :], bdc, start=True, stop=False, reads=[f"apr{gb}", "bdc"], writes=[f"ps{fb}"], skip_group_check=True)
                P.mm(out, api[gb][:, pi, :], bds, start=False, stop=True, reads=[f"api{gb}", "bds"], writes=[f"ps{fb}"], skip_group_check=True)
            src = ps[:, fb, :].rearrange("p (pi k c) -> p k pi c", pi=4, k=64, c=2)
            dst = fsb[:, :, 8 * grp:8 * grp + 8].rearrange("p k (pi c) -> p k pi c", c=2)
            P.cp("act" if grp % 2 == 0 else "dve", dst, src, reads=[f"ps{fb}"], writes=["fsb"])
        P.load(fo.rearrange("(k2 k1) c -> k1 k2 c", k1=128), fsb, reads=["fsb"], writes=["fo"])
        P.emit()
    return nc


def build_C():
    nc = bass.Bass("TRN2", target_bir_lowering=False)
    dram = lambda n, s, dt=F32, kind="ExternalInput": nc.dram_tensor(n, s, dt, kind=kind).ap()
    x1s = dram("x1s", [TOK, D])
    fsh = dram("fsh", [TOK, D], BF16)
    c_d = dram("c", [128, 8])
    adaw = dram("ada_w", [D, 6 * D]); adab = dram("ada_b", [6 * D])
    nmix = dram("norm_mix", [D]); nffn = dram("norm_ffn", [D])
    fwout = dram("fwout", [D, D])
    nfin = dram("norm_final", [D])
    ident_d = dram("ident", [128, 128], BF16); identf_d = dram("identf", [128, 128])
    wr = dram("wr", [128, 8, 20]); br = dram("br", [20])
    wg = dram("wg", [NE, D, DE]); wu = dram("wu", [NE, D, DE]); wd = dram("wd", [NE, DE, D])
    yo = dram("yo", [TOK, D], F32, "ExternalOutput")
    P = Prog(nc)
    with contextlib.ExitStack() as st:
        cx = Ctx()
        cx.P = P
        cx.A = A = Arena(nc, st)
        cx.ps = ps = st.enter_context(nc.psum_tensor("ps", [128, 8, 512], F32))
        setup_common(cx, ident_d, identf_d)
        stage_mod(cx, c_d, adaw, adab, nmix, nffn, "m1")
        mt = cx.modtag
        cx.xres = A.alloc([NT, D], F32)
        xkeys = [f"xres{t}" for t in range(NT)]
        P.load(cx.xres, x1s.rearrange("(t p) d -> p t d", p=128), writes=xkeys)
        m0 = A.mark()
        norm_scratch(cx)
        fsb = A.alloc([NT, D], BF16)
        fT = A.alloc([8, TOK], BF16)
        wob = A.alloc([8, D], BF16)
        P.load(fsb, fsh.rearrange("(t p) d -> p t d", p=128), writes=["fsb"])
        P.load(wob, fwout.rearrange("(kc p) n -> p kc n", p=128), writes=["wob"], eng="pool")
        for kc in range(8):
            P.tt("pool", wob[:, kc, :], wob[:, kc, :], cx.g1bc, ALU.mult, reads=["wob", mt + "g1bc"], writes=["wob"])
        for t in range(NT):
            transpose_tok(cx, fsb[:, t, :], "fsb", fT[:, :, t * 128:(t + 1) * 128], f"fT{t}", eng=("dve" if t % 2 else "act"))
        proj_residual(cx, fT, [f"fT{t}" for t in range(NT)], wob, "wob")
        P.fence()
        A.release(m0)
        stage_moe(cx, wr, br, wg, wu, wd, "e1")
        nfb = A.alloc([D], F32)
        P.load(nfb, nfin.partition_broadcast(128), writes=["nfb"])
        junk = A.alloc([D], BF16)
        fst = A.alloc([NT, 4], F32)
        ot = [A.alloc([D], F32) for _ in range(2)]
        yv = yo.rearrange("(t p) d -> t p d", p=128)
        for t in range(NT):
            P.act(junk, cx.xres[:, t, :], AF.Square, reads=[f"xres{t}"], writes=["fjunk", f"fst{t}"], accum_out=fst[:, t, 0:1])
            P.ts("dve", fst[:, t, 1:2], fst[:, t, 0:1], 1.0 / D, EPS, ALU.mult, ALU.add, reads=[f"fst{t}"], writes=[f"fst{t}"])
            P.act(fst[:, t, 2:3], fst[:, t, 1:2], AF.Sqrt, reads=[f"fst{t}"], writes=[f"fst{t}"])
            P.op("dve", lambda e, o=fst[:, t, 3:4], i_=fst[:, t, 2:3]: e.reciprocal(o, i_), reads=[f"fst{t}"], writes=[f"fst{t}"])
            ob = ot[t % 2]
            P.stt("dve", ob, cx.xres[:, t, :], fst[:, t, 3:4], nfb, ALU.mult, ALU.mult,
                  reads=[f"xres{t}", f"fst{t}", "nfb"], writes=[f"ot{t % 2}"])
            P.load(yv[t], ob, reads=[f"ot{t % 2}"], writes=[f"yo{t}"])
        P.emit()
    return nc


RG = [[0, 1, 2, 3], [4, 5, 6, 7]]
DEBUG_SKIP0 = False
DBG = {}


def fourier_body(cx, fwin, hTd, hTg, fd, tabs):
    P, A, ps = cx.P, cx.A, cx.ps
    mt = cx.modtag
    cs_d, r1_d, r2_d, twr_d, twi_d, bdc_d, bds_d = tabs
    m_f = A.mark()
    cs = A.alloc([2, 2, 256], BF16); r1 = A.alloc([256], BF16); r2 = A.alloc([256], BF16)
    twr = A.alloc([128], F32); twi = A.alloc([128], F32)
    bdc = A.alloc([128], BF16); bds = A.alloc([128], BF16)
    for ap_, d_, k_ in [(cs, cs_d, "cs"), (r1, r1_d, "r1"), (r2, r2_d, "r2"), (twr, twr_d, "twr"),
                        (twi, twi_d, "twi"), (bdc, bdc_d, "bdc"), (bds, bds_d, "bds")]:
        P.load(ap_, d_, writes=[k_])
    uT = A.alloc([2, S], BF16)
    u_off = A.last
    m2 = A.mark()
    norm_scratch(cx)
    hTo = A.alloc([8, TOK], BF16)
    vk1 = [mt + "A1", mt + "B1"]
    for t0 in range(0, NT, 4):
        ts4 = list(range(t0, t0 + 4))
        norm_T_multi(cx, [cx.xres[:, t, :] for t in ts4], [f"xres{t}" for t in ts4], cx.A1, cx.B1, vk1,
                     [hTo[:, :, t * 128:(t + 1) * 128] for t in ts4], [f"hTo{t}" for t in ts4])
    for i in range(4):
        P.load(hTd[i].rearrange("(kc p) t -> p kc t", p=128), hTo[:, :, i * 512:(i + 1) * 512],
               reads=[f"hTo{t}" for t in range(4 * i, 4 * i + 4)], writes=[f"hTd{i}"])
        P.cc("AllGather", RG, hTd[i], hTg[i], reads=[f"hTd{i}"], writes=[f"hTg{i}"])
    winb = A.alloc([8, 256], BF16)
    P.load(winb, fwin.rearrange("(kc p) n -> p kc n", p=128), writes=["winb"], eng="pool")
    hTc = [A.alloc([8, 512], BF16) for _ in range(2)]
    hTg_v = [g_.rearrange("(r kc p) t -> r p kc t", kc=8, p=128) for g_ in hTg]
    ipb = 0
    for it_ in range(S // 512):
        c4, r_ = it_ // 4, it_ % 4
        ch = 4 * r_ + c4
        cb = it_ % 2
        hk = f"hTc{cb}"
        P.load(hTc[cb], hTg_v[c4][r_], reads=[f"hTg{c4}"], writes=[hk])
        for mc in range(2):
            bank = ipb % 4
            ipb += 1
            for kc in range(8):
                P.mm(ps[:, bank, :], winb[:, kc, mc * 128:(mc + 1) * 128], hTc[cb][:, kc, :], start=(kc == 0), stop=(kc == 7),
                     reads=["winb", hk], writes=[f"ps{bank}"])
            P.cp("act" if mc == 0 else "dve", uT[:, mc, ch * 512:(ch + 1) * 512], ps[:, bank, :],
                 reads=[f"ps{bank}"], writes=[f"uT{ch}"])
    P.fence()
    A.release(m2)
    zsb = A.alloc([2, 128, 64], BF16)
    zflat = zsb.rearrange("p r c n -> p (r c) n")
    fsb = A.alloc([64, 256], BF16)
    p1 = A.alloc([4, 2, 128], F32)
    p2 = A.alloc([4, 2, 128], F32)
    apr = [A.alloc([4, 128], BF16) for _ in range(2)]
    api = [A.alloc([4, 128], BF16) for _ in range(2)]
    twr_b = twr[:, None, None, :].broadcast_to([128, 4, 2, 128])
    twi_b = twi[:, None, None, :].broadcast_to([128, 4, 2, 128])
    for hf in range(2):
        for n2 in range(64):
            bank = n2 % 2
            for mc in range(2):
                P.mm(ps[:, bank, 0:256], uT[:, mc, n2:S:64], cs[:, mc, hf, :], start=(mc == 0), stop=(mc == 1),
                     reads=["cs"], writes=[f"ps{bank}"])
            P.cp("act" if n2 % 2 == 0 else "dve", zflat[:, :, n2], ps[:, bank, 0:256], reads=[f"ps{bank}"], writes=["zsb"])
        for grp in range(16):
            gb = grp % 2
            ab = 2 + 2 * gb
            for pi in range(4):
                pp = grp * 4 + pi
                bank = ab + pi // 2
                out = ps[:, bank, (pi % 2) * 256:(pi % 2) * 256 + 256]
                P.mm(out, zsb[:, 0, 2 * pp:2 * pp + 2, :].rearrange("p c n -> p (c n)"), r1, start=True, stop=False,
                     reads=["r1", "zsb"], writes=[f"ps{bank}"], skip_group_check=True)
                P.mm(out, zsb[:, 1, 2 * pp:2 * pp + 2, :].rearrange("p c n -> p (c n)"), r2, start=False, stop=True,
                     reads=["r2", "zsb"], writes=[f"ps{bank}"], skip_group_check=True)
            Av = ps[:, ab:ab + 2, :].rearrange("p a (b r k) -> p (a b) r k", r=2, k=128)
            P.tt("dve", p1, Av, twr_b, ALU.mult, reads=[f"ps{ab}", f"ps{ab + 1}", "twr"], writes=["p1"])
            P.tt("dve", p2, Av, twi_b, ALU.mult, reads=[f"ps{ab}", f"ps{ab + 1}", "twi"], writes=["p2"])
            P.tt("pool", apr[gb], p1[:, :, 0, :], p2[:, :, 1, :], ALU.subtract, reads=["p1", "p2"], writes=[f"apr{gb}"])
            P.tt("pool", api[gb], p2[:, :, 0, :], p1[:, :, 1, :], ALU.add, reads=["p1", "p2"], writes=[f"api{gb}"])
            fb = 6 + gb
            for pi in range(4):
                out = ps[:, fb, pi * 128:(pi + 1) * 128]
                P.mm(out, apr[gb][:, pi, :], bdc, start=True, stop=False, reads=[f"apr{gb}", "bdc"], writes=[f"ps{fb}"], skip_group_check=True)
                P.mm(out, api[gb][:, pi, :], bds, start=False, stop=True, reads=[f"api{gb}", "bds"], writes=[f"ps{fb}"], skip_group_check=True)
            src = ps[:, fb, :].rearrange("p (pi k c) -> p k pi c", pi=4, k=64, c=2)
            c0 = hf * 128 + 8 * grp
            dst = fsb[:, :, c0:c0 + 8].rearrange("p k (pi c) -> p k pi c", c=2)
            P.cp("act" if grp % 2 == 0 else "dve", dst, src, reads=[f"ps{fb}"], writes=["fsb"])
    for i in range(4):
        P.load(fd[i].rearrange("(k2 k1) c -> k1 k2 c", k1=128), fsb[:, 16 * i:16 * (i + 1), :], reads=["fsb"], writes=[f"fd{i}"])
    P.fence()
    A.release(m_f)


def tail_body(cx, fd, fg, sel_d, fwout, nfin, wr, br, wg, wu, wd, yo):
    P, A, ps = cx.P, cx.A, cx.ps
    mt = cx.modtag
    for i in range(4):
        P.cc("AllGather", RG, fd[i], fg[i], reads=[f"fd{i}"], writes=[f"fg{i}"])
    m0 = A.mark()
    sel = A.alloc([4], F32)
    P.load(sel, sel_d, writes=["sel"])
    fsel = A.alloc([NT, D], BF16)
    wob = A.alloc([8, D], BF16)
    cand = A.alloc([NT, D], BF16)
    c_off = A.last
    P.load(wob, fwout.rearrange("(kc p) n -> p kc n", p=128), writes=["wob"], eng="pool")
    for kc in range(8):
        P.tt("pool", wob[:, kc, :], wob[:, kc, :], cx.g1bc, ALU.mult, reads=["wob", mt + "g1bc"], writes=["wob"])
    for r_ in range(4):
        fg_v = fg[r_].rearrange("(g t p) c -> g p t c", g=4, p=128)
        for g in range(4):
            P.load(cand[:, :, g * 256:(g + 1) * 256], fg_v[g], reads=[f"fg{r_}"], writes=["cand"])
        if r_ == 0:
            P.ts("dve", fsel, cand, sel[:, 0:1], None, ALU.mult, reads=["cand", "sel"], writes=["fsel"])
        else:
            P.stt("dve", fsel, cand, sel[:, r_:r_ + 1], fsel, ALU.mult, ALU.add, reads=["cand", "sel"], writes=["fsel"])
    P.fence()
    fT = A.view(c_off, [8, TOK], BF16)
    for t in range(NT):
        transpose_tok(cx, fsel[:, t, :], "fsel", fT[:, :, t * 128:(t + 1) * 128], f"fT{t}", eng=("dve" if t % 2 else "act"))
    proj_residual(cx, fT, [f"fT{t}" for t in range(NT)], wob, "wob")
    P.fence()
    A.release(m0)
    stage_moe(cx, wr, br, wg, wu, wd, "e1")
    nfb = A.alloc([D], F32)
    P.load(nfb, nfin.partition_broadcast(128), writes=["nfb"])
    junk = A.alloc([D], BF16)
    fst = A.alloc([NT, 4], F32)
    ot = [A.alloc([D], F32) for _ in range(2)]
    yv = yo.rearrange("(t p) d -> t p d", p=128)
    for t in range(NT):
        P.act(junk, cx.xres[:, t, :], AF.Square, reads=[f"xres{t}"], writes=["fjunk", f"fst{t}"], accum_out=fst[:, t, 0:1])
        P.ts("dve", fst[:, t, 1:2], fst[:, t, 0:1], 1.0 / D, EPS, ALU.mult, ALU.add, reads=[f"fst{t}"], writes=[f"fst{t}"])
        P.act(fst[:, t, 2:3], fst[:, t, 1:2], AF.Sqrt, reads=[f"fst{t}"], writes=[f"fst{t}"])
        P.op("dve", lambda e, o=fst[:, t, 3:4], i_=fst[:, t, 2:3]: e.reciprocal(o, i_), reads=[f"fst{t}"], writes=[f"fst{t}"])
        ob = ot[t % 2]
        P.stt("dve", ob, cx.xres[:, t, :], fst[:, t, 3:4], nfb, ALU.mult, ALU.mult,
              reads=[f"xres{t}", f"fst{t}", "nfb"], writes=[f"ot{t % 2}"])
        P.load(yv[t], ob, reads=[f"ot{t % 2}"], writes=[f"yo{t}"])


def build_F():
    nc = bass.Bass("TRN2", target_bir_lowering=False)
    dram = lambda n, s, dt=F32, kind="ExternalInput": nc.dram_tensor(n, s, dt, kind=kind).ap()
    xp = dram("xp", [S, D])
    c_d = dram("c", [128, 8])
    adaw = dram("ada_w", [2, D, 6 * D]); adab = dram("ada_b", [2, 6 * D])
    nmix = dram("norm_mix", [2, D]); nffn = dram("norm_ffn", [2, D])
    if not DEBUG_SKIP0:
        w_in = dram("w_in", [D, 3 * D]); w_out = dram("w_out", [D, D])
        lam = dram("lam", [4, 64]); subln = dram("subln", [128, 1])
        kaug = dram("kaug", [H, 8, 2, S], BF16); qaug = dram("qaug", [8, 2, TOK], BF16)
        dfix = dram("dfix", [128, H, 128], BF16)
    ident_d = dram("ident", [128, 128], BF16); identf_d = dram("identf", [128, 128])
    wr = dram("wr", [2, 128, 8, 20]); br = dram("br", [2, 20])
    wg = dram("wg", [2, NE, D, DE]); wu = dram("wu", [2, NE, D, DE]); wd = dram("wd", [2, NE, DE, D])
    fwin = dram("fwin", [D, 256]); fwout = dram("fwout", [D, D]); nfin = dram("norm_final", [D])
    cs_d = dram("cs", [128, 2, 2, 256], BF16)
    r1_d = dram("r1", [128, 256], BF16); r2_d = dram("r2", [128, 256], BF16)
    twr_d = dram("twr", [128, 128]); twi_d = dram("twi", [128, 128])
    bdc_d = dram("bdc", [128, 128], BF16); bds_d = dram("bds", [128, 128], BF16)
    sel_d = dram("sel", [128, 4])
    yo = dram("yo", [TOK, D], F32, "ExternalOutput")
    if not DEBUG_SKIP0:
        kscr = dram("kscr", [2, H, 64, S], BF16, "Internal")
        qscr = dram("qscr", [2, H, 64, TOK], BF16, "Internal")
        vscr = dram("vscr", [S // 128, 128, H, 129], BF16, "Internal")
    hTd = [dram(f"hTd{i}", [D, 512], BF16, "Internal") for i in range(4)]
    hTg = [dram(f"hTg{i}", [4 * D, 512], BF16, "Internal") for i in range(4)]
    fd = [dram(f"fd{i}", [TOK, 256], BF16, "Internal") for i in range(4)]
    fg = [dram(f"fg{i}", [4 * TOK, 256], BF16, "Internal") for i in range(4)]
    P = Prog(nc)
    with contextlib.ExitStack() as st:
        cx = Ctx()
        cx.P = P
        cx.A = A = Arena(nc, st)
        cx.ps = st.enter_context(nc.psum_tensor("ps", [128, 8, 512], F32))
        setup_common(cx, ident_d, identf_d)
        stage_mod(cx, c_d, adaw[0], adab[0], nmix[0], nffn[0], "m0")
        if DEBUG_SKIP0:
            cx.xres = A.alloc([NT, D], F32)
            P.load(cx.xres, xp[0:TOK].rearrange("(t p) d -> p t d", p=128), writes=[f"xres{t}" for t in range(NT)])
        else:
            layer0_body(cx, xp, w_in, w_out, lam, subln, kaug, qaug, dfix, kscr, qscr, vscr, wr[0], br[0], wg[0], wu[0], wd[0])
        stage_mod(cx, c_d, adaw[1], adab[1], nmix[1], nffn[1], "m1")
        fourier_body(cx, fwin, hTd, hTg, fd, (cs_d, r1_d, r2_d, twr_d, twi_d, bdc_d, bds_d))
        tail_body(cx, fd, fg, sel_d, fwout, nfin, wr[1], br[1], wg[1], wu[1], wd[1], yo)
        P.emit()
    return nc


def _bf(a):
    return np.asarray(a, dtype=np.float32).astype(ml_dtypes.bfloat16)


def _consts():
    ident = _bf(np.eye(128))
    identf = np.eye(128, dtype=np.float32)
    return ident, identf


def _attn_tables(j):
    own = np.arange(TOK * j, TOK * (j + 1))
    others = np.concatenate([np.arange(0, TOK * j), np.arange(TOK * (j + 1), S)])
    perm = np.concatenate([own, others])
    pos = perm.astype(np.float64)
    kt_abs = np.floor(pos / 128) * 128
    kr = pos - kt_abs
    slopes = 2.0 ** (-(np.arange(H) + 1.0))
    kaug = np.zeros((H, 8, 2, S), np.float64)
    after = (perm >= TOK * (j + 1))
    own_m = np.arange(S) < TOK
    use2 = np.where(own_m | after, 1.0, 0.0)
    for h in range(H):
        sl = slopes[h]
        set1 = np.stack([np.full(S, -sl), np.full(S, -sl), sl * kt_abs, sl * kr])
        kaug[h, 0:4, :, :] = set1[:, None, :]
        kaug[h, 4:8, :, :] = (-2.0 * set1 * use2[None, :])[:, None, :]
    qpos = own.astype(np.float64)
    qt_abs = np.floor(qpos / 128) * 128
    qr = qpos - qt_abs
    q4 = np.stack([qt_abs, qr, np.ones(TOK), np.ones(TOK)])
    qaug = np.zeros((8, 2, TOK), np.float64)
    qaug[0:4] = q4[:, None, :]
    qaug[4:8] = q4[:, None, :]
    krr = np.arange(128)[:, None]
    qrr = np.arange(128)[None, :]
    dfix = np.zeros((128, H, 128), np.float64)
    for h in range(H):
        dfix[:, h, :] = -2.0 * slopes[h] * np.maximum(krr - qrr, 0)
    return perm, _bf(kaug), _bf(qaug), _bf(dfix)


_NC_CACHE = {}


def _get(name, fn):
    if name not in _NC_CACHE:
        _NC_CACHE[name] = fn()
    return _NC_CACHE[name]


def run_A(inp):
    ident, identf = _consts()
    maps = []
    for c in range(NCORE):
        b, j = c // 4, c % 4
        perm, kaug, qaug, dfix = _attn_tables(j)
        lam = np.stack([inp["attn_lam_q1"][0], inp["attn_lam_k1"][0], inp["attn_lam_q2"][0], inp["attn_lam_k2"][0]])
        wr = np.concatenate([inp["router_group_w"][0], inp["router_expert_w"][0].reshape(D, 16)], axis=1)
        br = np.concatenate([inp["router_group_b"][0], inp["router_expert_b"][0].reshape(16)])
        maps.append({
            "xp": np.ascontiguousarray(inp["x"][b][perm]),
            "c": np.ascontiguousarray(inp["c"][b].reshape(8, 128).T),
            "ada_w": inp["ada_w"][0], "ada_b": inp["ada_b"][0],
            "norm_mix": inp["norm_mix"][0], "norm_ffn": inp["norm_ffn"][0],
            "w_in": inp["attn_w_in"][0], "w_out": inp["attn_w_out"][0],
            "lam": np.ascontiguousarray(lam), "subln": np.ascontiguousarray(inp["attn_subln"][0].reshape(128, 1)),
            "kaug": kaug, "qaug": qaug, "dfix": dfix, "ident": ident, "identf": identf,
            "wr": np.ascontiguousarray(wr.reshape(8, 128, 20).transpose(1, 0, 2)), "br": np.ascontiguousarray(br),
            "wg": inp["expert_w_gate"][0], "wu": inp["expert_w_up"][0], "wd": inp["expert_w_down"][0],
        })
    nc = _get("A", build_A)
    res = run_bass_kernel_spmd(nc, maps, core_ids=list(range(NCORE)))
    x1 = np.zeros((B, S, D), np.float32)
    for c in range(NCORE):
        b, j = c // 4, c % 4
        x1[b, TOK * j:TOK * (j + 1)] = res.results[c]["xo"]
    if DBG.get("on"):
        DBG["res"] = [{k: np.asarray(v) for k, v in r.items()} for r in res.results]
    return x1


def _fourier_tables():
    m = np.arange(256)[:, None].astype(np.float64); l = np.arange(256)[None, :].astype(np.float64)
    ang = 2 * np.pi * m * l / 256
    cs = np.concatenate([np.cos(ang), -np.sin(ang)], axis=1) / 16.0
    cs = cs.reshape(2, 128, 512).transpose(1, 0, 2)
    n1 = np.arange(128)[:, None].astype(np.float64); k1 = np.arange(128)[None, :].astype(np.float64)
    a = 2 * np.pi * n1 * k1 / 128
    r1 = np.concatenate([np.cos(a), -np.sin(a)], axis=1)
    r2 = np.concatenate([np.sin(a), np.cos(a)], axis=1)
    n2 = (np.arange(128) % 64)[:, None].astype(np.float64)
    tw = 2 * np.pi * n2 * k1 / 8192
    twr = np.cos(tw); twi = -np.sin(tw)
    bdc = np.zeros((128, 128)); bds = np.zeros((128, 128))
    sc = 1.0 / math.sqrt(8192.0)
    for c in range(2):
        nn = np.arange(64)[:, None].astype(np.float64); kk = np.arange(64)[None, :].astype(np.float64)
        a2 = 2 * np.pi * nn * kk / 64
        bdc[c * 64:(c + 1) * 64, c::2] = np.cos(a2) * sc
        bds[c * 64:(c + 1) * 64, c::2] = np.sin(a2) * sc
    return (_bf(cs), _bf(r1), _bf(r2), twr.astype(np.float32), twi.astype(np.float32), _bf(bdc), _bf(bds))


def _moe_router(inp, i):
    wr = np.concatenate([inp["router_group_w"][i], inp["router_expert_w"][i].reshape(D, 16)], axis=1)
    br = np.concatenate([inp["router_group_b"][i], inp["router_expert_b"][i].reshape(16)])
    return np.ascontiguousarray(wr.reshape(8, 128, 20).transpose(1, 0, 2)), np.ascontiguousarray(br)


def run_B(inp, x1):
    ident, identf = _consts()
    cs, r1, r2, twr, twi, bdc, bds = _fourier_tables()
    maps = []
    for c in range(NCORE):
        b, g = c // 4, c % 4
        maps.append({
            "x1": np.ascontiguousarray(x1[b]),
            "c": np.ascontiguousarray(inp["c"][b].reshape(8, 128).T),
            "ada_w": inp["ada_w"][1], "ada_b": inp["ada_b"][1],
            "norm_mix": inp["norm_mix"][1], "norm_ffn": inp["norm_ffn"][1],
            "fwin": np.ascontiguousarray(inp["fourier_w_in"][0][:, 256 * g:256 * (g + 1)]),
            "cs": cs, "r1": r1, "r2": r2, "twr": twr, "twi": twi, "bdc": bdc, "bds": bds,
            "ident": ident, "identf": identf,
        })
    nc = _get("B", build_B)
    res = run_bass_kernel_spmd(nc, maps, core_ids=list(range(NCORE)))
    f = np.zeros((B, S, D), ml_dtypes.bfloat16)
    for c in range(NCORE):
        b, g = c // 4, c % 4
        f[b, :, 256 * g:256 * (g + 1)] = res.results[c]["fo"]
    return f


def run_C(inp, x1, f):
    ident, identf = _consts()
    wr, br = _moe_router(inp, 1)
    maps = []
    for c in range(NCORE):
        b, j = c // 4, c % 4
        maps.append({
            "x1s": np.ascontiguousarray(x1[b, TOK * j:TOK * (j + 1)]),
            "fsh": np.ascontiguousarray(f[b, TOK * j:TOK * (j + 1)]),
            "c": np.ascontiguousarray(inp["c"][b].reshape(8, 128).T),
            "ada_w": inp["ada_w"][1], "ada_b": inp["ada_b"][1],
            "norm_mix": inp["norm_mix"][1], "norm_ffn": inp["norm_ffn"][1],
            "fwout": inp["fourier_w_out"][0], "norm_final": inp["norm_final"],
            "ident": ident, "identf": identf, "wr": wr, "br": br,
            "wg": inp["expert_w_gate"][1], "wu": inp["expert_w_up"][1], "wd": inp["expert_w_down"][1],
        })
    nc = _get("C", build_C)
    res = run_bass_kernel_spmd(nc, maps, core_ids=list(range(NCORE)))
    out = np.zeros((B, S, D), np.float32)
    for c in range(NCORE):
        b, j = c // 4, c % 4
        out[b, TOK * j:TOK * (j + 1)] = res.results[c]["yo"]
    return out


def run_F(inp):
    ident, identf = _consts()
    cs, r1, r2, twr, twi, bdc, bds = _fourier_tables()
    csf = np.asarray(cs).reshape(128, 2, 2, 2, 128).transpose(0, 1, 3, 2, 4).reshape(128, 2, 2, 256)
    csf = np.ascontiguousarray(csf)
    lam = np.ascontiguousarray(np.stack([inp["attn_lam_q1"][0], inp["attn_lam_k1"][0],
                                         inp["attn_lam_q2"][0], inp["attn_lam_k2"][0]]))
    wr0, br0 = _moe_router(inp, 0)
    wr1, br1 = _moe_router(inp, 1)
    wr = np.ascontiguousarray(np.stack([wr0, wr1])); br = np.ascontiguousarray(np.stack([br0, br1]))
    maps = []
    for c in range(NCORE):
        b, j = c // 4, c % 4
        perm, kaug, qaug, dfix = _attn_tables(j)
        sel = np.zeros((128, 4), np.float32)
        sel[:, j] = 1.0
        maps.append({
            "xp": np.ascontiguousarray(inp["x"][b][perm]),
            "c": np.ascontiguousarray(inp["c"][b].reshape(8, 128).T),
            "ada_w": inp["ada_w"], "ada_b": inp["ada_b"],
            "norm_mix": inp["norm_mix"], "norm_ffn": inp["norm_ffn"],
            "w_in": inp["attn_w_in"][0], "w_out": inp["attn_w_out"][0],
            "lam": lam, "subln": np.ascontiguousarray(inp["attn_subln"][0].reshape(128, 1)),
            "kaug": kaug, "qaug": qaug, "dfix": dfix, "ident": ident, "identf": identf,
            "wr": wr, "br": br,
            "wg": inp["expert_w_gate"], "wu": inp["expert_w_up"], "wd": inp["expert_w_down"],
            "fwin": np.ascontiguousarray(inp["fourier_w_in"][0][:, 256 * j:256 * (j + 1)]),
            "fwout": inp["fourier_w_out"][0], "norm_final": inp["norm_final"],
            "cs": csf, "r1": r1, "r2": r2, "twr": twr, "twi": twi, "bdc": bdc, "bds": bds, "sel": sel,
        })
    nc = _get("F", build_F)
    if DEBUG_SKIP0:
        drop = {"w_in", "w_out", "lam", "subln", "kaug", "qaug", "dfix"}
        maps = [{k: v for k, v in m.items() if k not in drop} for m in maps]
    res = run_bass_kernel_spmd(nc, maps, core_ids=list(range(NCORE)))
    out = np.zeros((B, S, D), np.float32)
    for c in range(NCORE):
        b, j = c // 4, c % 4
        out[b, TOK * j:TOK * (j + 1)] = res.results[c]["yo"]
    return out


def kernel(**inputs):
    inp = {k: np.asarray(v, dtype=np.float32) for k, v in inputs.items()}
    return run_F(inp)
```
